# Optimizing a Trainium2 kernel written in Bass

```python
import math
import jax
import jax.numpy as jnp
from jax import lax
import numpy as np

D_MODEL = 1024
BATCH = 8
SEQ = 2048
DEPTH = 1

N_ATTN_HEADS = 8
HEAD_DIM = 64
D_ATTN = N_ATTN_HEADS * HEAD_DIM
MOBA_BLOCK = 256
MOBA_TOPK = 3
MOBA_QUERY_CHUNK = 32
SSM_GROUP = 16
D_SSM = D_MODEL - D_ATTN
N_SSM_GROUPS = D_SSM // SSM_GROUP
SSM_STATE = 64
DT_MIN = 1e-3
DT_MAX = 1e-1
D_MIX = D_ATTN + D_SSM
D_IN = 3 * D_ATTN + D_SSM
D_FF = 256 * ((8 * D_MODEL // 3 + 255) // 256)
RMS_EPS = 1e-6
NEG_INF = -1e30

kernel_name = 'hymba_moba_s5_macaron_layer'


def rmsnorm(x, g):
    xf = x.astype(jnp.float32)
    y = xf * lax.rsqrt(jnp.mean(xf * xf, axis=-1, keepdims=True) + RMS_EPS)
    return (y * g.astype(jnp.float32)).astype(x.dtype)


def swiglu(x, w_gate, w_up, w_down):
    return (jax.nn.silu(x @ w_gate) * (x @ w_up)) @ w_down


def alibi_slopes(n_heads):
    return jnp.asarray(2.0 ** (-8.0 * np.arange(1, n_heads + 1) / n_heads), dtype=jnp.float32)


def moba_attention(q, k, v):
    bsz, s, nh, dh = q.shape
    L = MOBA_BLOCK
    C = MOBA_QUERY_CHUNK
    nb = -(-s // L)
    sp = nb * L
    nc = sp // C
    topk = min(MOBA_TOPK, nb)
    scale = 1.0 / math.sqrt(dh)
    slopes = alibi_slopes(nh)
    padw = ((0, 0), (0, sp - s), (0, 0), (0, 0))
    q, k, v = (jnp.pad(t, padw).transpose(0, 2, 1, 3) for t in (q, k, v))
    kb = k.reshape(bsz, nh, nb, L, dh)
    vb = v.reshape(bsz, nh, nb, L, dh)

    qblk = jnp.arange(sp) // L
    kmean = jnp.mean(kb.astype(jnp.float32), axis=3)
    gate = jnp.einsum('bhtd,bhnd->bhtn', q.astype(jnp.float32), kmean)
    past = jnp.arange(nb)[None, :] < qblk[:, None]
    gate = jnp.where(past, gate, NEG_INF)
    _, sel = lax.top_k(gate, topk)

    q_c = q.reshape(bsz, nh, nc, C, dh).transpose(2, 0, 1, 3, 4)
    sel_c = sel.reshape(bsz, nh, nc, C, topk).transpose(2, 0, 1, 3, 4)
    b_ix = jnp.arange(bsz)[:, None, None, None]
    h_ix = jnp.arange(nh)[None, :, None, None]
    key_off = jnp.arange(L)

    def chunk(args):
        qc, selc, ci = args
        t = ci * C + jnp.arange(C)
        own = (ci * C) // L
        k_sel = kb[b_ix, h_ix, selc]
        v_sel = vb[b_ix, h_ix, selc]
        k_own = lax.dynamic_index_in_dim(kb, own, axis=2, keepdims=False)
        v_own = lax.dynamic_index_in_dim(vb, own, axis=2, keepdims=False)
        s_sel = jnp.einsum('bhcd,bhckld->bhckl', qc, k_sel).astype(jnp.float32) * scale
        dist_sel = (t[None, None, :, None, None] - (selc[..., None] * L + key_off)).astype(jnp.float32)
        s_sel = s_sel - slopes[None, :, None, None, None] * dist_sel
        slot_ok = jnp.arange(topk)[None, :] < (t // L)[:, None]
        s_sel = jnp.where(slot_ok[None, None, :, :, None], s_sel, NEG_INF)
        s_own = jnp.einsum('bhcd,bhld->bhcl', qc, k_own).astype(jnp.float32) * scale
        dist_own = t[:, None] - (own * L + key_off)[None, :]
        s_own = jnp.where(dist_own >= 0,
                          s_own - slopes[None, :, None, None] * dist_own.astype(jnp.float32),
                          NEG_INF)
        scores = jnp.concatenate([s_sel.reshape(bsz, nh, C, topk * L), s_own], axis=-1)
        p = jax.nn.softmax(scores, axis=-1).astype(v_sel.dtype)
        p_sel = p[..., :topk * L].reshape(bsz, nh, C, topk, L)
        p_own = p[..., topk * L:]
        return (jnp.einsum('bhckl,bhckld->bhcd', p_sel, v_sel)
                + jnp.einsum('bhcl,bhld->bhcd', p_own, v_own))

    out = lax.map(chunk, (q_c, sel_c, jnp.arange(nc)))
    out = out.transpose(1, 2, 0, 3, 4).reshape(bsz, nh, sp, dh)[:, :, :s]
    return out.transpose(0, 2, 1, 3)


def s5_ssm(u, lam_re, lam_im, log_dt, b_re, b_im, c_re, c_im, d_skip, w_glu, b_glu):
    bsz, s, _ = u.shape
    f32 = jnp.float32
    uf = u.astype(f32).reshape(bsz, s, N_SSM_GROUPS, SSM_GROUP)
    lr = lam_re.astype(f32)
    li = lam_im.astype(f32)
    dt = jnp.exp(log_dt.astype(f32))[:, None]
    mag = jnp.exp(lr * dt)
    ang = li * dt
    a_re = mag * jnp.cos(ang)
    a_im = mag * jnp.sin(ang)
    den = lr * lr + li * li
    f_re = ((a_re - 1.0) * lr + a_im * li) / den
    f_im = (a_im * lr - (a_re - 1.0) * li) / den
    br = b_re.astype(f32)
    bi = b_im.astype(f32)
    bb_re = f_re[..., None] * br - f_im[..., None] * bi
    bb_im = f_re[..., None] * bi + f_im[..., None] * br
    x_re = jnp.einsum('bsgh,gph->bsgp', uf, bb_re)
    x_im = jnp.einsum('bsgh,gph->bsgp', uf, bb_im)
    a_re_s = jnp.broadcast_to(a_re, (1, s) + a_re.shape)
    a_im_s = jnp.broadcast_to(a_im, (1, s) + a_im.shape)

    def combine(e1, e2):
        a1r, a1i, x1r, x1i = e1
        a2r, a2i, x2r, x2i = e2
        return (a1r * a2r - a1i * a2i,
                a1r * a2i + a1i * a2r,
                a2r * x1r - a2i * x1i + x2r,
                a2r * x1i + a2i * x1r + x2i)

    _, _, h_re, h_im = lax.associative_scan(combine, (a_re_s, a_im_s, x_re, x_im), axis=1)
    y = (jnp.einsum('bsgp,ghp->bsgh', h_re, c_re.astype(f32))
         - jnp.einsum('bsgp,ghp->bsgh', h_im, c_im.astype(f32))
         + d_skip.astype(f32) * uf)
    y = jax.nn.gelu(y.reshape(bsz, s, D_SSM))
    y = y * jax.nn.sigmoid(y @ w_glu.astype(f32) + b_glu.astype(f32))
    return y.astype(u.dtype)


def setup_inputs(seed: int = 0) -> dict:
    key = jax.random.key(seed)
    ks = jax.random.split(key, 32)
    f32 = jnp.float32
    G, P, Hg = N_SSM_GROUPS, SSM_STATE, SSM_GROUP

    def nrm(k, shape, scale):
        return jax.random.normal(k, shape, f32) * scale

    def gain(k, n):
        return 1.0 + 0.02 * jax.random.normal(k, (DEPTH, n), f32)

    lam_re = -0.5 + 0.01 * jax.random.normal(ks[8], (DEPTH, G, P), f32)
    lam_im = math.pi * jnp.arange(P, dtype=f32)[None, None, :] + 0.01 * jax.random.normal(ks[9], (DEPTH, G, P), f32)
    log_dt = jax.random.uniform(ks[10], (DEPTH, G), f32, math.log(DT_MIN), math.log(DT_MAX))
    return {
        'x': jax.random.normal(ks[0], (BATCH, SEQ, D_MODEL), f32),
        'ffn1_pre_g': gain(ks[1], D_MODEL),
        'ffn1_w_gate': nrm(ks[2], (DEPTH, D_MODEL, D_FF), D_MODEL ** -0.5),
        'ffn1_w_up': nrm(ks[3], (DEPTH, D_MODEL, D_FF), D_MODEL ** -0.5),
        'ffn1_w_down': nrm(ks[4], (DEPTH, D_FF, D_MODEL), D_FF ** -0.5),
        'ffn1_post_g': gain(ks[5], D_MODEL),
        'mix_pre_g': gain(ks[6], D_MODEL),
        'w_in': nrm(ks[7], (DEPTH, D_MODEL, D_IN), D_MODEL ** -0.5),
        'lam_re': lam_re,
        'lam_im': lam_im,
        'log_dt': log_dt,
        'b_re': nrm(ks[11], (DEPTH, G, P, Hg), (2.0 * Hg) ** -0.5),
        'b_im': nrm(ks[12], (DEPTH, G, P, Hg), (2.0 * Hg) ** -0.5),
        'c_re': nrm(ks[13], (DEPTH, G, Hg, P), (2.0 * P) ** -0.5),
        'c_im': nrm(ks[14], (DEPTH, G, Hg, P), (2.0 * P) ** -0.5),
        'd_skip': nrm(ks[15], (DEPTH, G, Hg), 1.0),
        'w_glu': nrm(ks[16], (DEPTH, D_SSM, D_SSM), D_SSM ** -0.5),
        'b_glu': nrm(ks[17], (DEPTH, D_SSM), 0.01),
        'attn_out_g': gain(ks[18], D_ATTN),
        'ssm_out_g': gain(ks[19], D_SSM),
        'w_out': nrm(ks[20], (DEPTH, D_MIX, D_MODEL), D_MIX ** -0.5),
        'mix_post_g': gain(ks[21], D_MODEL),
        'ffn2_pre_g': gain(ks[22], D_MODEL),
        'ffn2_w_gate': nrm(ks[23], (DEPTH, D_MODEL, D_FF), D_MODEL ** -0.5),
        'ffn2_w_up': nrm(ks[24], (DEPTH, D_MODEL, D_FF), D_MODEL ** -0.5),
        'ffn2_w_down': nrm(ks[25], (DEPTH, D_FF, D_MODEL), D_FF ** -0.5),
        'ffn2_post_g': gain(ks[26], D_MODEL),
    }


def reference(x, ffn1_pre_g, ffn1_w_gate, ffn1_w_up, ffn1_w_down, ffn1_post_g,
              mix_pre_g, w_in, lam_re, lam_im, log_dt, b_re, b_im, c_re, c_im, d_skip,
              w_glu, b_glu, attn_out_g, ssm_out_g, w_out, mix_post_g,
              ffn2_pre_g, ffn2_w_gate, ffn2_w_up, ffn2_w_down, ffn2_post_g):
    bsz, s, _ = x.shape
    h = x
    for l in range(DEPTH):
        f = swiglu(rmsnorm(h, ffn1_pre_g[l]), ffn1_w_gate[l], ffn1_w_up[l], ffn1_w_down[l])
        h = h + 0.5 * rmsnorm(f, ffn1_post_g[l])
        u = rmsnorm(h, mix_pre_g[l])
        proj = u @ w_in[l]
        q = proj[..., :D_ATTN].reshape(bsz, s, N_ATTN_HEADS, HEAD_DIM)
        k = proj[..., D_ATTN:2 * D_ATTN].reshape(bsz, s, N_ATTN_HEADS, HEAD_DIM)
        v = proj[..., 2 * D_ATTN:3 * D_ATTN].reshape(bsz, s, N_ATTN_HEADS, HEAD_DIM)
        us = proj[..., 3 * D_ATTN:]
        attn = moba_attention(q, k, v).reshape(bsz, s, D_ATTN)
        ssm = s5_ssm(us, lam_re[l], lam_im[l], log_dt[l], b_re[l], b_im[l], c_re[l], c_im[l],
                     d_skip[l], w_glu[l], b_glu[l])
        merged = jnp.concatenate([rmsnorm(attn, attn_out_g[l]), rmsnorm(ssm, ssm_out_g[l])], axis=-1)
        h = h + rmsnorm(merged @ w_out[l], mix_post_g[l])
        f = swiglu(rmsnorm(h, ffn2_pre_g[l]), ffn2_w_gate[l], ffn2_w_up[l], ffn2_w_down[l])
        h = h + 0.5 * rmsnorm(f, ffn2_post_g[l])
    return h
```

```python
import contextlib
import math
import numpy as np
import concourse.bass as bass
import concourse.mybir as mybir
from concourse.bass_utils import run_bass_kernel_spmd

F32 = mybir.dt.float32
BF = mybir.dt.bfloat16
I32 = mybir.dt.int32
AF = mybir.ActivationFunctionType
ALU = mybir.AluOpType
AX = mybir.AxisListType
PI = math.pi
S = 2048
DFF = 2816
NFT = 22
EPS = 1e-6
BIG = 30000.0

G_F1PRE, G_F1POST, G_MIXPRE, G_ATT, G_SSM, G_MIXPOST, G_F2PRE, G_F2POST, G_BGLU = 0, 8, 16, 24, 28, 32, 40, 48, 56


class Eng:
    def __init__(self, nc, e, name, es):
        self.e, self.name = e, name
        self.sem = es.enter_context(nc.semaphore("s_" + name))
        self.cnt = 0
        self.seen = {}


class DS:
    def __init__(self, nc, name, es):
        self.sem = es.enter_context(nc.semaphore(name))
        self.cnt = 0
        self.name = name


class Buf:
    def __init__(self, t, ds=None, dj=False, psum=False):
        self.psum = psum
        self.dj = dj
        self.t = t
        self.w = None
        self.r = {}
        self.ds = ds


class Trk:
    def __init__(self, nc, es):
        self.nc = nc
        self.es = es
        self.PE = Eng(nc, nc.tensor, "pe", es)
        self.ACT = Eng(nc, nc.scalar, "act", es)
        self.DVE = Eng(nc, nc.vector, "dve", es)
        self.POOL = Eng(nc, nc.gpsimd, "pool", es)
        self.SP = Eng(nc, nc.sync, "sp", es)
        self.engs = [self.PE, self.ACT, self.DVE, self.POOL, self.SP]
        self.dss = []
        self.nds = 0
        self.n = 0
        self.live = True
        self.cut = CUT

    def newds(self):
        self.nds += 1
        d = DS(self.nc, "d%d" % self.nds, self.es)
        self.dss.append(d)
        return d

    def wait(self, E, tok):
        src, val = tok
        if src is E and E is self.PE:
            return
        k = id(src)
        if E.seen.get(k, 0) >= val:
            return
        E.e.wait_ge(src.sem, val)
        E.seen[k] = val

    def pre(self, E, reads=(), writes=()):
        if self.n >= self.cut:
            self.live = False
        if not self.live:
            return
        for b in reads:
            if b.w is not None:
                self.wait(E, b.w)
            if b.psum:
                for src, val in list(b.r.values()):
                    if src is not E:
                        self.wait(E, (src, val))
        for b in writes:
            if b.w is not None and (b.w[0] is not E or not b.dj):
                self.wait(E, b.w)
            for src, val in list(b.r.values()):
                self.wait(E, (src, val))

    def _reg(self, tok, reads, writes):
        for b in reads:
            b.r[id(tok[0])] = tok
        for b in writes:
            b.w = tok
            b.r = {}

    def post(self, E, inst, reads=(), writes=()):
        if not self.live:
            return
        self.n += 1
        E.cnt += 1
        inst.then_inc(E.sem, 1)
        self._reg((E, E.cnt), reads, writes)

    def op(self, E, fn, reads=(), writes=()):
        self.pre(E, reads, writes)
        if not self.live:
            return
        inst = fn()
        self.post(E, inst, reads, writes)

    def dma(self, Q, out, in_, ds, reads=(), writes=(), **kw):
        self.pre(Q, reads, writes)
        if not self.live:
            return
        self.n += 1
        inst = Q.e.dma_start(out=out, in_=in_, **kw)
        ds.cnt += 16
        inst.then_inc(ds.sem, 16)
        self._reg((ds, ds.cnt), reads, writes)

    def mark(self, name):
        MARKS.append((name, self.n))

    def barrier(self):
        if not self.live:
            return
        for E in self.engs:
            for Fe in self.engs:
                if Fe is not E and Fe.cnt > 0:
                    self.wait(E, (Fe, Fe.cnt))
            for d in self.dss:
                if d.cnt > 0:
                    self.wait(E, (d, d.cnt))


import os
STOP = int(os.environ.get('KSTOP', '99'))
CUT = int(os.environ.get('KCUT', str(10 ** 9)))
MARKS = []


def build():
    nc = bass.Bass("TRN2", target_bir_lowering=False)

    def D(name, shape, kind="ExternalInput", dt=F32):
        return nc.dram_tensor(name, shape, dt, kind=kind).ap()

    xT_d = D("xT", [128, 8, S])
    gv_d = D("gv", [128, 64])
    w1gu_d = D("w1gu", [NFT, 128, 2048])
    w1d_d = D("w1d", [8, 128, DFF])
    w2gu_d = D("w2gu", [NFT, 128, 2048])
    w2d_d = D("w2d", [8, 128, DFF])
    win_d = D("win", [16, 128, 1024])
    wglu_d = D("wglu", [128, 2048])
    wout_d = D("wout", [8, 128, 1024])
    lamst_d = D("lamst", [128, 48])
    lambc_d = D("lambc", [128, 3 * 2048])
    blay_d = D("blay", [128, 2 * 2048])
    clay_d = D("clay", [128, 2 * 1024])
    dlay_d = D("dlay", [128, 512])
    kaug_d = D("kaugc", [12, S])
    qaug_d = D("qaugc", [4, 8, S])
    cst_d = D("cst", [128, 3 * 128])
    out_d = D("yT", [128, 8, S], kind="ExternalOutput")

    es = contextlib.ExitStack()
    with es:
        T = Trk(nc, es)
        PE, ACT, DVE, POOL, SP = T.PE, T.ACT, T.DVE, T.POOL, T.SP
        cnt = [0]

        def sbt(stack, shape, dt, ds=False, dj=False):
            cnt[0] += 1
            t = stack.enter_context(nc.sbuf_tensor("t%d" % cnt[0], shape, dt))
            return Buf(t, T.newds() if ds else None, dj)

        xT = sbt(es, [128, 8, S], F32, ds=True)
        gv = sbt(es, [128, 64], F32, ds=True)
        ident_f = sbt(es, [128, 128], F32, ds=True)
        ident_b = sbt(es, [128, 128], BF, ds=True)
        trineg = sbt(es, [128, 128], BF, ds=True)
        ones_b = sbt(es, [128, 128], BF)
        banks = []
        for i in range(8):
            banks.append(Buf(es.enter_context(nc.psum_tensor("bank%d" % i, [128, 512], F32)), psum=True))

        T.dma(SP, xT.t[:], xT_d, xT.ds, writes=[xT])
        T.dma(SP, gv.t[:], gv_d, gv.ds, writes=[gv])
        T.dma(SP, ident_f.t[:], cst_d[:, 0:128], ident_f.ds, writes=[ident_f])
        T.dma(POOL, ident_b.t[:], cst_d[:, 0:128], ident_b.ds, writes=[ident_b])
        T.dma(POOL, trineg.t[:], cst_d[:, 128:256], trineg.ds, writes=[trineg])
        T.op(DVE, lambda: nc.vector.memset(ones_b.t[:], 1.0), writes=[ones_b])

        def gcol(c):
            return gv.t[:, c:c + 1]

        def MM(*a, **k):
            return nc.tensor.matmul(*a, **k) if T.live else None

        def MT(*a, **k):
            return nc.tensor.transpose(*a, **k) if T.live else None

        def cast_dma(buf, out_ap, in_ap, ncols, ds, chunk=512):
            nchunk = (ncols + chunk - 1) // chunk
            w = (ncols + nchunk - 1) // nchunk
            for a in range(0, ncols, w):
                b = min(ncols, a + w)
                T.dma(POOL, out_ap[:, a:b], in_ap[:, a:b], ds, writes=[buf])

        class Stream:
            def __init__(self, stack, shape, nslots):
                self.slots = [sbt(stack, shape, BF, ds=True) for _ in range(nslots)]
                self.n = nslots
                self.jobs = []
                self.issued = 0
                self.base = 0

            def start(self, jobs):
                self.jobs = self.jobs + list(jobs)

            def _issue(self, k):
                sl = self.slots[k % self.n]
                cast_dma(sl, sl.t, self.jobs[k], sl.t.shape[1], sl.ds, chunk=512)

            def get(self, i):
                hi = min(i + self.n - 1, len(self.jobs) - 1)
                while self.issued <= hi:
                    self._issue(self.issued)
                    self.issued += 1
                return self.slots[i % self.n]

        def sq_and_sum(stack_bufs, src_ap_fn, src_bufs, ntiles, ssb, ncols=512):
            sqb = stack_bufs
            for i in range(ntiles):
                q = sqb[i % len(sqb)]
                T.op(ACT, lambda q=q, i=i: nc.scalar.activation(out=q.t[:, :ncols], in_=src_ap_fn(i), func=AF.Square),
                     reads=src_bufs, writes=[q])
                T.op(PE, lambda q=q, i=i: MM(ssb.t[:, :ncols], lhsT=ones_b.t[:], rhs=q.t[:, :ncols],
                                                          start=(i == 0), stop=(i == ntiles - 1)),
                     reads=[q, ones_b], writes=[ssb])

        def rstd_from(ssb, rt, rstd, dim, ncols=512):
            T.op(DVE, lambda: nc.vector.tensor_scalar(out=rt.t[:, :ncols], in0=ssb.t[:, :ncols], scalar1=1.0 / dim,
                                                      scalar2=EPS, op0=ALU.mult, op1=ALU.add),
                 reads=[ssb], writes=[rt])
            T.op(ACT, lambda: nc.scalar.activation(out=rt.t[:, :ncols], in_=rt.t[:, :ncols], func=AF.Sqrt),
                 reads=[rt], writes=[rt])
            T.op(DVE, lambda: nc.vector.reciprocal(out=rstd.t[:, :ncols], in_=rt.t[:, :ncols]),
                 reads=[rt], writes=[rstd])

        def prenorm(c0, g0, outb, ocol0, sqb, ssb, rt, rstd):
            sq_and_sum(sqb, lambda i: xT.t[:, i, c0:c0 + 512], [xT], 8, ssb)
            rstd_from(ssb, rt, rstd, 1024.0)
            for kc in range(8):
                T.op(DVE, lambda kc=kc: nc.vector.scalar_tensor_tensor(
                    out=outb.t[:, kc, ocol0:ocol0 + 512], in0=xT.t[:, kc, c0:c0 + 512], scalar=gcol(g0 + kc),
                    in1=rstd.t[:, :], op0=ALU.mult, op1=ALU.mult), reads=[xT, rstd, gv], writes=[outb])

        def down_project(stream, job0, nk, rhs_fn, rhs_bufs, fTs, c0s, gpost, half_scale, ybanks, ssbs, sqb, rt, rstd):
            pend = None
            step = 0
            ng = len(c0s)
            for dt in range(8):
                sl = stream.get(job0 + dt)
                for ti in range(ng):
                    yb = ybanks[step % 2]
                    q = sqb[step % 2]
                    step += 1
                    fT, ssb = fTs[ti], ssbs[ti]
                    T.pre(PE, reads=[sl] + rhs_bufs, writes=[yb])
                    for k in range(nk):
                        ins = MM(yb.t[:, :], lhsT=sl.t[:, k * 128:(k + 1) * 128], rhs=rhs_fn(k, ti),
                                               start=(k == 0), stop=(k == nk - 1))
                    T.post(PE, ins, reads=[sl] + rhs_bufs, writes=[yb])
                    if pend is not None:
                        pend()
                    T.op(ACT, lambda q=q, yb=yb: nc.scalar.activation(out=q.t[:, :], in_=yb.t[:, :], func=AF.Square),
                         reads=[yb], writes=[q])
                    T.op(DVE, lambda yb=yb, dt=dt, fT=fT: nc.vector.tensor_copy(out=fT.t[:, dt, :], in_=yb.t[:, :]),
                         reads=[yb], writes=[fT])

                    def pend(q=q, dt=dt, ssb=ssb):
                        T.op(PE, lambda: MM(ssb.t[:, :], lhsT=ones_b.t[:], rhs=q.t[:, :],
                                                          start=(dt == 0), stop=(dt == 7)),
                             reads=[q, ones_b], writes=[ssb])
            pend()
            T.mark('dp_finalize')
            for ti in range(ng):
                fT, c0 = fTs[ti], c0s[ti]
                rstd_from(ssbs[ti], rt, rstd, 1024.0)
                for dt in range(8):
                    T.op(DVE, lambda dt=dt, fT=fT: nc.vector.scalar_tensor_tensor(
                        out=fT.t[:, dt, :], in0=fT.t[:, dt, :], scalar=gcol(gpost + dt), in1=rstd.t[:, :],
                        op0=ALU.mult, op1=ALU.mult), reads=[fT, rstd, gv], writes=[fT])
                    T.op(DVE, lambda dt=dt, fT=fT, c0=c0: nc.vector.scalar_tensor_tensor(
                        out=xT.t[:, dt, c0:c0 + 512], in0=fT.t[:, dt, :], scalar=half_scale, in1=xT.t[:, dt, c0:c0 + 512],
                        op0=ALU.mult, op1=ALU.add), reads=[fT, xT], writes=[xT])

        def ffn(gpre, gpost, wgu_d, wd_d):
            with contextlib.ExitStack() as fs:
                xn = sbt(fs, [128, 8, 1024], BF, dj=True)
                actT = sbt(fs, [128, NFT, 1024], BF, dj=True)
                fTs = [sbt(fs, [128, 8, 512], F32) for _ in range(2)]
                sg = [sbt(fs, [128, 512], BF) for _ in range(2)]
                sqb = [sbt(fs, [128, 512], BF) for _ in range(2)]
                rt = sbt(fs, [128, 512], F32)
                rstd = sbt(fs, [128, 512], F32)
                SA = Stream(fs, [128, 2048], 3)
                SB = Stream(fs, [128, DFF], 2)
                for half in range(2):
                    SA.start([wgu_d[ft] for ft in range(NFT)])
                    SB.start([wd_d[dt] for dt in range(8)])
                for half in range(2):
                    h0 = half * 1024
                    T.mark('ffn_half%d_start' % half)
                    SA.get(half * NFT)
                    for tg in range(2):
                        prenorm(h0 + tg * 512, gpre, xn, tg * 512, sqb, banks[6 + tg], rt, rstd)
                    T.mark('phaseA_start')
                    for ft in range(NFT):
                        sl = SA.get(half * NFT + ft)
                        if ft == 1:
                            T.mark('phaseA_ft1')
                        for tg in range(2):
                            hg, hu = banks[2 * tg], banks[2 * tg + 1]
                            T.pre(PE, reads=[sl, xn], writes=[hg, hu])
                            for kc in range(8):
                                MM(hg.t[:, :], lhsT=sl.t[:, kc * 128:(kc + 1) * 128],
                                                 rhs=xn.t[:, kc, tg * 512:(tg + 1) * 512], start=(kc == 0), stop=(kc == 7))
                            for kc in range(8):
                                ins = MM(hu.t[:, :], lhsT=sl.t[:, 1024 + kc * 128:1024 + (kc + 1) * 128],
                                                       rhs=xn.t[:, kc, tg * 512:(tg + 1) * 512], start=(kc == 0), stop=(kc == 7))
                            T.post(PE, ins, reads=[sl, xn], writes=[hg, hu])
                            s = sg[tg]
                            T.op(ACT, lambda s=s, hg=hg: nc.scalar.activation(out=s.t[:, :], in_=hg.t[:, :], func=AF.Silu),
                                 reads=[hg], writes=[s])
                            T.op(DVE, lambda s=s, hu=hu, ft=ft, tg=tg: nc.vector.tensor_tensor(
                                out=actT.t[:, ft, tg * 512:(tg + 1) * 512], in0=hu.t[:, :], in1=s.t[:, :], op=ALU.mult),
                                reads=[hu, s], writes=[actT])
                    T.mark('phaseB_start')
                    down_project(SB, half * 8, NFT, lambda k, ti: actT.t[:, k, ti * 512:(ti + 1) * 512], [actT],
                                 fTs, [h0, h0 + 512], gpost, 0.5, [banks[4], banks[5]], [banks[6], banks[7]], sqb, rt, rstd)

        T.barrier()
        if STOP >= 1:
            ffn(G_F1PRE, G_F1POST, w1gu_d, w1d_d)
        T.barrier()

        with contextlib.ExitStack() as ms:
          if STOP >= 2:
            usT = sbt(ms, [128, 4, S], BF, dj=True)
            matt = sbt(ms, [128, 4, S], BF, dj=True)
            with contextlib.ExitStack() as as_:
                Kpk = sbt(as_, [128, 4, S], BF, dj=True)
                Kx = sbt(as_, [128, S], BF, ds=True)
                V = sbt(as_, [128, 16, 512], BF, dj=True)
                Qm = sbt(as_, [128, 8, 512], BF)
                Qx = sbt(as_, [128, 8, 512], BF, ds=True)
                un = sbt(as_, [128, 8, 512], BF, dj=True)
                ksum = sbt(as_, [128, 4, 8], F32)
                ksum_b = sbt(as_, [128, 4, 8], BF)
                Gsb = sbt(as_, [128, 32, 8], F32)
                M8 = sbt(as_, [128, 32, 8], F32)
                SEL = sbt(as_, [128, 32, 8], F32)
                mbias = sbt(as_, [128, 4, 64], F32)
                MTsb = sbt(as_, [64, 512], BF)
                pT = [sbt(as_, [128, 512], BF) for _ in range(3)]
                rden = sbt(as_, [128, 512], F32)
                sqb = [sbt(as_, [128, 512], BF) for _ in range(2)]
                rt = sbt(as_, [128, 512], F32)
                rstd = sbt(as_, [128, 512], F32)
                att = sbt(as_, [128, 4, 512], F32)
                WS = Stream(as_, [128, 1024], 4)
                T.op(DVE, lambda: nc.vector.memset(ksum.t[:], 0.0), writes=[ksum])
                T.op(DVE, lambda: nc.vector.memset(Kx.t[:], 0.0), writes=[Kx])
                T.op(DVE, lambda: nc.vector.memset(Qx.t[:], 0.0), writes=[Qx])
                T.op(DVE, lambda: nc.vector.memset(Qm.t[:], 0.0), writes=[Qm])
                Qx_ds2 = T.newds()
                cast_dma(Kx, Kx.t[0:12, :], kaug_d, S, Kx.ds)
                for tg in range(4):
                    WS.start([win_d[n] for n in range(16)])
                for tg in range(4):
                    c0 = tg * 512
                    WS.get(tg * 16)
                    T.dma(POOL, Qx.t[8:12, :, :], qaug_d[:, :, c0:c0 + 512], Qx.ds, writes=[Qx])
                    prenorm(c0, G_MIXPRE, un, 0, sqb, banks[6], rt, rstd)
                    for n in range(8):
                        sl = WS.get(tg * 16 + n)
                        pb = banks[n % 2]
                        T.pre(PE, reads=[sl, un], writes=[pb])
                        for kc in range(8):
                            ins = MM(pb.t[:, :], lhsT=sl.t[:, kc * 128:(kc + 1) * 128], rhs=un.t[:, kc, :],
                                                   start=(kc == 0), stop=(kc == 7))
                        T.post(PE, ins, reads=[sl, un], writes=[pb])
                        if n < 4:
                            T.op(ACT, lambda pb=pb, n=n: nc.scalar.copy(out=Kpk.t[:, n, c0:c0 + 512], in_=pb.t[:, :]),
                                 reads=[pb], writes=[Kpk])
                            T.op(DVE, lambda pb=pb, n=n: nc.vector.tensor_reduce(
                                out=ksum.t[:, n, 2 * tg:2 * tg + 2], in_=pb.t[:, :].rearrange("p (a b) -> p a b", b=256),
                                axis=AX.X, op=ALU.add), reads=[pb], writes=[ksum])
                        else:
                            for e in range(2):
                                T.op(ACT, lambda pb=pb, n=n, e=e: nc.scalar.mul(out=Qm.t[64 * e:64 * e + 64, 2 * (n - 4) + e, :],
                                                                               in_=pb.t[64 * e:64 * e + 64, :], mul=0.125),
                                     reads=[pb], writes=[Qm])
                    for c in range(4):
                        sl = WS.get(tg * 16 + 8 + c)
                        pb = banks[2 + c % 2]
                        T.pre(PE, reads=[sl, un], writes=[pb])
                        for tt in range(4):
                            for kc in range(8):
                                ins = MM(pb.t[:, tt * 128:(tt + 1) * 128], lhsT=un.t[:, kc, tt * 128:(tt + 1) * 128],
                                                       rhs=sl.t[:, kc * 128:(kc + 1) * 128], start=(kc == 0), stop=(kc == 7))
                        T.post(PE, ins, reads=[sl, un], writes=[pb])
                        T.op(DVE, lambda pb=pb, c=c: nc.vector.tensor_copy(
                            out=V.t[:, 4 * tg:4 * tg + 4, c * 128:(c + 1) * 128],
                            in_=pb.t[:, :].rearrange("p (a b) -> p a b", b=128)), reads=[pb], writes=[V])
                    for k in range(4):
                        sl = WS.get(tg * 16 + 12 + k)
                        pb = banks[4 + k % 2]
                        T.pre(PE, reads=[sl, un], writes=[pb])
                        for kc in range(8):
                            ins = MM(pb.t[:, :], lhsT=sl.t[:, kc * 128:(kc + 1) * 128], rhs=un.t[:, kc, :],
                                                   start=(kc == 0), stop=(kc == 7))
                        T.post(PE, ins, reads=[sl, un], writes=[pb])
                        T.op(ACT, lambda pb=pb, k=k: nc.scalar.copy(out=usT.t[:, k, c0:c0 + 512], in_=pb.t[:, :]),
                             reads=[pb], writes=[usT])
                    if tg < 2:
                        T.op(DVE, lambda: nc.vector.memset(mbias.t[:], 0.0), writes=[mbias])
                    else:
                        T.op(DVE, lambda: nc.vector.tensor_copy(out=ksum_b.t[:], in_=ksum.t[:]), reads=[ksum], writes=[ksum_b])
                        gb = banks[7]
                        T.pre(PE, reads=[Qm, ksum_b], writes=[gb])
                        for qt in range(4):
                            for h in range(8):
                                e, hp = h % 2, h // 2
                                ins = MM(gb.t[:, (qt * 8 + h) * 8:(qt * 8 + h) * 8 + 8],
                                                       lhsT=Qm.t[:, h, qt * 128:(qt + 1) * 128],
                                                       rhs=ksum_b.t[:, hp, :], start=True, stop=True)
                        T.post(PE, ins, reads=[Qm, ksum_b], writes=[gb])
                        T.op(DVE, lambda: nc.vector.memset(Gsb.t[:], -1e30), writes=[Gsb])
                        for half2 in range(2):
                            nb = 2 * tg + half2
                            T.op(DVE, lambda half2=half2, nb=nb: nc.vector.tensor_copy(
                                out=Gsb.t[:, 16 * half2:16 * half2 + 16, 0:nb],
                                in_=gb.t[:, 128 * half2:128 * half2 + 128].rearrange("p (a b) -> p a b", b=8)[:, :, 0:nb]),
                                reads=[gb], writes=[Gsb])
                        for i in range(32):
                            T.op(DVE, lambda i=i: nc.vector.max(out=M8.t[:, i, :], in_=Gsb.t[:, i, :]), reads=[Gsb], writes=[M8])
                        T.op(DVE, lambda: nc.vector.tensor_tensor(out=SEL.t[:], in0=Gsb.t[:], in1=M8.t[:, :, 2:3].to_broadcast([128, 32, 8]),
                                                                  op=ALU.is_ge), reads=[Gsb, M8], writes=[SEL])
                        T.op(DVE, lambda: nc.vector.tensor_scalar(out=mbias.t[:].rearrange("p a (h n) -> p (a h) n", n=8), in0=SEL.t[:],
                                                                  scalar1=1.0, scalar2=BIG, op0=ALU.subtract, op1=ALU.mult),
                             reads=[SEL], writes=[mbias])
                        for half2 in range(2):
                            nb = 2 * tg + half2
                            T.op(DVE, lambda half2=half2, nb=nb: nc.vector.memset(
                                mbias.t[:, 2 * half2:2 * half2 + 2, :].rearrange("p a (h n) -> p a h n", n=8)[:, :, :, nb:nb + 1], 0.0),
                                writes=[mbias])
                    mb = banks[7]
                    T.pre(PE, reads=[mbias, ident_f], writes=[mb])
                    for qt in range(4):
                        ins = MT(mb.t[0:64, qt * 128:(qt + 1) * 128], mbias.t[:, qt, :], ident_f.t[:, :])
                    T.post(PE, ins, reads=[mbias, ident_f], writes=[mb])
                    T.op(ACT, lambda: nc.scalar.copy(out=MTsb.t[:, :], in_=mb.t[0:64, :]), reads=[mb], writes=[MTsb])
                    for h in range(8):
                        T.dma(SP, Qx.t[0:8, h, :], MTsb.t[8 * h:8 * h + 8, :], Qx_ds2, reads=[MTsb], writes=[Qx])
                    nkt = 4 * tg + 4
                    for hp in range(4):
                        numP, denP = banks[2 + hp % 2], banks[4 + hp % 2]
                        for e in range(2):
                            h = 2 * hp + e
                            rows = slice(64 * e, 64 * e + 64)
                            pend = None
                            for kt in range(nkt):
                                j = kt - 4 * tg
                                ql = 128 * j if j > 0 else 0
                                sP = banks[kt % 2]
                                rd = [Kpk, Qm, Kx, Qx, ident_b, trineg]
                                T.pre(PE, reads=rd, writes=[sP])
                                MM(sP.t[:, ql:512], lhsT=Kpk.t[:, hp, kt * 128:(kt + 1) * 128],
                                                 rhs=Qm.t[:, h, ql:512], start=True, stop=False)
                                ins = MM(sP.t[:, ql:512], lhsT=Kx.t[:, kt * 128:(kt + 1) * 128],
                                                       rhs=Qx.t[:, h, ql:512], start=False, stop=(j < 0))
                                if j >= 0:
                                    ins = MM(sP.t[:, ql:ql + 128], lhsT=ident_b.t[:, :], rhs=trineg.t[:, :],
                                                           start=False, stop=True)
                                T.post(PE, ins, reads=rd, writes=[sP])
                                p = pT[kt % 3]
                                T.op(ACT, lambda p=p, sP=sP, ql=ql: nc.scalar.activation(out=p.t[:, ql:512], in_=sP.t[:, ql:512], func=AF.Exp),
                                     reads=[sP], writes=[p])
                                if pend is not None:
                                    pend()

                                def pend(p=p, kt=kt, ql=ql):
                                    T.pre(PE, reads=[p, V, ones_b], writes=[numP, denP])
                                    MM(numP.t[rows, ql:512], lhsT=V.t[:, kt, h * 64:(h + 1) * 64], rhs=p.t[:, ql:512],
                                                     start=(kt == 0), stop=(kt == nkt - 1))
                                    ins2 = MM(denP.t[rows, ql:512], lhsT=ones_b.t[:, 0:64], rhs=p.t[:, ql:512],
                                                            start=(kt == 0), stop=(kt == nkt - 1))
                                    T.post(PE, ins2, reads=[p, V, ones_b], writes=[numP, denP])
                            pend()
                            T.op(DVE, lambda: nc.vector.reciprocal(out=rden.t[rows, :], in_=denP.t[rows, :]), reads=[denP], writes=[rden])
                            T.op(DVE, lambda: nc.vector.tensor_tensor(out=att.t[rows, hp, :], in0=numP.t[rows, :], in1=rden.t[rows, :],
                                                                      op=ALU.mult), reads=[numP, rden], writes=[att])
                    sq_and_sum(sqb, lambda i: att.t[:, i, :], [att], 4, banks[6])
                    rstd_from(banks[6], rt, rstd, 512.0)
                    for hp in range(4):
                        T.op(DVE, lambda hp=hp: nc.vector.scalar_tensor_tensor(
                            out=matt.t[:, hp, c0:c0 + 512], in0=att.t[:, hp, :], scalar=gcol(G_ATT + hp), in1=rstd.t[:, :],
                            op0=ALU.mult, op1=ALU.mult), reads=[att, rstd, gv], writes=[matt])
            T.barrier()
            if STOP >= 3:
             with contextlib.ExitStack() as ss_:
                CS = sbt(ss_, [128, 2, 16, 128], BF)
                BB = sbt(ss_, [128, 16, 2, 128], BF)
                CC = sbt(ss_, [128, 16, 3, 64], BF)
                DDm = sbt(ss_, [128, 512], BF, ds=True)
                wglu = sbt(ss_, [128, 2048], BF, ds=True)
                mag_s = sbt(ss_, [128, 16], F32)
                ECS = sbt(ss_, [128, 2, 16], F32)
                Hc = sbt(ss_, [128, 2, 16], F32)
                cast_dma(DDm, DDm.t, dlay_d, 512, DDm.ds)
                cast_dma(wglu, wglu.t, wglu_d, 2048, wglu.ds)

                def sincos(stk, ang, n, sinb, cosb, tmpF, tmpI):
                    T.op(DVE, lambda: nc.vector.tensor_scalar(out=tmpI.t[:, :n], in0=ang.t[:, :n], scalar1=1.0 / (2 * PI), scalar2=None,
                                                              op0=ALU.mult), reads=[ang], writes=[tmpI])
                    T.op(DVE, lambda: nc.vector.tensor_copy(out=tmpF.t[:, :n], in_=tmpI.t[:, :n]), reads=[tmpI], writes=[tmpF])
                    T.op(DVE, lambda: nc.vector.scalar_tensor_tensor(out=tmpF.t[:, :n], in0=tmpF.t[:, :n], scalar=-2 * PI, in1=ang.t[:, :n],
                                                                     op0=ALU.mult, op1=ALU.add), reads=[tmpF, ang], writes=[tmpF])
                    T.op(DVE, lambda: nc.vector.tensor_scalar(out=tmpF.t[:, :n], in0=tmpF.t[:, :n], scalar1=-PI, scalar2=PI,
                                                              op0=ALU.max, op1=ALU.min), reads=[tmpF], writes=[tmpF])
                    T.op(ACT, lambda: nc.scalar.activation(out=sinb.t[:, :n], in_=tmpF.t[:, :n], func=AF.Sin), reads=[tmpF], writes=[sinb])
                    T.op(ACT, lambda: nc.scalar.activation(out=tmpF.t[:, :n], in_=tmpF.t[:, :n], func=AF.Abs), reads=[tmpF, sinb], writes=[tmpF])
                    T.op(DVE, lambda: nc.vector.tensor_scalar(out=tmpF.t[:, :n], in0=tmpF.t[:, :n], scalar1=-1.0, scalar2=PI / 2,
                                                              op0=ALU.mult, op1=ALU.add), reads=[tmpF], writes=[tmpF])
                    T.op(ACT, lambda: nc.scalar.activation(out=cosb.t[:, :n], in_=tmpF.t[:, :n], func=AF.Sin), reads=[tmpF], writes=[cosb])

                TT = lambda o, a, b, op, rd, wr: T.op(DVE, lambda: nc.vector.tensor_tensor(out=o, in0=a, in1=b, op=op), reads=rd, writes=wr)
                with contextlib.ExitStack() as st:
                    lamst = sbt(st, [128, 48], F32, ds=True)
                    iota = sbt(st, [128, 128], F32, ds=True)
                    clay = sbt(st, [128, 2 * 1024], F32, ds=True)
                    T.dma(SP, lamst.t[:], lamst_d, lamst.ds, writes=[lamst])
                    T.dma(SP, iota.t[:], cst_d[:, 256:384], iota.ds, writes=[iota])
                    T.dma(SP, clay.t[:], clay_d, clay.ds, writes=[clay])
                    W = [sbt(st, [128, 2048], F32) for _ in range(3)]
                    WI = sbt(st, [128, 2048], I32)
                    sS = sbt(st, [128, 2048], F32)
                    sC = sbt(st, [128, 2048], F32)
                    dts, th_s, e1 = W[0], W[1], W[2]
                    T.op(ACT, lambda: nc.scalar.activation(out=dts.t[:, :16], in_=lamst.t[:, 32:48], func=AF.Exp), reads=[lamst], writes=[dts])
                    TT(e1.t[:, :16], lamst.t[:, 0:16], dts.t[:, :16], ALU.mult, [lamst, dts], [e1])
                    T.op(ACT, lambda: nc.scalar.activation(out=mag_s.t[:, :], in_=e1.t[:, :16], func=AF.Exp), reads=[e1], writes=[mag_s])
                    TT(th_s.t[:, :16], lamst.t[:, 16:32], dts.t[:, :16], ALU.mult, [lamst, dts], [th_s])
                    PH = W[2]
                    for j in range(16):
                        T.op(DVE, lambda j=j: nc.vector.tensor_scalar(out=PH.t[:, j * 128:(j + 1) * 128], in0=iota.t[:, :],
                                                                      scalar1=th_s.t[:, j:j + 1], scalar2=None, op0=ALU.mult),
                             reads=[iota, th_s], writes=[PH])
                    sincos(st, PH, 2048, sS, sC, W[0], WI)
                    j3 = lambda ap: ap.rearrange("p (j t) -> p j t", t=128)
                    T.op(DVE, lambda: nc.vector.tensor_copy(out=CS.t[:, 0, :, :], in_=j3(sC.t[:, :])), reads=[sC], writes=[CS])
                    T.op(DVE, lambda: nc.vector.tensor_copy(out=CS.t[:, 1, :, :], in_=j3(sS.t[:, :])), reads=[sS], writes=[CS])
                    T.op(DVE, lambda: nc.vector.tensor_copy(out=ECS.t[:, 0, :], in_=j3(sC.t[:, :])[:, :, 127]), reads=[sC], writes=[ECS])
                    T.op(DVE, lambda: nc.vector.tensor_copy(out=ECS.t[:, 1, :], in_=j3(sS.t[:, :])[:, :, 127]), reads=[sS], writes=[ECS])
                    c3 = lambda ap: ap.rearrange("p (j c) -> p j c", c=64)
                    T.op(DVE, lambda: nc.vector.tensor_copy(out=CC.t[:, :, 0, :], in_=c3(clay.t[:, 0:1024])), reads=[clay], writes=[CC])
                    T.op(DVE, lambda: nc.vector.tensor_scalar(out=CC.t[:, :, 1, :], in0=c3(clay.t[:, 0:1024]), scalar1=-1.0, scalar2=None,
                                                              op0=ALU.mult), reads=[clay], writes=[CC])
                    T.op(DVE, lambda: nc.vector.tensor_scalar(out=CC.t[:, :, 2, :], in0=c3(clay.t[:, 1024:2048]), scalar1=-1.0, scalar2=None,
                                                              op0=ALU.mult), reads=[clay], writes=[CC])
                T.barrier()
                with contextlib.ExitStack() as st:
                    NH = 1024
                    lamh = sbt(st, [128, 3 * NH], F32, ds=True)
                    blh = sbt(st, [128, 2 * NH], F32, ds=True)
                    W = [sbt(st, [128, NH], F32) for _ in range(7)]
                    WI = sbt(st, [128, NH], I32)
                    sS = sbt(st, [128, NH], F32)
                    sC = sbt(st, [128, NH], F32)
                    for hf in range(2):
                        for q in range(3):
                            T.dma(SP, lamh.t[:, q * NH:(q + 1) * NH], lambc_d[:, q * 2048 + hf * NH:q * 2048 + (hf + 1) * NH], lamh.ds, writes=[lamh])
                        for q in range(2):
                            T.dma(SP, blh.t[:, q * NH:(q + 1) * NH], blay_d[:, q * 2048 + hf * NH:q * 2048 + (hf + 1) * NH], blh.ds, writes=[blh])
                        lr, li, ldt = lamh.t[:, 0:NH], lamh.t[:, NH:2 * NH], lamh.t[:, 2 * NH:3 * NH]
                        dtb, thb, magb = W[0], W[1], W[2]
                        T.op(ACT, lambda: nc.scalar.activation(out=dtb.t[:, :], in_=ldt, func=AF.Exp), reads=[lamh], writes=[dtb])
                        TT(magb.t[:, :], lr, dtb.t[:, :], ALU.mult, [lamh, dtb], [magb])
                        T.op(ACT, lambda: nc.scalar.activation(out=magb.t[:, :], in_=magb.t[:, :], func=AF.Exp), reads=[magb], writes=[magb])
                        TT(thb.t[:, :], li, dtb.t[:, :], ALU.mult, [lamh, dtb], [thb])
                        sincos(st, thb, NH, sS, sC, W[4], WI)
                        are, aim, den, fre, fim, tmp = W[3], W[4], W[5], W[6], W[0], W[1]
                        TT(are.t[:, :], magb.t[:, :], sC.t[:, :], ALU.mult, [magb, sC], [are])
                        TT(aim.t[:, :], magb.t[:, :], sS.t[:, :], ALU.mult, [magb, sS], [aim])
                        T.op(DVE, lambda: nc.vector.tensor_scalar(out=are.t[:, :], in0=are.t[:, :], scalar1=-1.0, scalar2=None, op0=ALU.add),
                             reads=[are], writes=[are])
                        TT(den.t[:, :], lr, lr, ALU.mult, [lamh], [den])
                        TT(tmp.t[:, :], li, li, ALU.mult, [lamh], [tmp])
                        TT(den.t[:, :], den.t[:, :], tmp.t[:, :], ALU.add, [den, tmp], [den])
                        T.op(DVE, lambda: nc.vector.reciprocal(out=den.t[:, :], in_=den.t[:, :]), reads=[den], writes=[den])
                        TT(fre.t[:, :], are.t[:, :], lr, ALU.mult, [are, lamh], [fre])
                        TT(tmp.t[:, :], aim.t[:, :], li, ALU.mult, [aim, lamh], [tmp])
                        TT(fre.t[:, :], fre.t[:, :], tmp.t[:, :], ALU.add, [fre, tmp], [fre])
                        TT(fre.t[:, :], fre.t[:, :], den.t[:, :], ALU.mult, [fre, den], [fre])
                        TT(fim.t[:, :], aim.t[:, :], lr, ALU.mult, [aim, lamh], [fim])
                        TT(tmp.t[:, :], are.t[:, :], li, ALU.mult, [are, lamh], [tmp])
                        TT(fim.t[:, :], fim.t[:, :], tmp.t[:, :], ALU.subtract, [fim, tmp], [fim])
                        TT(fim.t[:, :], fim.t[:, :], den.t[:, :], ALU.mult, [fim, den], [fim])
                        bre, bim = blh.t[:, 0:NH], blh.t[:, NH:2 * NH]
                        t2, t3 = W[2], W[3]
                        v3 = lambda ap: ap.rearrange("p (j c) -> p j c", c=128)
                        js = slice(8 * hf, 8 * hf + 8)
                        TT(t2.t[:, :], fre.t[:, :], bre, ALU.mult, [fre, blh], [t2])
                        TT(t3.t[:, :], fim.t[:, :], bim, ALU.mult, [fim, blh], [t3])
                        TT(BB.t[:, js, 0, :], v3(t2.t[:, :]), v3(t3.t[:, :]), ALU.subtract, [t2, t3], [BB])
                        TT(t2.t[:, :], fre.t[:, :], bim, ALU.mult, [fre, blh], [t2])
                        TT(t3.t[:, :], fim.t[:, :], bre, ALU.mult, [fim, blh], [t3])
                        TT(BB.t[:, js, 1, :], v3(t2.t[:, :]), v3(t3.t[:, :]), ALU.add, [t2, t3], [BB])
                T.barrier()
                TTq = sbt(ss_, [128, 4, 4, 128], BF)
                XTq = sbt(ss_, [128, 2, 4, 128], BF)
                Gq = sbt(ss_, [128, 2, 4, 128], F32)
                Pq = [sbt(ss_, [128, 4, 4, 128], BF) for _ in range(2)]
                ctmp = sbt(ss_, [128, 4, 4], F32)
                ygf = sbt(ss_, [128, 4, 512], F32)
                ygb = sbt(ss_, [128, 4, 512], BF)
                sig = sbt(ss_, [128, 512], F32)
                mssm = sbt(ss_, [128, 4, 512], BF)
                fT = sbt(ss_, [128, 8, 512], F32)
                sqb = [sbt(ss_, [128, 512], BF) for _ in range(2)]
                rt = sbt(ss_, [128, 512], F32)
                rstd = sbt(ss_, [128, 512], F32)
                WO = Stream(ss_, [128, 1024], 3)
                for tg in range(4):
                    WO.start([wout_d[dt] for dt in range(8)])
                it = 0
                for tg in range(4):
                    c0 = tg * 512
                    for cc in range(4):
                        c = 4 * tg + cc
                        t0 = c0 + 128 * cc
                        for k in range(4):
                            XA, XB = banks[2 * (it % 2)], banks[2 * (it % 2) + 1]
                            P = Pq[it % 2]
                            it += 1
                            T.pre(PE, reads=[BB, usT], writes=[XA, XB])
                            for jl in range(4):
                                j, e = 4 * k + jl, jl // 2
                                rows = slice(64 * e, 64 * e + 64)
                                MM(XA.t[:, jl * 128:(jl + 1) * 128], lhsT=BB.t[:, j, 0, :], rhs=usT.t[:, k, t0:t0 + 128],
                                                 start=True, stop=True)
                                ins = MM(XB.t[:, jl * 128:(jl + 1) * 128], lhsT=BB.t[:, j, 1, :], rhs=usT.t[:, k, t0:t0 + 128],
                                                       start=True, stop=True)
                            T.post(PE, ins, reads=[BB, usT], writes=[XA, XB])
                            q4 = slice(4 * k, 4 * k + 4)
                            xa = XA.t[:, :].rearrange("p (a b) -> p a b", b=128)
                            xb = XB.t[:, :].rearrange("p (a b) -> p a b", b=128)
                            cosq, sinq = CS.t[:, 0, q4, :], CS.t[:, 1, q4, :]
                            T.op(DVE, lambda: nc.vector.tensor_tensor(out=TTq.t[:, 0], in0=xa, in1=cosq, op=ALU.mult), reads=[XA, CS], writes=[TTq])
                            T.op(DVE, lambda: nc.vector.tensor_tensor(out=TTq.t[:, 1], in0=xb, in1=sinq, op=ALU.mult), reads=[XB, CS], writes=[TTq])
                            T.op(DVE, lambda: nc.vector.tensor_tensor(out=TTq.t[:, 2], in0=xb, in1=cosq, op=ALU.mult), reads=[XB, CS], writes=[TTq])
                            T.op(DVE, lambda: nc.vector.tensor_tensor(out=TTq.t[:, 3], in0=xa, in1=sinq, op=ALU.mult), reads=[XA, CS], writes=[TTq])
                            T.op(DVE, lambda: nc.vector.tensor_tensor(out=XTq.t[:, 0], in0=TTq.t[:, 0], in1=TTq.t[:, 1], op=ALU.add),
                                 reads=[TTq], writes=[XTq])
                            T.op(DVE, lambda: nc.vector.tensor_tensor(out=XTq.t[:, 1], in0=TTq.t[:, 2], in1=TTq.t[:, 3], op=ALU.subtract),
                                 reads=[TTq], writes=[XTq])
                            for jl in range(4):
                                j = 4 * k + jl
                                for r in range(2):
                                    T.op(DVE, lambda jl=jl, j=j, r=r: nc.vector.tensor_tensor_scan(
                                        out=Gq.t[:, r, jl, :], data0=mag_s.t[:, j:j + 1].to_broadcast([128, 128]), data1=XTq.t[:, r, jl, :],
                                        initial=(Hc.t[:, r, j:j + 1] if c > 0 else 0.0), op0=ALU.mult, op1=ALU.add),
                                        reads=[XTq, mag_s, Hc], writes=[Gq])
                            T.op(DVE, lambda: nc.vector.tensor_tensor(out=P.t[:, 0:2], in0=Gq.t[:, 0:2], in1=CS.t[:, 0:2, q4, :], op=ALU.mult),
                                 reads=[Gq, CS], writes=[P])
                            T.op(DVE, lambda: nc.vector.tensor_tensor(out=P.t[:, 2], in0=Gq.t[:, 1], in1=cosq, op=ALU.mult), reads=[Gq, CS], writes=[P])
                            T.op(DVE, lambda: nc.vector.tensor_tensor(out=P.t[:, 3], in0=Gq.t[:, 0], in1=sinq, op=ALU.mult), reads=[Gq, CS], writes=[P])
                            gre, gim = Gq.t[:, 0, :, 127], Gq.t[:, 1, :, 127]
                            ec, esn = ECS.t[:, 0, q4], ECS.t[:, 1, q4]
                            T.op(DVE, lambda: nc.vector.tensor_tensor(out=ctmp.t[:, 0], in0=gre, in1=ec, op=ALU.mult), reads=[Gq, ECS], writes=[ctmp])
                            T.op(DVE, lambda: nc.vector.tensor_tensor(out=ctmp.t[:, 1], in0=gim, in1=esn, op=ALU.mult), reads=[Gq, ECS], writes=[ctmp])
                            T.op(DVE, lambda: nc.vector.tensor_tensor(out=ctmp.t[:, 2], in0=gim, in1=ec, op=ALU.mult), reads=[Gq, ECS], writes=[ctmp])
                            T.op(DVE, lambda: nc.vector.tensor_tensor(out=ctmp.t[:, 3], in0=gre, in1=esn, op=ALU.mult), reads=[Gq, ECS], writes=[ctmp])
                            T.op(DVE, lambda: nc.vector.tensor_tensor(out=Hc.t[:, 0, q4], in0=ctmp.t[:, 0], in1=ctmp.t[:, 1], op=ALU.subtract),
                                 reads=[ctmp], writes=[Hc])
                            T.op(DVE, lambda: nc.vector.tensor_tensor(out=Hc.t[:, 1, q4], in0=ctmp.t[:, 2], in1=ctmp.t[:, 3], op=ALU.add),
                                 reads=[ctmp], writes=[Hc])
                            Y = banks[4 + k]
                            rd = [CC, P, DDm, usT]
                            T.pre(PE, reads=rd, writes=[Y])
                            for e in range(2):
                                rows = slice(64 * e, 64 * e + 64)
                                yo = Y.t[rows, cc * 128:(cc + 1) * 128]
                                first = True
                                for jl in (2 * e, 2 * e + 1):
                                    j = 4 * k + jl
                                    for (ci, pi_) in ((0, 0), (1, 1), (2, 2), (2, 3)):
                                        MM(yo, lhsT=CC.t[:, j, ci, :], rhs=P.t[:, pi_, jl, :], start=first, stop=False)
                                        first = False
                                ins = MM(yo, lhsT=DDm.t[:, k * 128 + 64 * e:k * 128 + 64 * e + 64],
                                                       rhs=usT.t[:, k, t0:t0 + 128], start=False, stop=True)
                            T.post(PE, ins, reads=rd, writes=[Y])
                    for k in range(4):
                        Y = banks[4 + k]
                        T.op(ACT, lambda k=k, Y=Y: nc.scalar.activation(out=ygf.t[:, k, :], in_=Y.t[:, :], func=AF.Gelu_apprx_tanh),
                             reads=[Y], writes=[ygf])
                        T.op(DVE, lambda k=k: nc.vector.tensor_copy(out=ygb.t[:, k, :], in_=ygf.t[:, k, :]), reads=[ygf], writes=[ygb])
                    for ko in range(4):
                        GP = banks[ko % 2]
                        T.pre(PE, reads=[wglu, ygb], writes=[GP])
                        for ki in range(4):
                            ins = MM(GP.t[:, :], lhsT=wglu.t[:, ki * 512 + ko * 128:ki * 512 + (ko + 1) * 128], rhs=ygb.t[:, ki, :],
                                                   start=(ki == 0), stop=(ki == 3))
                        T.post(PE, ins, reads=[wglu, ygb], writes=[GP])
                        T.op(ACT, lambda GP=GP, ko=ko: nc.scalar.activation(out=sig.t[:, :], in_=GP.t[:, :], func=AF.Sigmoid,
                                                                          bias=gcol(G_BGLU + ko)), reads=[GP, gv], writes=[sig])
                        T.op(DVE, lambda ko=ko: nc.vector.tensor_tensor(out=ygf.t[:, ko, :], in0=ygf.t[:, ko, :], in1=sig.t[:, :], op=ALU.mult),
                             reads=[ygf, sig], writes=[ygf])
                    sq_and_sum(sqb, lambda i: ygf.t[:, i, :], [ygf], 4, banks[2])
                    rstd_from(banks[2], rt, rstd, 512.0)
                    for ko in range(4):
                        T.op(DVE, lambda ko=ko: nc.vector.scalar_tensor_tensor(
                            out=mssm.t[:, ko, :], in0=ygf.t[:, ko, :], scalar=gcol(G_SSM + ko), in1=rstd.t[:, :],
                            op0=ALU.mult, op1=ALU.mult), reads=[ygf, rstd, gv], writes=[mssm])
                    down_project(WO, tg * 8, 8,
                                 lambda m, ti, c0=c0: (matt.t[:, m, c0:c0 + 512] if m < 4 else mssm.t[:, m - 4, :]), [matt, mssm],
                                 [fT], [c0], G_MIXPOST, 1.0, [banks[0], banks[1]], [banks[3]], sqb, rt, rstd)
            T.barrier()

        if STOP >= 4:
            ffn(G_F2PRE, G_F2POST, w2gu_d, w2d_d)
        T.barrier()
        T.live = True
        T.cut = 10 ** 9
        T.barrier()
        od = T.newds()
        T.dma(SP, out_d, xT.t[:], od, reads=[xT])
        SP.e.wait_ge(od.sem, od.cnt)
    return nc


def _shared_layouts(inp):
    f = np.float32
    A = lambda k: np.ascontiguousarray(np.asarray(inp[k], dtype=f)[0])
    o = {}
    gv = np.zeros((128, 64), f)

    def put(c0, v):
        n = v.shape[0] // 128
        gv[:, c0:c0 + n] = v.reshape(n, 128).T
    put(G_F1PRE, A("ffn1_pre_g")); put(G_F1POST, A("ffn1_post_g")); put(G_MIXPRE, A("mix_pre_g"))
    put(G_ATT, A("attn_out_g")); put(G_SSM, A("ssm_out_g")); put(G_MIXPOST, A("mix_post_g"))
    put(G_F2PRE, A("ffn2_pre_g")); put(G_F2POST, A("ffn2_post_g")); put(G_BGLU, A("b_glu"))
    o["gv"] = gv

    def gu(wg, wu):
        a = wg.reshape(8, 128, NFT, 128).transpose(2, 1, 0, 3)
        b = wu.reshape(8, 128, NFT, 128).transpose(2, 1, 0, 3)
        return np.ascontiguousarray(np.concatenate([a, b], axis=2).reshape(NFT, 128, 2048))

    def dn(wd):
        return np.ascontiguousarray(wd.reshape(NFT, 128, 8, 128).transpose(2, 1, 0, 3).reshape(8, 128, DFF))
    o["w1gu"] = gu(A("ffn1_w_gate"), A("ffn1_w_up")); o["w1d"] = dn(A("ffn1_w_down"))
    o["w2gu"] = gu(A("ffn2_w_gate"), A("ffn2_w_up")); o["w2d"] = dn(A("ffn2_w_down"))
    win = A("w_in")
    order = [512 + 128 * i for i in range(4)] + [128 * i for i in range(4)] + [1024 + 128 * i for i in range(8)]
    o["win"] = np.ascontiguousarray(np.stack([win[:, c:c + 128].reshape(8, 128, 128).transpose(1, 0, 2).reshape(128, 1024) for c in order]))
    o["wglu"] = np.ascontiguousarray(A("w_glu").reshape(4, 128, 512).transpose(1, 0, 2).reshape(128, 2048))
    wo = A("w_out")
    o["wout"] = np.ascontiguousarray(wo.reshape(8, 128, 8, 128).transpose(2, 1, 0, 3).reshape(8, 128, 1024))
    lr, li, ld = A("lam_re"), A("lam_im"), A("log_dt")
    st = lambda a: a.reshape(16, 2, 64).transpose(1, 2, 0).reshape(128, 16)
    ldf = np.repeat(ld[:, None], 64, axis=1)
    o["lamst"] = np.ascontiguousarray(np.concatenate([st(lr), st(li), st(ldf)], axis=1))
    bc = np.concatenate([lr.reshape(-1), li.reshape(-1), ldf.reshape(-1)])
    o["lambc"] = np.ascontiguousarray(np.broadcast_to(bc[None, :], (128, 6144)))
    bre, bim = A("b_re"), A("b_im")
    bl = np.zeros((2, 128, 16, 128), f)
    cre, cim = A("c_re"), A("c_im")
    cl = np.zeros((2, 128, 16, 64), f)
    for j in range(16):
        q = j % 4
        for g2 in range(2):
            g = 2 * j + g2
            r0 = 32 * q + 16 * g2
            bl[0, r0:r0 + 16, j, 64 * g2:64 * g2 + 64] = bre[g].T
            bl[1, r0:r0 + 16, j, 64 * g2:64 * g2 + 64] = bim[g].T
            cc0 = 32 * (j % 2) + 16 * g2
            cl[0, 64 * g2:64 * g2 + 64, j, cc0:cc0 + 16] = cre[g].T
            cl[1, 64 * g2:64 * g2 + 64, j, cc0:cc0 + 16] = cim[g].T
    o["blay"] = np.ascontiguousarray(bl.transpose(1, 0, 2, 3).reshape(128, 4096))
    o["clay"] = np.ascontiguousarray(cl.transpose(1, 0, 2, 3).reshape(128, 2048))
    dsk = A("d_skip").reshape(512)
    dl = np.zeros((128, 4, 128), f)
    for k in range(4):
        dl[np.arange(128), k, np.arange(128)] = dsk[k * 128:(k + 1) * 128]
    o["dlay"] = dl.reshape(128, 512)
    pos = np.arange(S)
    ka = np.zeros((12, S), f)
    ka[pos // 256, pos] = 1.0
    ka[8] = 1.0; ka[9] = 1.0; ka[10] = 128.0 * (pos // 128); ka[11] = pos % 128
    o["kaugc"] = ka
    qa = np.zeros((4, 8, S), f)
    for h in range(8):
        sl = 2.0 ** (-(h + 1))
        qa[0, h] = -sl * 128.0 * (pos // 128); qa[1, h] = -sl * (pos % 128); qa[2, h] = sl; qa[3, h] = sl
    o["qaugc"] = qa
    cst = np.zeros((128, 384), f)
    cst[:, 0:128] = np.eye(128, dtype=f)
    kk, qq = np.meshgrid(np.arange(128), np.arange(128), indexing="ij")
    cst[:, 128:256] = np.where(kk > qq, -BIG, 0.0)
    cst[:, 256:384] = np.arange(1, 129, dtype=f)[None, :]
    o["cst"] = cst
    return o


_NC = None


def kernel(**inputs):
    global _NC
    x = np.asarray(inputs["x"], dtype=np.float32)
    nb = x.shape[0]
    sh = _shared_layouts(inputs)
    in_maps = []
    for b in range(nb):
        m = dict(sh)
        m["xT"] = np.ascontiguousarray(x[b].T.reshape(8, 128, S).transpose(1, 0, 2))
        in_maps.append(m)
    if _NC is None:
        _NC = build()
    res = run_bass_kernel_spmd(_NC, in_maps, core_ids=list(range(nb)))
    out = np.empty_like(x)
    for b in range(nb):
        yT = np.asarray(res.results[b]["yT"], dtype=np.float32)
        out[b] = yT.transpose(1, 0, 2).reshape(1024, S).T
    return out
```

```python
import contextlib
import math
import numpy as np
import concourse.bass as bass
import concourse.mybir as mybir
from concourse.bass_utils import run_bass_kernel_spmd

F32 = mybir.dt.float32
BF = mybir.dt.bfloat16
I32 = mybir.dt.int32
AF = mybir.ActivationFunctionType
ALU = mybir.AluOpType
AX = mybir.AxisListType
PI = math.pi
S = 2048
DFF = 2816
NFT = 22
EPS = 1e-6
BIG = 30000.0

G_F1PRE, G_F1POST, G_MIXPRE, G_ATT, G_SSM, G_MIXPOST, G_F2PRE, G_F2POST, G_BGLU = 0, 8, 16, 24, 28, 32, 40, 48, 56


class Eng:
    def __init__(self, nc, e, name, es):
        self.e, self.name = e, name
        self.sem = es.enter_context(nc.semaphore("s_" + name))
        self.cnt = 0
        self.seen = {}


class DS:
    def __init__(self, nc, name, es):
        self.sem = es.enter_context(nc.semaphore(name))
        self.cnt = 0
        self.name = name


class Buf:
    def __init__(self, t, ds=None, dj=False, psum=False):
        self.psum = psum
        self.dj = dj
        self.t = t
        self.w = None
        self.r = {}
        self.ds = ds


class Trk:
    def __init__(self, nc, es):
        self.nc = nc
        self.es = es
        self.PE = Eng(nc, nc.tensor, "pe", es)
        self.ACT = Eng(nc, nc.scalar, "act", es)
        self.DVE = Eng(nc, nc.vector, "dve", es)
        self.POOL = Eng(nc, nc.gpsimd, "pool", es)
        self.SP = Eng(nc, nc.sync, "sp", es)
        self.engs = [self.PE, self.ACT, self.DVE, self.POOL, self.SP]
        self.dss = []
        self.nds = 0
        self.n = 0
        self.live = True
        self.cut = CUT

    def newds(self):
        self.nds += 1
        d = DS(self.nc, "d%d" % self.nds, self.es)
        self.dss.append(d)
        return d

    def wait(self, E, tok):
        src, val = tok
        if src is E and E is self.PE:
            return
        k = id(src)
        if E.seen.get(k, 0) >= val:
            return
        E.e.wait_ge(src.sem, val)
        E.seen[k] = val

    def pre(self, E, reads=(), writes=()):
        if self.n >= self.cut:
            self.live = False
        if not self.live:
            return
        for b in reads:
            if b.w is not None:
                self.wait(E, b.w)
            if b.psum:
                for src, val in list(b.r.values()):
                    if src is not E:
                        self.wait(E, (src, val))
        for b in writes:
            if b.w is not None and (b.w[0] is not E or not b.dj):
                self.wait(E, b.w)
            for src, val in list(b.r.values()):
                self.wait(E, (src, val))

    def _reg(self, tok, reads, writes):
        for b in reads:
            b.r[id(tok[0])] = tok
        for b in writes:
            b.w = tok
            b.r = {}

    def post(self, E, inst, reads=(), writes=()):
        if not self.live:
            return
        self.n += 1
        E.cnt += 1
        inst.then_inc(E.sem, 1)
        self._reg((E, E.cnt), reads, writes)

    def op(self, E, fn, reads=(), writes=()):
        self.pre(E, reads, writes)
        if not self.live:
            return
        inst = fn()
        self.post(E, inst, reads, writes)

    def dma(self, Q, out, in_, ds, reads=(), writes=(), **kw):
        self.pre(Q, reads, writes)
        if not self.live:
            return
        self.n += 1
        inst = Q.e.dma_start(out=out, in_=in_, **kw)
        ds.cnt += 16
        inst.then_inc(ds.sem, 16)
        self._reg((ds, ds.cnt), reads, writes)

    def mark(self, name):
        MARKS.append((name, self.n))

    def barrier(self):
        if not self.live:
            return
        for E in self.engs:
            for Fe in self.engs:
                if Fe is not E and Fe.cnt > 0:
                    self.wait(E, (Fe, Fe.cnt))
            for d in self.dss:
                if d.cnt > 0:
                    self.wait(E, (d, d.cnt))


import os
STOP = int(os.environ.get('KSTOP', '99'))
CUT = int(os.environ.get('KCUT', str(10 ** 9)))
MARKS = []


def build():
    nc = bass.Bass("TRN2", target_bir_lowering=False)

    def D(name, shape, kind="ExternalInput", dt=F32):
        return nc.dram_tensor(name, shape, dt, kind=kind).ap()

    xT_d = D("xT", [128, 8, S])
    gv_d = D("gv", [128, 64])
    w1gu_d = D("w1gu", [NFT, 128, 2048])
    w1d_d = D("w1d", [8, 128, DFF])
    w2gu_d = D("w2gu", [NFT, 128, 2048])
    w2d_d = D("w2d", [8, 128, DFF])
    win_d = D("win", [16, 128, 1024])
    wglu_d = D("wglu", [128, 2048])
    wout_d = D("wout", [8, 128, 1024])
    lamst_d = D("lamst", [128, 48])
    lambc_d = D("lambc", [128, 3 * 2048])
    blay_d = D("blay", [128, 2 * 2048])
    clay_d = D("clay", [128, 2 * 1024])
    dlay_d = D("dlay", [128, 512])
    kaug_d = D("kaugc", [12, S])
    qaug_d = D("qaugc", [4, 8, S])
    cst_d = D("cst", [128, 3 * 128])
    out_d = D("yT", [128, 8, S], kind="ExternalOutput")

    es = contextlib.ExitStack()
    with es:
        T = Trk(nc, es)
        PE, ACT, DVE, POOL, SP = T.PE, T.ACT, T.DVE, T.POOL, T.SP
        cnt = [0]

        def sbt(stack, shape, dt, ds=False, dj=False):
            cnt[0] += 1
            t = stack.enter_context(nc.sbuf_tensor("t%d" % cnt[0], shape, dt))
            return Buf(t, T.newds() if ds else None, dj)

        xT = sbt(es, [128, 8, S], F32, ds=True)
        gv = sbt(es, [128, 64], F32, ds=True)
        ident_f = sbt(es, [128, 128], F32, ds=True)
        ident_b = sbt(es, [128, 128], BF, ds=True)
        trineg = sbt(es, [128, 128], BF, ds=True)
        ones_b = sbt(es, [128, 128], BF)
        banks = []
        for i in range(8):
            banks.append(Buf(es.enter_context(nc.psum_tensor("bank%d" % i, [128, 512], F32)), psum=True))

        T.dma(SP, xT.t[:], xT_d, xT.ds, writes=[xT])
        T.dma(SP, gv.t[:], gv_d, gv.ds, writes=[gv])
        T.dma(SP, ident_f.t[:], cst_d[:, 0:128], ident_f.ds, writes=[ident_f])
        T.dma(POOL, ident_b.t[:], cst_d[:, 0:128], ident_b.ds, writes=[ident_b])
        T.dma(POOL, trineg.t[:], cst_d[:, 128:256], trineg.ds, writes=[trineg])
        T.op(DVE, lambda: nc.vector.memset(ones_b.t[:], 1.0), writes=[ones_b])

        def gcol(c):
            return gv.t[:, c:c + 1]

        def MM(*a, **k):
            return nc.tensor.matmul(*a, **k) if T.live else None

        def MT(*a, **k):
            return nc.tensor.transpose(*a, **k) if T.live else None

        def cast_dma(buf, out_ap, in_ap, ncols, ds, chunk=512):
            nchunk = (ncols + chunk - 1) // chunk
            w = (ncols + nchunk - 1) // nchunk
            for a in range(0, ncols, w):
                b = min(ncols, a + w)
                T.dma(POOL, out_ap[:, a:b], in_ap[:, a:b], ds, writes=[buf])

        class Stream:
            def __init__(self, stack, shape, nslots, chunk=512):
                self.chunk = chunk
                self.slots = [sbt(stack, shape, BF, ds=True) for _ in range(nslots)]
                self.n = nslots
                self.jobs = []
                self.issued = 0
                self.base = 0

            def start(self, jobs):
                self.jobs = self.jobs + list(jobs)

            def _issue(self, k):
                sl = self.slots[k % self.n]
                cast_dma(sl, sl.t, self.jobs[k], sl.t.shape[1], sl.ds, chunk=self.chunk)

            def get(self, i):
                hi = min(i + self.n - 1, len(self.jobs) - 1)
                while self.issued <= hi:
                    self._issue(self.issued)
                    self.issued += 1
                return self.slots[i % self.n]

        def sq_and_sum(stack_bufs, src_ap_fn, src_bufs, ntiles, ssb, ncols=512):
            sqb = stack_bufs
            for i in range(ntiles):
                q = sqb[i % len(sqb)]
                T.op(ACT, lambda q=q, i=i: nc.scalar.activation(out=q.t[:, :ncols], in_=src_ap_fn(i), func=AF.Square),
                     reads=src_bufs, writes=[q])
                T.op(PE, lambda q=q, i=i: MM(ssb.t[:, :ncols], lhsT=ones_b.t[:], rhs=q.t[:, :ncols],
                                                          start=(i == 0), stop=(i == ntiles - 1)),
                     reads=[q, ones_b], writes=[ssb])

        def rstd_from(ssb, rt, rstd, dim, ncols=512):
            T.op(DVE, lambda: nc.vector.tensor_scalar(out=rt.t[:, :ncols], in0=ssb.t[:, :ncols], scalar1=1.0 / dim,
                                                      scalar2=EPS, op0=ALU.mult, op1=ALU.add),
                 reads=[ssb], writes=[rt])
            T.op(ACT, lambda: nc.scalar.activation(out=rt.t[:, :ncols], in_=rt.t[:, :ncols], func=AF.Sqrt),
                 reads=[rt], writes=[rt])
            T.op(DVE, lambda: nc.vector.reciprocal(out=rstd.t[:, :ncols], in_=rt.t[:, :ncols]),
                 reads=[rt], writes=[rstd])

        def prenorm(c0, g0, outb, ocol0, sqb, ssb, rt, rstd):
            sq_and_sum(sqb, lambda i: xT.t[:, i, c0:c0 + 512], [xT], 8, ssb)
            rstd_from(ssb, rt, rstd, 1024.0)
            for kc in range(8):
                T.op(DVE, lambda kc=kc: nc.vector.scalar_tensor_tensor(
                    out=outb.t[:, kc, ocol0:ocol0 + 512], in0=xT.t[:, kc, c0:c0 + 512], scalar=gcol(g0 + kc),
                    in1=rstd.t[:, :], op0=ALU.mult, op1=ALU.mult), reads=[xT, rstd, gv], writes=[outb])

        def down_project(stream, job0, nk, rhs_fn, rhs_bufs, fTs, c0s, gpost, half_scale, ybanks, ssbs, sqb, rt, rstd):
            pend = None
            step = 0
            ng = len(c0s)
            for dt in range(8):
                sl = stream.get(job0 + dt)
                for ti in range(ng):
                    yb = ybanks[step % 2]
                    q = sqb[step % 2]
                    step += 1
                    fT, ssb = fTs[ti], ssbs[ti]
                    T.pre(PE, reads=[sl] + rhs_bufs, writes=[yb])
                    for k in range(nk):
                        ins = MM(yb.t[:, :], lhsT=sl.t[:, k * 128:(k + 1) * 128], rhs=rhs_fn(k, ti),
                                               start=(k == 0), stop=(k == nk - 1))
                    T.post(PE, ins, reads=[sl] + rhs_bufs, writes=[yb])
                    if pend is not None:
                        pend()
                    T.op(ACT, lambda q=q, yb=yb: nc.scalar.activation(out=q.t[:, :], in_=yb.t[:, :], func=AF.Square),
                         reads=[yb], writes=[q])
                    T.op(DVE, lambda yb=yb, dt=dt, fT=fT: nc.vector.tensor_copy(out=fT.t[:, dt, :], in_=yb.t[:, :]),
                         reads=[yb], writes=[fT])

                    def pend(q=q, dt=dt, ssb=ssb):
                        T.op(PE, lambda: MM(ssb.t[:, :], lhsT=ones_b.t[:], rhs=q.t[:, :],
                                                          start=(dt == 0), stop=(dt == 7)),
                             reads=[q, ones_b], writes=[ssb])
            pend()
            T.mark('dp_finalize')
            for ti in range(ng):
                fT, c0 = fTs[ti], c0s[ti]
                rstd_from(ssbs[ti], rt, rstd, 1024.0)
                for dt in range(8):
                    T.op(DVE, lambda dt=dt, fT=fT: nc.vector.scalar_tensor_tensor(
                        out=fT.t[:, dt, :], in0=fT.t[:, dt, :], scalar=gcol(gpost + dt), in1=rstd.t[:, :],
                        op0=ALU.mult, op1=ALU.mult), reads=[fT, rstd, gv], writes=[fT])
                    T.op(DVE, lambda dt=dt, fT=fT, c0=c0: nc.vector.scalar_tensor_tensor(
                        out=xT.t[:, dt, c0:c0 + 512], in0=fT.t[:, dt, :], scalar=half_scale, in1=xT.t[:, dt, c0:c0 + 512],
                        op0=ALU.mult, op1=ALU.add), reads=[fT, xT], writes=[xT])

        def ffn(gpre, gpost, wgu_d, wd_d):
            with contextlib.ExitStack() as fs:
                xn = sbt(fs, [128, 8, 1024], BF, dj=True)
                actT = sbt(fs, [128, NFT, 1024], BF, dj=True)
                fTs = [sbt(fs, [128, 8, 512], F32) for _ in range(2)]
                sg = [sbt(fs, [128, 512], BF) for _ in range(2)]
                sqb = [sbt(fs, [128, 512], BF) for _ in range(2)]
                rt = sbt(fs, [128, 512], F32)
                rstd = sbt(fs, [128, 512], F32)
                SA = Stream(fs, [128, 2048], 5, chunk=1024)
                SB = Stream(fs, [128, DFF], 3, chunk=1408)
                for half in range(2):
                    SA.start([wgu_d[ft] for ft in range(NFT)])
                    SB.start([wd_d[dt] for dt in range(8)])
                for half in range(2):
                    h0 = half * 1024
                    T.mark('ffn_half%d_start' % half)
                    SA.get(half * NFT)
                    for tg in range(2):
                        prenorm(h0 + tg * 512, gpre, xn, tg * 512, sqb, banks[6 + tg], rt, rstd)
                    T.mark('phaseA_start')
                    for ft in range(NFT):
                        sl = SA.get(half * NFT + ft)
                        if ft == 1:
                            T.mark('phaseA_ft1')
                        for tg in range(2):
                            hg, hu = banks[2 * tg], banks[2 * tg + 1]
                            T.pre(PE, reads=[sl, xn], writes=[hg, hu])
                            for kc in range(8):
                                MM(hg.t[:, :], lhsT=sl.t[:, kc * 128:(kc + 1) * 128],
                                                 rhs=xn.t[:, kc, tg * 512:(tg + 1) * 512], start=(kc == 0), stop=(kc == 7))
                            for kc in range(8):
                                ins = MM(hu.t[:, :], lhsT=sl.t[:, 1024 + kc * 128:1024 + (kc + 1) * 128],
                                                       rhs=xn.t[:, kc, tg * 512:(tg + 1) * 512], start=(kc == 0), stop=(kc == 7))
                            T.post(PE, ins, reads=[sl, xn], writes=[hg, hu])
                            s = sg[tg]
                            T.op(ACT, lambda s=s, hg=hg: nc.scalar.activation(out=s.t[:, :], in_=hg.t[:, :], func=AF.Silu),
                                 reads=[hg], writes=[s])
                            T.op(DVE, lambda s=s, hu=hu, ft=ft, tg=tg: nc.vector.tensor_tensor(
                                out=actT.t[:, ft, tg * 512:(tg + 1) * 512], in0=hu.t[:, :], in1=s.t[:, :], op=ALU.mult),
                                reads=[hu, s], writes=[actT])
                    T.mark('phaseB_start')
                    down_project(SB, half * 8, NFT, lambda k, ti: actT.t[:, k, ti * 512:(ti + 1) * 512], [actT],
                                 fTs, [h0, h0 + 512], gpost, 0.5, [banks[4], banks[5]], [banks[6], banks[7]], sqb, rt, rstd)

        T.barrier()
        if STOP >= 1:
            ffn(G_F1PRE, G_F1POST, w1gu_d, w1d_d)
        T.barrier()

        with contextlib.ExitStack() as ms:
          if STOP >= 2:
            usT = sbt(ms, [128, 4, S], BF, dj=True)
            matt = sbt(ms, [128, 4, S], BF, dj=True)
            with contextlib.ExitStack() as as_:
                Kpk = sbt(as_, [128, 4, S], BF, dj=True)
                Kx = sbt(as_, [128, S], BF, ds=True)
                V = sbt(as_, [128, 16, 512], BF, dj=True)
                Qm = sbt(as_, [128, 8, 512], BF)
                Qx = sbt(as_, [128, 8, 512], BF, ds=True)
                un = sbt(as_, [128, 8, 512], BF, dj=True)
                ksum = sbt(as_, [128, 4, 8], F32)
                ksum_b = sbt(as_, [128, 4, 8], BF)
                Gsb = sbt(as_, [128, 32, 8], F32)
                M8 = sbt(as_, [128, 32, 8], F32)
                SEL = sbt(as_, [128, 32, 8], F32)
                mbias = sbt(as_, [128, 4, 64], F32)
                MTsb = sbt(as_, [64, 512], BF)
                pT = [sbt(as_, [128, 512], BF) for _ in range(3)]
                rden = sbt(as_, [128, 512], F32)
                sqb = [sbt(as_, [128, 512], BF) for _ in range(2)]
                rt = sbt(as_, [128, 512], F32)
                rstd = sbt(as_, [128, 512], F32)
                att = sbt(as_, [128, 4, 512], F32)
                WS = Stream(as_, [128, 1024], 4, chunk=1024)
                T.op(DVE, lambda: nc.vector.memset(ksum.t[:], 0.0), writes=[ksum])
                T.op(DVE, lambda: nc.vector.memset(Kx.t[:], 0.0), writes=[Kx])
                T.op(DVE, lambda: nc.vector.memset(Qx.t[:], 0.0), writes=[Qx])
                T.op(DVE, lambda: nc.vector.memset(Qm.t[:], 0.0), writes=[Qm])
                Qx_ds2 = T.newds()
                cast_dma(Kx, Kx.t[0:12, :], kaug_d, S, Kx.ds)
                for tg in range(4):
                    WS.start([win_d[n] for n in range(16)])
                for tg in range(4):
                    c0 = tg * 512
                    WS.get(tg * 16)
                    T.dma(POOL, Qx.t[8:12, :, :], qaug_d[:, :, c0:c0 + 512], Qx.ds, writes=[Qx])
                    prenorm(c0, G_MIXPRE, un, 0, sqb, banks[6], rt, rstd)
                    for n in range(8):
                        sl = WS.get(tg * 16 + n)
                        pb = banks[n % 2]
                        T.pre(PE, reads=[sl, un], writes=[pb])
                        for kc in range(8):
                            ins = MM(pb.t[:, :], lhsT=sl.t[:, kc * 128:(kc + 1) * 128], rhs=un.t[:, kc, :],
                                                   start=(kc == 0), stop=(kc == 7))
                        T.post(PE, ins, reads=[sl, un], writes=[pb])
                        if n < 4:
                            T.op(ACT, lambda pb=pb, n=n: nc.scalar.copy(out=Kpk.t[:, n, c0:c0 + 512], in_=pb.t[:, :]),
                                 reads=[pb], writes=[Kpk])
                            T.op(DVE, lambda pb=pb, n=n: nc.vector.tensor_reduce(
                                out=ksum.t[:, n, 2 * tg:2 * tg + 2], in_=pb.t[:, :].rearrange("p (a b) -> p a b", b=256),
                                axis=AX.X, op=ALU.add), reads=[pb], writes=[ksum])
                        else:
                            for e in range(2):
                                T.op(ACT, lambda pb=pb, n=n, e=e: nc.scalar.mul(out=Qm.t[64 * e:64 * e + 64, 2 * (n - 4) + e, :],
                                                                               in_=pb.t[64 * e:64 * e + 64, :], mul=0.125),
                                     reads=[pb], writes=[Qm])
                    for c in range(4):
                        sl = WS.get(tg * 16 + 8 + c)
                        pb = banks[2 + c % 2]
                        T.pre(PE, reads=[sl, un], writes=[pb])
                        for tt in range(4):
                            for kc in range(8):
                                ins = MM(pb.t[:, tt * 128:(tt + 1) * 128], lhsT=un.t[:, kc, tt * 128:(tt + 1) * 128],
                                                       rhs=sl.t[:, kc * 128:(kc + 1) * 128], start=(kc == 0), stop=(kc == 7))
                        T.post(PE, ins, reads=[sl, un], writes=[pb])
                        T.op(DVE, lambda pb=pb, c=c: nc.vector.tensor_copy(
                            out=V.t[:, 4 * tg:4 * tg + 4, c * 128:(c + 1) * 128],
                            in_=pb.t[:, :].rearrange("p (a b) -> p a b", b=128)), reads=[pb], writes=[V])
                    for k in range(4):
                        sl = WS.get(tg * 16 + 12 + k)
                        pb = banks[4 + k % 2]
                        T.pre(PE, reads=[sl, un], writes=[pb])
                        for kc in range(8):
                            ins = MM(pb.t[:, :], lhsT=sl.t[:, kc * 128:(kc + 1) * 128], rhs=un.t[:, kc, :],
                                                   start=(kc == 0), stop=(kc == 7))
                        T.post(PE, ins, reads=[sl, un], writes=[pb])
                        T.op(ACT, lambda pb=pb, k=k: nc.scalar.copy(out=usT.t[:, k, c0:c0 + 512], in_=pb.t[:, :]),
                             reads=[pb], writes=[usT])
                    if tg < 2:
                        T.op(DVE, lambda: nc.vector.memset(mbias.t[:], 0.0), writes=[mbias])
                    else:
                        T.op(DVE, lambda: nc.vector.tensor_copy(out=ksum_b.t[:], in_=ksum.t[:]), reads=[ksum], writes=[ksum_b])
                        gb = banks[7]
                        T.pre(PE, reads=[Qm, ksum_b], writes=[gb])
                        for qt in range(4):
                            for h in range(8):
                                e, hp = h % 2, h // 2
                                ins = MM(gb.t[:, (qt * 8 + h) * 8:(qt * 8 + h) * 8 + 8],
                                                       lhsT=Qm.t[:, h, qt * 128:(qt + 1) * 128],
                                                       rhs=ksum_b.t[:, hp, :], start=True, stop=True)
                        T.post(PE, ins, reads=[Qm, ksum_b], writes=[gb])
                        T.op(DVE, lambda: nc.vector.memset(Gsb.t[:], -1e30), writes=[Gsb])
                        for half2 in range(2):
                            nb = 2 * tg + half2
                            T.op(DVE, lambda half2=half2, nb=nb: nc.vector.tensor_copy(
                                out=Gsb.t[:, 16 * half2:16 * half2 + 16, 0:nb],
                                in_=gb.t[:, 128 * half2:128 * half2 + 128].rearrange("p (a b) -> p a b", b=8)[:, :, 0:nb]),
                                reads=[gb], writes=[Gsb])
                        for i in range(32):
                            T.op(DVE, lambda i=i: nc.vector.max(out=M8.t[:, i, :], in_=Gsb.t[:, i, :]), reads=[Gsb], writes=[M8])
                        T.op(DVE, lambda: nc.vector.tensor_tensor(out=SEL.t[:], in0=Gsb.t[:], in1=M8.t[:, :, 2:3].to_broadcast([128, 32, 8]),
                                                                  op=ALU.is_ge), reads=[Gsb, M8], writes=[SEL])
                        T.op(DVE, lambda: nc.vector.tensor_scalar(out=mbias.t[:].rearrange("p a (h n) -> p (a h) n", n=8), in0=SEL.t[:],
                                                                  scalar1=1.0, scalar2=BIG, op0=ALU.subtract, op1=ALU.mult),
                             reads=[SEL], writes=[mbias])
                        for half2 in range(2):
                            nb = 2 * tg + half2
                            T.op(DVE, lambda half2=half2, nb=nb: nc.vector.memset(
                                mbias.t[:, 2 * half2:2 * half2 + 2, :].rearrange("p a (h n) -> p a h n", n=8)[:, :, :, nb:nb + 1], 0.0),
                                writes=[mbias])
                    mb = banks[7]
                    T.pre(PE, reads=[mbias, ident_f], writes=[mb])
                    for qt in range(4):
                        ins = MT(mb.t[0:64, qt * 128:(qt + 1) * 128], mbias.t[:, qt, :], ident_f.t[:, :])
                    T.post(PE, ins, reads=[mbias, ident_f], writes=[mb])
                    T.op(ACT, lambda: nc.scalar.copy(out=MTsb.t[:, :], in_=mb.t[0:64, :]), reads=[mb], writes=[MTsb])
                    for h in range(8):
                        T.dma(SP, Qx.t[0:8, h, :], MTsb.t[8 * h:8 * h + 8, :], Qx_ds2, reads=[MTsb], writes=[Qx])
                    nkt = 4 * tg + 4
                    for hp in range(4):
                        numP, denP = banks[2 + hp % 2], banks[4 + hp % 2]
                        for e in range(2):
                            h = 2 * hp + e
                            rows = slice(64 * e, 64 * e + 64)
                            pend = None
                            for kt in range(nkt):
                                j = kt - 4 * tg
                                ql = 128 * j if j > 0 else 0
                                sP = banks[kt % 2]
                                rd = [Kpk, Qm, Kx, Qx, ident_b, trineg]
                                T.pre(PE, reads=rd, writes=[sP])
                                MM(sP.t[:, ql:512], lhsT=Kpk.t[:, hp, kt * 128:(kt + 1) * 128],
                                                 rhs=Qm.t[:, h, ql:512], start=True, stop=False)
                                ins = MM(sP.t[:, ql:512], lhsT=Kx.t[:, kt * 128:(kt + 1) * 128],
                                                       rhs=Qx.t[:, h, ql:512], start=False, stop=(j < 0))
                                if j >= 0:
                                    ins = MM(sP.t[:, ql:ql + 128], lhsT=ident_b.t[:, :], rhs=trineg.t[:, :],
                                                           start=False, stop=True)
                                T.post(PE, ins, reads=rd, writes=[sP])
                                p = pT[kt % 3]
                                T.op(ACT, lambda p=p, sP=sP, ql=ql: nc.scalar.activation(out=p.t[:, ql:512], in_=sP.t[:, ql:512], func=AF.Exp),
                                     reads=[sP], writes=[p])
                                if pend is not None:
                                    pend()

                                def pend(p=p, kt=kt, ql=ql):
                                    T.pre(PE, reads=[p, V, ones_b], writes=[numP, denP])
                                    MM(numP.t[rows, ql:512], lhsT=V.t[:, kt, h * 64:(h + 1) * 64], rhs=p.t[:, ql:512],
                                                     start=(kt == 0), stop=(kt == nkt - 1))
                                    ins2 = MM(denP.t[rows, ql:512], lhsT=ones_b.t[:, 0:64], rhs=p.t[:, ql:512],
                                                            start=(kt == 0), stop=(kt == nkt - 1))
                                    T.post(PE, ins2, reads=[p, V, ones_b], writes=[numP, denP])
                            pend()
                        T.op(DVE, lambda: nc.vector.reciprocal(out=rden.t[:, :], in_=denP.t[:, :]), reads=[denP], writes=[rden])
                        T.op(DVE, lambda: nc.vector.tensor_tensor(out=att.t[:, hp, :], in0=numP.t[:, :], in1=rden.t[:, :],
                                                                  op=ALU.mult), reads=[numP, rden], writes=[att])
                    sq_and_sum(sqb, lambda i: att.t[:, i, :], [att], 4, banks[6])
                    rstd_from(banks[6], rt, rstd, 512.0)
                    for hp in range(4):
                        T.op(DVE, lambda hp=hp: nc.vector.scalar_tensor_tensor(
                            out=matt.t[:, hp, c0:c0 + 512], in0=att.t[:, hp, :], scalar=gcol(G_ATT + hp), in1=rstd.t[:, :],
                            op0=ALU.mult, op1=ALU.mult), reads=[att, rstd, gv], writes=[matt])
            T.barrier()
            if STOP >= 3:
             with contextlib.ExitStack() as ss_:
                CS = sbt(ss_, [128, 2, 16, 128], BF)
                BB = sbt(ss_, [128, 16, 2, 128], BF)
                CC = sbt(ss_, [128, 16, 3, 64], BF)
                DDm = sbt(ss_, [128, 512], BF, ds=True)
                wglu = sbt(ss_, [128, 2048], BF, ds=True)
                mag_s = sbt(ss_, [128, 16], F32)
                ECS = sbt(ss_, [128, 2, 16], F32)
                Hc = sbt(ss_, [128, 2, 16], F32)
                cast_dma(DDm, DDm.t, dlay_d, 512, DDm.ds)
                cast_dma(wglu, wglu.t, wglu_d, 2048, wglu.ds)

                def sincos(stk, ang, n, sinb, cosb, tmpF, tmpI):
                    T.op(DVE, lambda: nc.vector.tensor_scalar(out=tmpI.t[:, :n], in0=ang.t[:, :n], scalar1=1.0 / (2 * PI), scalar2=None,
                                                              op0=ALU.mult), reads=[ang], writes=[tmpI])
                    T.op(DVE, lambda: nc.vector.tensor_copy(out=tmpF.t[:, :n], in_=tmpI.t[:, :n]), reads=[tmpI], writes=[tmpF])
                    T.op(DVE, lambda: nc.vector.scalar_tensor_tensor(out=tmpF.t[:, :n], in0=tmpF.t[:, :n], scalar=-2 * PI, in1=ang.t[:, :n],
                                                                     op0=ALU.mult, op1=ALU.add), reads=[tmpF, ang], writes=[tmpF])
                    T.op(DVE, lambda: nc.vector.tensor_scalar(out=tmpF.t[:, :n], in0=tmpF.t[:, :n], scalar1=-PI, scalar2=PI,
                                                              op0=ALU.max, op1=ALU.min), reads=[tmpF], writes=[tmpF])
                    T.op(ACT, lambda: nc.scalar.activation(out=sinb.t[:, :n], in_=tmpF.t[:, :n], func=AF.Sin), reads=[tmpF], writes=[sinb])
                    T.op(ACT, lambda: nc.scalar.activation(out=tmpF.t[:, :n], in_=tmpF.t[:, :n], func=AF.Abs), reads=[tmpF, sinb], writes=[tmpF])
                    T.op(DVE, lambda: nc.vector.tensor_scalar(out=tmpF.t[:, :n], in0=tmpF.t[:, :n], scalar1=-1.0, scalar2=PI / 2,
                                                              op0=ALU.mult, op1=ALU.add), reads=[tmpF], writes=[tmpF])
                    T.op(ACT, lambda: nc.scalar.activation(out=cosb.t[:, :n], in_=tmpF.t[:, :n], func=AF.Sin), reads=[tmpF], writes=[cosb])

                TT = lambda o, a, b, op, rd, wr: T.op(DVE, lambda: nc.vector.tensor_tensor(out=o, in0=a, in1=b, op=op), reads=rd, writes=wr)
                with contextlib.ExitStack() as st:
                    lamst = sbt(st, [128, 48], F32, ds=True)
                    iota = sbt(st, [128, 128], F32, ds=True)
                    clay = sbt(st, [128, 2 * 1024], F32, ds=True)
                    T.dma(SP, lamst.t[:], lamst_d, lamst.ds, writes=[lamst])
                    T.dma(SP, iota.t[:], cst_d[:, 256:384], iota.ds, writes=[iota])
                    T.dma(SP, clay.t[:], clay_d, clay.ds, writes=[clay])
                    W = [sbt(st, [128, 2048], F32) for _ in range(3)]
                    WI = sbt(st, [128, 2048], I32)
                    sS = sbt(st, [128, 2048], F32)
                    sC = sbt(st, [128, 2048], F32)
                    dts, th_s, e1 = W[0], W[1], W[2]
                    T.op(ACT, lambda: nc.scalar.activation(out=dts.t[:, :16], in_=lamst.t[:, 32:48], func=AF.Exp), reads=[lamst], writes=[dts])
                    TT(e1.t[:, :16], lamst.t[:, 0:16], dts.t[:, :16], ALU.mult, [lamst, dts], [e1])
                    T.op(ACT, lambda: nc.scalar.activation(out=mag_s.t[:, :], in_=e1.t[:, :16], func=AF.Exp), reads=[e1], writes=[mag_s])
                    TT(th_s.t[:, :16], lamst.t[:, 16:32], dts.t[:, :16], ALU.mult, [lamst, dts], [th_s])
                    PH = W[2]
                    for j in range(16):
                        T.op(DVE, lambda j=j: nc.vector.tensor_scalar(out=PH.t[:, j * 128:(j + 1) * 128], in0=iota.t[:, :],
                                                                      scalar1=th_s.t[:, j:j + 1], scalar2=None, op0=ALU.mult),
                             reads=[iota, th_s], writes=[PH])
                    sincos(st, PH, 2048, sS, sC, W[0], WI)
                    j3 = lambda ap: ap.rearrange("p (j t) -> p j t", t=128)
                    T.op(DVE, lambda: nc.vector.tensor_copy(out=CS.t[:, 0, :, :], in_=j3(sC.t[:, :])), reads=[sC], writes=[CS])
                    T.op(DVE, lambda: nc.vector.tensor_copy(out=CS.t[:, 1, :, :], in_=j3(sS.t[:, :])), reads=[sS], writes=[CS])
                    T.op(DVE, lambda: nc.vector.tensor_copy(out=ECS.t[:, 0, :], in_=j3(sC.t[:, :])[:, :, 127]), reads=[sC], writes=[ECS])
                    T.op(DVE, lambda: nc.vector.tensor_copy(out=ECS.t[:, 1, :], in_=j3(sS.t[:, :])[:, :, 127]), reads=[sS], writes=[ECS])
                    c3 = lambda ap: ap.rearrange("p (j c) -> p j c", c=64)
                    T.op(DVE, lambda: nc.vector.tensor_copy(out=CC.t[:, :, 0, :], in_=c3(clay.t[:, 0:1024])), reads=[clay], writes=[CC])
                    T.op(DVE, lambda: nc.vector.tensor_scalar(out=CC.t[:, :, 1, :], in0=c3(clay.t[:, 0:1024]), scalar1=-1.0, scalar2=None,
                                                              op0=ALU.mult), reads=[clay], writes=[CC])
                    T.op(DVE, lambda: nc.vector.tensor_scalar(out=CC.t[:, :, 2, :], in0=c3(clay.t[:, 1024:2048]), scalar1=-1.0, scalar2=None,
                                                              op0=ALU.mult), reads=[clay], writes=[CC])
                T.barrier()
                with contextlib.ExitStack() as st:
                    NH = 1024
                    lamh = sbt(st, [128, 3 * NH], F32, ds=True)
                    blh = sbt(st, [128, 2 * NH], F32, ds=True)
                    W = [sbt(st, [128, NH], F32) for _ in range(7)]
                    WI = sbt(st, [128, NH], I32)
                    sS = sbt(st, [128, NH], F32)
                    sC = sbt(st, [128, NH], F32)
                    for hf in range(2):
                        for q in range(3):
                            T.dma(SP, lamh.t[:, q * NH:(q + 1) * NH], lambc_d[:, q * 2048 + hf * NH:q * 2048 + (hf + 1) * NH], lamh.ds, writes=[lamh])
                        for q in range(2):
                            T.dma(SP, blh.t[:, q * NH:(q + 1) * NH], blay_d[:, q * 2048 + hf * NH:q * 2048 + (hf + 1) * NH], blh.ds, writes=[blh])
                        lr, li, ldt = lamh.t[:, 0:NH], lamh.t[:, NH:2 * NH], lamh.t[:, 2 * NH:3 * NH]
                        dtb, thb, magb = W[0], W[1], W[2]
                        T.op(ACT, lambda: nc.scalar.activation(out=dtb.t[:, :], in_=ldt, func=AF.Exp), reads=[lamh], writes=[dtb])
                        TT(magb.t[:, :], lr, dtb.t[:, :], ALU.mult, [lamh, dtb], [magb])
                        T.op(ACT, lambda: nc.scalar.activation(out=magb.t[:, :], in_=magb.t[:, :], func=AF.Exp), reads=[magb], writes=[magb])
                        TT(thb.t[:, :], li, dtb.t[:, :], ALU.mult, [lamh, dtb], [thb])
                        sincos(st, thb, NH, sS, sC, W[4], WI)
                        are, aim, den, fre, fim, tmp = W[3], W[4], W[5], W[6], W[0], W[1]
                        TT(are.t[:, :], magb.t[:, :], sC.t[:, :], ALU.mult, [magb, sC], [are])
                        TT(aim.t[:, :], magb.t[:, :], sS.t[:, :], ALU.mult, [magb, sS], [aim])
                        T.op(DVE, lambda: nc.vector.tensor_scalar(out=are.t[:, :], in0=are.t[:, :], scalar1=-1.0, scalar2=None, op0=ALU.add),
                             reads=[are], writes=[are])
                        TT(den.t[:, :], lr, lr, ALU.mult, [lamh], [den])
                        TT(tmp.t[:, :], li, li, ALU.mult, [lamh], [tmp])
                        TT(den.t[:, :], den.t[:, :], tmp.t[:, :], ALU.add, [den, tmp], [den])
                        T.op(DVE, lambda: nc.vector.reciprocal(out=den.t[:, :], in_=den.t[:, :]), reads=[den], writes=[den])
                        TT(fre.t[:, :], are.t[:, :], lr, ALU.mult, [are, lamh], [fre])
                        TT(tmp.t[:, :], aim.t[:, :], li, ALU.mult, [aim, lamh], [tmp])
                        TT(fre.t[:, :], fre.t[:, :], tmp.t[:, :], ALU.add, [fre, tmp], [fre])
                        TT(fre.t[:, :], fre.t[:, :], den.t[:, :], ALU.mult, [fre, den], [fre])
                        TT(fim.t[:, :], aim.t[:, :], lr, ALU.mult, [aim, lamh], [fim])
                        TT(tmp.t[:, :], are.t[:, :], li, ALU.mult, [are, lamh], [tmp])
                        TT(fim.t[:, :], fim.t[:, :], tmp.t[:, :], ALU.subtract, [fim, tmp], [fim])
                        TT(fim.t[:, :], fim.t[:, :], den.t[:, :], ALU.mult, [fim, den], [fim])
                        bre, bim = blh.t[:, 0:NH], blh.t[:, NH:2 * NH]
                        t2, t3 = W[2], W[3]
                        v3 = lambda ap: ap.rearrange("p (j c) -> p j c", c=128)
                        js = slice(8 * hf, 8 * hf + 8)
                        TT(t2.t[:, :], fre.t[:, :], bre, ALU.mult, [fre, blh], [t2])
                        TT(t3.t[:, :], fim.t[:, :], bim, ALU.mult, [fim, blh], [t3])
                        TT(BB.t[:, js, 0, :], v3(t2.t[:, :]), v3(t3.t[:, :]), ALU.subtract, [t2, t3], [BB])
                        TT(t2.t[:, :], fre.t[:, :], bim, ALU.mult, [fre, blh], [t2])
                        TT(t3.t[:, :], fim.t[:, :], bre, ALU.mult, [fim, blh], [t3])
                        TT(BB.t[:, js, 1, :], v3(t2.t[:, :]), v3(t3.t[:, :]), ALU.add, [t2, t3], [BB])
                T.barrier()
                TTq = sbt(ss_, [128, 4, 4, 128], BF)
                XTq = sbt(ss_, [128, 2, 4, 128], BF)
                Gq = sbt(ss_, [128, 2, 4, 128], F32)
                Pq = [sbt(ss_, [128, 4, 4, 128], BF) for _ in range(2)]
                ctmp = sbt(ss_, [128, 4, 4], F32)
                ygf = sbt(ss_, [128, 4, 512], F32)
                ygb = sbt(ss_, [128, 4, 512], BF)
                sig = sbt(ss_, [128, 512], F32)
                mssm = sbt(ss_, [128, 4, 512], BF)
                fT = sbt(ss_, [128, 8, 512], F32)
                sqb = [sbt(ss_, [128, 512], BF) for _ in range(2)]
                rt = sbt(ss_, [128, 512], F32)
                rstd = sbt(ss_, [128, 512], F32)
                WO = Stream(ss_, [128, 1024], 3, chunk=1024)
                for tg in range(4):
                    WO.start([wout_d[dt] for dt in range(8)])
                it = 0
                for tg in range(4):
                    c0 = tg * 512
                    for cc in range(4):
                        c = 4 * tg + cc
                        t0 = c0 + 128 * cc
                        for k in range(4):
                            XA, XB = banks[2 * (it % 2)], banks[2 * (it % 2) + 1]
                            P = Pq[it % 2]
                            it += 1
                            T.pre(PE, reads=[BB, usT], writes=[XA, XB])
                            for jl in range(4):
                                j, e = 4 * k + jl, jl // 2
                                rows = slice(64 * e, 64 * e + 64)
                                MM(XA.t[:, jl * 128:(jl + 1) * 128], lhsT=BB.t[:, j, 0, :], rhs=usT.t[:, k, t0:t0 + 128],
                                                 start=True, stop=True)
                                ins = MM(XB.t[:, jl * 128:(jl + 1) * 128], lhsT=BB.t[:, j, 1, :], rhs=usT.t[:, k, t0:t0 + 128],
                                                       start=True, stop=True)
                            T.post(PE, ins, reads=[BB, usT], writes=[XA, XB])
                            q4 = slice(4 * k, 4 * k + 4)
                            xa = XA.t[:, :].rearrange("p (a b) -> p a b", b=128)
                            xb = XB.t[:, :].rearrange("p (a b) -> p a b", b=128)
                            cosq, sinq = CS.t[:, 0, q4, :], CS.t[:, 1, q4, :]
                            T.op(DVE, lambda: nc.vector.tensor_tensor(out=TTq.t[:, 0], in0=xa, in1=cosq, op=ALU.mult), reads=[XA, CS], writes=[TTq])
                            T.op(DVE, lambda: nc.vector.tensor_tensor(out=TTq.t[:, 1], in0=xb, in1=sinq, op=ALU.mult), reads=[XB, CS], writes=[TTq])
                            T.op(DVE, lambda: nc.vector.tensor_tensor(out=TTq.t[:, 2], in0=xb, in1=cosq, op=ALU.mult), reads=[XB, CS], writes=[TTq])
                            T.op(DVE, lambda: nc.vector.tensor_tensor(out=TTq.t[:, 3], in0=xa, in1=sinq, op=ALU.mult), reads=[XA, CS], writes=[TTq])
                            T.op(DVE, lambda: nc.vector.tensor_tensor(out=XTq.t[:, 0], in0=TTq.t[:, 0], in1=TTq.t[:, 1], op=ALU.add),
                                 reads=[TTq], writes=[XTq])
                            T.op(DVE, lambda: nc.vector.tensor_tensor(out=XTq.t[:, 1], in0=TTq.t[:, 2], in1=TTq.t[:, 3], op=ALU.subtract),
                                 reads=[TTq], writes=[XTq])
                            for jl in range(4):
                                j = 4 * k + jl
                                for r in range(2):
                                    T.op(DVE, lambda jl=jl, j=j, r=r: nc.vector.tensor_tensor_scan(
                                        out=Gq.t[:, r, jl, :], data0=mag_s.t[:, j:j + 1].to_broadcast([128, 128]), data1=XTq.t[:, r, jl, :],
                                        initial=(Hc.t[:, r, j:j + 1] if c > 0 else 0.0), op0=ALU.mult, op1=ALU.add),
                                        reads=[XTq, mag_s, Hc], writes=[Gq])
                            T.op(DVE, lambda: nc.vector.tensor_tensor(out=P.t[:, 0:2], in0=Gq.t[:, 0:2], in1=CS.t[:, 0:2, q4, :], op=ALU.mult),
                                 reads=[Gq, CS], writes=[P])
                            T.op(DVE, lambda: nc.vector.tensor_tensor(out=P.t[:, 2], in0=Gq.t[:, 1], in1=cosq, op=ALU.mult), reads=[Gq, CS], writes=[P])
                            T.op(DVE, lambda: nc.vector.tensor_tensor(out=P.t[:, 3], in0=Gq.t[:, 0], in1=sinq, op=ALU.mult), reads=[Gq, CS], writes=[P])
                            gre, gim = Gq.t[:, 0, :, 127], Gq.t[:, 1, :, 127]
                            ec, esn = ECS.t[:, 0, q4], ECS.t[:, 1, q4]
                            T.op(DVE, lambda: nc.vector.tensor_tensor(out=ctmp.t[:, 0], in0=gre, in1=ec, op=ALU.mult), reads=[Gq, ECS], writes=[ctmp])
                            T.op(DVE, lambda: nc.vector.tensor_tensor(out=ctmp.t[:, 1], in0=gim, in1=esn, op=ALU.mult), reads=[Gq, ECS], writes=[ctmp])
                            T.op(DVE, lambda: nc.vector.tensor_tensor(out=ctmp.t[:, 2], in0=gim, in1=ec, op=ALU.mult), reads=[Gq, ECS], writes=[ctmp])
                            T.op(DVE, lambda: nc.vector.tensor_tensor(out=ctmp.t[:, 3], in0=gre, in1=esn, op=ALU.mult), reads=[Gq, ECS], writes=[ctmp])
                            T.op(DVE, lambda: nc.vector.tensor_tensor(out=Hc.t[:, 0, q4], in0=ctmp.t[:, 0], in1=ctmp.t[:, 1], op=ALU.subtract),
                                 reads=[ctmp], writes=[Hc])
                            T.op(DVE, lambda: nc.vector.tensor_tensor(out=Hc.t[:, 1, q4], in0=ctmp.t[:, 2], in1=ctmp.t[:, 3], op=ALU.add),
                                 reads=[ctmp], writes=[Hc])
                            Y = banks[4 + k]
                            rd = [CC, P, DDm, usT]
                            T.pre(PE, reads=rd, writes=[Y])
                            for e in range(2):
                                rows = slice(64 * e, 64 * e + 64)
                                yo = Y.t[rows, cc * 128:(cc + 1) * 128]
                                first = True
                                for jl in (2 * e, 2 * e + 1):
                                    j = 4 * k + jl
                                    for (ci, pi_) in ((0, 0), (1, 1), (2, 2), (2, 3)):
                                        MM(yo, lhsT=CC.t[:, j, ci, :], rhs=P.t[:, pi_, jl, :], start=first, stop=False)
                                        first = False
                                ins = MM(yo, lhsT=DDm.t[:, k * 128 + 64 * e:k * 128 + 64 * e + 64],
                                                       rhs=usT.t[:, k, t0:t0 + 128], start=False, stop=True)
                            T.post(PE, ins, reads=rd, writes=[Y])
                    for k in range(4):
                        Y = banks[4 + k]
                        T.op(ACT, lambda k=k, Y=Y: nc.scalar.activation(out=ygf.t[:, k, :], in_=Y.t[:, :], func=AF.Gelu_apprx_tanh),
                             reads=[Y], writes=[ygf])
                        T.op(DVE, lambda k=k: nc.vector.tensor_copy(out=ygb.t[:, k, :], in_=ygf.t[:, k, :]), reads=[ygf], writes=[ygb])
                    for ko in range(4):
                        GP = banks[ko % 2]
                        T.pre(PE, reads=[wglu, ygb], writes=[GP])
                        for ki in range(4):
                            ins = MM(GP.t[:, :], lhsT=wglu.t[:, ki * 512 + ko * 128:ki * 512 + (ko + 1) * 128], rhs=ygb.t[:, ki, :],
                                                   start=(ki == 0), stop=(ki == 3))
                        T.post(PE, ins, reads=[wglu, ygb], writes=[GP])
                        T.op(ACT, lambda GP=GP, ko=ko: nc.scalar.activation(out=sig.t[:, :], in_=GP.t[:, :], func=AF.Sigmoid,
                                                                          bias=gcol(G_BGLU + ko)), reads=[GP, gv], writes=[sig])
                        T.op(DVE, lambda ko=ko: nc.vector.tensor_tensor(out=ygf.t[:, ko, :], in0=ygf.t[:, ko, :], in1=sig.t[:, :], op=ALU.mult),
                             reads=[ygf, sig], writes=[ygf])
                    sq_and_sum(sqb, lambda i: ygf.t[:, i, :], [ygf], 4, banks[2])
                    rstd_from(banks[2], rt, rstd, 512.0)
                    for ko in range(4):
                        T.op(DVE, lambda ko=ko: nc.vector.scalar_tensor_tensor(
                            out=mssm.t[:, ko, :], in0=ygf.t[:, ko, :], scalar=gcol(G_SSM + ko), in1=rstd.t[:, :],
                            op0=ALU.mult, op1=ALU.mult), reads=[ygf, rstd, gv], writes=[mssm])
                    down_project(WO, tg * 8, 8,
                                 lambda m, ti, c0=c0: (matt.t[:, m, c0:c0 + 512] if m < 4 else mssm.t[:, m - 4, :]), [matt, mssm],
                                 [fT], [c0], G_MIXPOST, 1.0, [banks[0], banks[1]], [banks[3]], sqb, rt, rstd)
            T.barrier()

        if STOP >= 4:
            ffn(G_F2PRE, G_F2POST, w2gu_d, w2d_d)
        T.barrier()
        T.live = True
        T.cut = 10 ** 9
        T.barrier()
        od = T.newds()
        T.dma(SP, out_d, xT.t[:], od, reads=[xT])
        SP.e.wait_ge(od.sem, od.cnt)
    return nc


def _shared_layouts(inp):
    f = np.float32
    A = lambda k: np.ascontiguousarray(np.asarray(inp[k], dtype=f)[0])
    o = {}
    gv = np.zeros((128, 64), f)

    def put(c0, v):
        n = v.shape[0] // 128
        gv[:, c0:c0 + n] = v.reshape(n, 128).T
    put(G_F1PRE, A("ffn1_pre_g")); put(G_F1POST, A("ffn1_post_g")); put(G_MIXPRE, A("mix_pre_g"))
    put(G_ATT, A("attn_out_g")); put(G_SSM, A("ssm_out_g")); put(G_MIXPOST, A("mix_post_g"))
    put(G_F2PRE, A("ffn2_pre_g")); put(G_F2POST, A("ffn2_post_g")); put(G_BGLU, A("b_glu"))
    o["gv"] = gv

    def gu(wg, wu):
        a = wg.reshape(8, 128, NFT, 128).transpose(2, 1, 0, 3)
        b = wu.reshape(8, 128, NFT, 128).transpose(2, 1, 0, 3)
        return np.ascontiguousarray(np.concatenate([a, b], axis=2).reshape(NFT, 128, 2048))

    def dn(wd):
        return np.ascontiguousarray(wd.reshape(NFT, 128, 8, 128).transpose(2, 1, 0, 3).reshape(8, 128, DFF))
    o["w1gu"] = gu(A("ffn1_w_gate"), A("ffn1_w_up")); o["w1d"] = dn(A("ffn1_w_down"))
    o["w2gu"] = gu(A("ffn2_w_gate"), A("ffn2_w_up")); o["w2d"] = dn(A("ffn2_w_down"))
    win = A("w_in")
    order = [512 + 128 * i for i in range(4)] + [128 * i for i in range(4)] + [1024 + 128 * i for i in range(8)]
    o["win"] = np.ascontiguousarray(np.stack([win[:, c:c + 128].reshape(8, 128, 128).transpose(1, 0, 2).reshape(128, 1024) for c in order]))
    o["wglu"] = np.ascontiguousarray(A("w_glu").reshape(4, 128, 512).transpose(1, 0, 2).reshape(128, 2048))
    wo = A("w_out")
    o["wout"] = np.ascontiguousarray(wo.reshape(8, 128, 8, 128).transpose(2, 1, 0, 3).reshape(8, 128, 1024))
    lr, li, ld = A("lam_re"), A("lam_im"), A("log_dt")
    st = lambda a: a.reshape(16, 2, 64).transpose(1, 2, 0).reshape(128, 16)
    ldf = np.repeat(ld[:, None], 64, axis=1)
    o["lamst"] = np.ascontiguousarray(np.concatenate([st(lr), st(li), st(ldf)], axis=1))
    bc = np.concatenate([lr.reshape(-1), li.reshape(-1), ldf.reshape(-1)])
    o["lambc"] = np.ascontiguousarray(np.broadcast_to(bc[None, :], (128, 6144)))
    bre, bim = A("b_re"), A("b_im")
    bl = np.zeros((2, 128, 16, 128), f)
    cre, cim = A("c_re"), A("c_im")
    cl = np.zeros((2, 128, 16, 64), f)
    for j in range(16):
        q = j % 4
        for g2 in range(2):
            g = 2 * j + g2
            r0 = 32 * q + 16 * g2
            bl[0, r0:r0 + 16, j, 64 * g2:64 * g2 + 64] = bre[g].T
            bl[1, r0:r0 + 16, j, 64 * g2:64 * g2 + 64] = bim[g].T
            cc0 = 32 * (j % 2) + 16 * g2
            cl[0, 64 * g2:64 * g2 + 64, j, cc0:cc0 + 16] = cre[g].T
            cl[1, 64 * g2:64 * g2 + 64, j, cc0:cc0 + 16] = cim[g].T
    o["blay"] = np.ascontiguousarray(bl.transpose(1, 0, 2, 3).reshape(128, 4096))
    o["clay"] = np.ascontiguousarray(cl.transpose(1, 0, 2, 3).reshape(128, 2048))
    dsk = A("d_skip").reshape(512)
    dl = np.zeros((128, 4, 128), f)
    for k in range(4):
        dl[np.arange(128), k, np.arange(128)] = dsk[k * 128:(k + 1) * 128]
    o["dlay"] = dl.reshape(128, 512)
    pos = np.arange(S)
    ka = np.zeros((12, S), f)
    ka[pos // 256, pos] = 1.0
    ka[8] = 1.0; ka[9] = 1.0; ka[10] = 128.0 * (pos // 128); ka[11] = pos % 128
    o["kaugc"] = ka
    qa = np.zeros((4, 8, S), f)
    for h in range(8):
        sl = 2.0 ** (-(h + 1))
        qa[0, h] = -sl * 128.0 * (pos // 128); qa[1, h] = -sl * (pos % 128); qa[2, h] = sl; qa[3, h] = sl
    o["qaugc"] = qa
    cst = np.zeros((128, 384), f)
    cst[:, 0:128] = np.eye(128, dtype=f)
    kk, qq = np.meshgrid(np.arange(128), np.arange(128), indexing="ij")
    cst[:, 128:256] = np.where(kk > qq, -BIG, 0.0)
    cst[:, 256:384] = np.arange(1, 129, dtype=f)[None, :]
    o["cst"] = cst
    return o


_NC = None


def kernel(**inputs):
    global _NC
    x = np.asarray(inputs["x"], dtype=np.float32)
    nb = x.shape[0]
    sh = _shared_layouts(inputs)
    in_maps = []
    for b in range(nb):
        m = dict(sh)
        m["xT"] = np.ascontiguousarray(x[b].T.reshape(8, 128, S).transpose(1, 0, 2))
        in_maps.append(m)
    if _NC is None:
        _NC = build()
    res = run_bass_kernel_spmd(_NC, in_maps, core_ids=list(range(nb)))
    out = np.empty_like(x)
    for b in range(nb):
        yT = np.asarray(res.results[b]["yT"], dtype=np.float32)
        out[b] = yT.transpose(1, 0, 2).reshape(1024, S).T
    return out
```

```python
import contextlib
import math
import numpy as np
import concourse.bass as bass
import concourse.mybir as mybir
from concourse.bass_utils import run_bass_kernel_spmd

F32 = mybir.dt.float32
BF = mybir.dt.bfloat16
I32 = mybir.dt.int32
AF = mybir.ActivationFunctionType
ALU = mybir.AluOpType
AX = mybir.AxisListType
PI = math.pi
S = 2048
DFF = 2816
NFT = 22
EPS = 1e-6
BIG = 30000.0

G_F1PRE, G_F1POST, G_MIXPRE, G_ATT, G_SSM, G_MIXPOST, G_F2PRE, G_F2POST, G_BGLU = 0, 8, 16, 24, 28, 32, 40, 48, 56


class Eng:
    def __init__(self, nc, e, name, es):
        self.e, self.name = e, name
        self.sem = es.enter_context(nc.semaphore("s_" + name))
        self.cnt = 0
        self.seen = {}


class DS:
    def __init__(self, nc, name, es):
        self.sem = es.enter_context(nc.semaphore(name))
        self.cnt = 0
        self.name = name


class Buf:
    def __init__(self, t, ds=None, dj=False, psum=False):
        self.psum = psum
        self.dj = dj
        self.t = t
        self.w = None
        self.r = {}
        self.ds = ds


class Trk:
    def __init__(self, nc, es):
        self.nc = nc
        self.es = es
        self.PE = Eng(nc, nc.tensor, "pe", es)
        self.ACT = Eng(nc, nc.scalar, "act", es)
        self.DVE = Eng(nc, nc.vector, "dve", es)
        self.POOL = Eng(nc, nc.gpsimd, "pool", es)
        self.SP = Eng(nc, nc.sync, "sp", es)
        self.engs = [self.PE, self.ACT, self.DVE, self.POOL, self.SP]
        self.dss = []
        self.nds = 0
        self.n = 0
        self.live = True
        self.cut = CUT

    def newds(self):
        self.nds += 1
        d = DS(self.nc, "d%d" % self.nds, self.es)
        self.dss.append(d)
        return d

    def wait(self, E, tok):
        src, val = tok
        if src is E and E is self.PE:
            return
        k = id(src)
        if E.seen.get(k, 0) >= val:
            return
        E.e.wait_ge(src.sem, val)
        E.seen[k] = val

    def pre(self, E, reads=(), writes=()):
        if self.n >= self.cut:
            self.live = False
        if not self.live:
            return
        for b in reads:
            if b.w is not None:
                self.wait(E, b.w)
            if b.psum:
                for src, val in list(b.r.values()):
                    if src is not E:
                        self.wait(E, (src, val))
        for b in writes:
            if b.w is not None and (b.w[0] is not E or not b.dj):
                self.wait(E, b.w)
            for src, val in list(b.r.values()):
                self.wait(E, (src, val))

    def _reg(self, tok, reads, writes):
        for b in reads:
            b.r[id(tok[0])] = tok
        for b in writes:
            b.w = tok
            b.r = {}

    def post(self, E, inst, reads=(), writes=()):
        if not self.live:
            return
        self.n += 1
        E.cnt += 1
        inst.then_inc(E.sem, 1)
        self._reg((E, E.cnt), reads, writes)

    def op(self, E, fn, reads=(), writes=()):
        self.pre(E, reads, writes)
        if not self.live:
            return
        inst = fn()
        self.post(E, inst, reads, writes)

    def dma(self, Q, out, in_, ds, reads=(), writes=(), **kw):
        self.pre(Q, reads, writes)
        if not self.live:
            return
        self.n += 1
        inst = Q.e.dma_start(out=out, in_=in_, **kw)
        ds.cnt += 16
        inst.then_inc(ds.sem, 16)
        self._reg((ds, ds.cnt), reads, writes)

    def mark(self, name):
        MARKS.append((name, self.n))

    def barrier(self):
        if not self.live:
            return
        for E in self.engs:
            for Fe in self.engs:
                if Fe is not E and Fe.cnt > 0:
                    self.wait(E, (Fe, Fe.cnt))
            for d in self.dss:
                if d.cnt > 0:
                    self.wait(E, (d, d.cnt))


import os
STOP = int(os.environ.get('KSTOP', '99'))
CUT = int(os.environ.get('KCUT', str(10 ** 9)))
MARKS = []


def build():
    nc = bass.Bass("TRN2", target_bir_lowering=False)

    def D(name, shape, kind="ExternalInput", dt=F32):
        return nc.dram_tensor(name, shape, dt, kind=kind).ap()

    xT_d = D("xT", [128, 8, S])
    gv_d = D("gv", [128, 64])
    w1gu_d = D("w1gu", [NFT, 128, 2048])
    w1d_d = D("w1d", [8, 128, DFF])
    w2gu_d = D("w2gu", [NFT, 128, 2048])
    w2d_d = D("w2d", [8, 128, DFF])
    win_d = D("win", [16, 128, 1024])
    wglu_d = D("wglu", [128, 2048])
    wout_d = D("wout", [8, 128, 1024])
    lamst_d = D("lamst", [128, 48])
    lambc_d = D("lambc", [128, 3 * 2048])
    blay_d = D("blay", [128, 2 * 2048])
    clay_d = D("clay", [128, 2 * 1024])
    dlay_d = D("dlay", [128, 512])
    kaug_d = D("kaugc", [12, S])
    qaug_d = D("qaugc", [4, 8, S])
    cst_d = D("cst", [128, 3 * 128])
    out_d = D("yT", [128, 8, S], kind="ExternalOutput")

    es = contextlib.ExitStack()
    with es:
        T = Trk(nc, es)
        PE, ACT, DVE, POOL, SP = T.PE, T.ACT, T.DVE, T.POOL, T.SP
        cnt = [0]

        def sbt(stack, shape, dt, ds=False, dj=False):
            cnt[0] += 1
            t = stack.enter_context(nc.sbuf_tensor("t%d" % cnt[0], shape, dt))
            return Buf(t, T.newds() if ds else None, dj)

        xT = sbt(es, [128, 8, S], F32, ds=True)
        gv = sbt(es, [128, 64], F32, ds=True)
        ident_f = sbt(es, [128, 128], F32, ds=True)
        ident_b = sbt(es, [128, 128], BF, ds=True)
        trineg = sbt(es, [128, 128], BF, ds=True)
        ones_b = sbt(es, [128, 128], BF)
        banks = []
        for i in range(8):
            banks.append(Buf(es.enter_context(nc.psum_tensor("bank%d" % i, [128, 512], F32)), psum=True))

        T.dma(SP, xT.t[:], xT_d, xT.ds, writes=[xT])
        T.dma(SP, gv.t[:], gv_d, gv.ds, writes=[gv])
        T.dma(SP, ident_f.t[:], cst_d[:, 0:128], ident_f.ds, writes=[ident_f])
        T.dma(POOL, ident_b.t[:], cst_d[:, 0:128], ident_b.ds, writes=[ident_b])
        T.dma(POOL, trineg.t[:], cst_d[:, 128:256], trineg.ds, writes=[trineg])
        T.op(DVE, lambda: nc.vector.memset(ones_b.t[:], 1.0), writes=[ones_b])

        def gcol(c):
            return gv.t[:, c:c + 1]

        def MM(*a, **k):
            return nc.tensor.matmul(*a, **k) if T.live else None

        def MT(*a, **k):
            return nc.tensor.transpose(*a, **k) if T.live else None

        def cast_dma(buf, out_ap, in_ap, ncols, ds, chunk=512):
            nchunk = (ncols + chunk - 1) // chunk
            w = (ncols + nchunk - 1) // nchunk
            for a in range(0, ncols, w):
                b = min(ncols, a + w)
                T.dma(POOL, out_ap[:, a:b], in_ap[:, a:b], ds, writes=[buf])

        class Stream:
            def __init__(self, stack, shape, nslots, chunk=512):
                self.chunk = chunk
                self.slots = [sbt(stack, shape, BF, ds=True) for _ in range(nslots)]
                self.n = nslots
                self.jobs = []
                self.issued = 0
                self.base = 0

            def start(self, jobs):
                self.jobs = self.jobs + list(jobs)

            def _issue(self, k):
                sl = self.slots[k % self.n]
                cast_dma(sl, sl.t, self.jobs[k], sl.t.shape[1], sl.ds, chunk=self.chunk)

            def get(self, i):
                hi = min(i + self.n - 1, len(self.jobs) - 1)
                while self.issued <= hi:
                    self._issue(self.issued)
                    self.issued += 1
                return self.slots[i % self.n]

        def sq_and_sum(stack_bufs, src_ap_fn, src_bufs, ntiles, ssb, ncols=512):
            sqb = stack_bufs
            for i in range(ntiles):
                q = sqb[i % len(sqb)]
                T.op(ACT, lambda q=q, i=i: nc.scalar.activation(out=q.t[:, :ncols], in_=src_ap_fn(i), func=AF.Square),
                     reads=src_bufs, writes=[q])
                T.op(PE, lambda q=q, i=i: MM(ssb.t[:, :ncols], lhsT=ones_b.t[:], rhs=q.t[:, :ncols],
                                                          start=(i == 0), stop=(i == ntiles - 1)),
                     reads=[q, ones_b], writes=[ssb])

        def rstd_from(ssb, rt, rstd, dim, ncols=512):
            T.op(DVE, lambda: nc.vector.tensor_scalar(out=rt.t[:, :ncols], in0=ssb.t[:, :ncols], scalar1=1.0 / dim,
                                                      scalar2=EPS, op0=ALU.mult, op1=ALU.add),
                 reads=[ssb], writes=[rt])
            T.op(ACT, lambda: nc.scalar.activation(out=rt.t[:, :ncols], in_=rt.t[:, :ncols], func=AF.Sqrt),
                 reads=[rt], writes=[rt])
            T.op(DVE, lambda: nc.vector.reciprocal(out=rstd.t[:, :ncols], in_=rt.t[:, :ncols]),
                 reads=[rt], writes=[rstd])

        def prenorm(c0, g0, outb, ocol0, sqb, ssb, rt, rstd):
            sq_and_sum(sqb, lambda i: xT.t[:, i, c0:c0 + 512], [xT], 8, ssb)
            rstd_from(ssb, rt, rstd, 1024.0)
            for kc in range(8):
                T.op(DVE, lambda kc=kc: nc.vector.scalar_tensor_tensor(
                    out=outb.t[:, kc, ocol0:ocol0 + 512], in0=xT.t[:, kc, c0:c0 + 512], scalar=gcol(g0 + kc),
                    in1=rstd.t[:, :], op0=ALU.mult, op1=ALU.mult), reads=[xT, rstd, gv], writes=[outb])

        def down_project(stream, job0, nk, rhs_fn, rhs_bufs, fTs, c0s, gpost, half_scale, ybanks, ssbs, sqb, rt, rstd):
            pend = None
            step = 0
            ng = len(c0s)
            for dt in range(8):
                sl = stream.get(job0 + dt)
                for ti in range(ng):
                    yb = ybanks[step % 2]
                    q = sqb[step % 2]
                    step += 1
                    fT, ssb = fTs[ti], ssbs[ti]
                    T.pre(PE, reads=[sl] + rhs_bufs, writes=[yb])
                    for k in range(nk):
                        ins = MM(yb.t[:, :], lhsT=sl.t[:, k * 128:(k + 1) * 128], rhs=rhs_fn(k, ti),
                                               start=(k == 0), stop=(k == nk - 1))
                    T.post(PE, ins, reads=[sl] + rhs_bufs, writes=[yb])
                    if pend is not None:
                        pend()
                    T.op(ACT, lambda q=q, yb=yb: nc.scalar.activation(out=q.t[:, :], in_=yb.t[:, :], func=AF.Square),
                         reads=[yb], writes=[q])
                    T.op(DVE, lambda yb=yb, dt=dt, fT=fT: nc.vector.tensor_copy(out=fT.t[:, dt, :], in_=yb.t[:, :]),
                         reads=[yb], writes=[fT])

                    def pend(q=q, dt=dt, ssb=ssb):
                        T.op(PE, lambda: MM(ssb.t[:, :], lhsT=ones_b.t[:], rhs=q.t[:, :],
                                                          start=(dt == 0), stop=(dt == 7)),
                             reads=[q, ones_b], writes=[ssb])
            pend()
            T.mark('dp_finalize')
            for ti in range(ng):
                fT, c0 = fTs[ti], c0s[ti]
                rstd_from(ssbs[ti], rt, rstd, 1024.0)
                for dt in range(8):
                    T.op(DVE, lambda dt=dt, fT=fT: nc.vector.scalar_tensor_tensor(
                        out=fT.t[:, dt, :], in0=fT.t[:, dt, :], scalar=gcol(gpost + dt), in1=rstd.t[:, :],
                        op0=ALU.mult, op1=ALU.mult), reads=[fT, rstd, gv], writes=[fT])
                    T.op(DVE, lambda dt=dt, fT=fT, c0=c0: nc.vector.scalar_tensor_tensor(
                        out=xT.t[:, dt, c0:c0 + 512], in0=fT.t[:, dt, :], scalar=half_scale, in1=xT.t[:, dt, c0:c0 + 512],
                        op0=ALU.mult, op1=ALU.add), reads=[fT, xT], writes=[xT])

        def ffn(gpre, gpost, wgu_d, wd_d):
            with contextlib.ExitStack() as fs:
                xn = sbt(fs, [128, 8, 1024], BF, dj=True)
                actT = sbt(fs, [128, NFT, 1024], BF, dj=True)
                fTs = [sbt(fs, [128, 8, 512], F32) for _ in range(2)]
                sg = [sbt(fs, [128, 512], BF) for _ in range(2)]
                sqb = [sbt(fs, [128, 512], BF) for _ in range(2)]
                rt = sbt(fs, [128, 512], F32)
                rstd = sbt(fs, [128, 512], F32)
                SA = Stream(fs, [128, 2048], 5, chunk=1024)
                SB = Stream(fs, [128, DFF], 3, chunk=1408)
                for half in range(2):
                    SA.start([wgu_d[ft] for ft in range(NFT)])
                    SB.start([wd_d[dt] for dt in range(8)])
                for half in range(2):
                    h0 = half * 1024
                    T.mark('ffn_half%d_start' % half)
                    SA.get(half * NFT)
                    for tg in range(2):
                        prenorm(h0 + tg * 512, gpre, xn, tg * 512, sqb, banks[6 + tg], rt, rstd)
                    T.mark('phaseA_start')
                    for ft in range(NFT):
                        sl = SA.get(half * NFT + ft)
                        if ft == 1:
                            T.mark('phaseA_ft1')
                        for tg in range(2):
                            hg, hu = banks[2 * tg], banks[2 * tg + 1]
                            T.pre(PE, reads=[sl, xn], writes=[hg, hu])
                            for kc in range(8):
                                MM(hg.t[:, :], lhsT=sl.t[:, kc * 128:(kc + 1) * 128],
                                                 rhs=xn.t[:, kc, tg * 512:(tg + 1) * 512], start=(kc == 0), stop=(kc == 7))
                            for kc in range(8):
                                ins = MM(hu.t[:, :], lhsT=sl.t[:, 1024 + kc * 128:1024 + (kc + 1) * 128],
                                                       rhs=xn.t[:, kc, tg * 512:(tg + 1) * 512], start=(kc == 0), stop=(kc == 7))
                            T.post(PE, ins, reads=[sl, xn], writes=[hg, hu])
                            s = sg[tg]
                            T.op(ACT, lambda s=s, hg=hg: nc.scalar.activation(out=s.t[:, :], in_=hg.t[:, :], func=AF.Silu),
                                 reads=[hg], writes=[s])
                            T.op(DVE, lambda s=s, hu=hu, ft=ft, tg=tg: nc.vector.tensor_tensor(
                                out=actT.t[:, ft, tg * 512:(tg + 1) * 512], in0=hu.t[:, :], in1=s.t[:, :], op=ALU.mult),
                                reads=[hu, s], writes=[actT])
                    T.mark('phaseB_start')
                    down_project(SB, half * 8, NFT, lambda k, ti: actT.t[:, k, ti * 512:(ti + 1) * 512], [actT],
                                 fTs, [h0, h0 + 512], gpost, 0.5, [banks[4], banks[5]], [banks[6], banks[7]], sqb, rt, rstd)

        T.barrier()
        if STOP >= 1:
            ffn(G_F1PRE, G_F1POST, w1gu_d, w1d_d)
        T.barrier()

        with contextlib.ExitStack() as ms:
          if STOP >= 2:
            usT = sbt(ms, [128, 4, S], BF, dj=True)
            matt = sbt(ms, [128, 4, S], BF, dj=True)
            with contextlib.ExitStack() as as_:
                Kpk = sbt(as_, [128, 4, S], BF, dj=True)
                Kx = sbt(as_, [128, S], BF, ds=True)
                V = sbt(as_, [128, 16, 512], BF, dj=True)
                Qm = sbt(as_, [128, 8, 512], BF)
                Qx = sbt(as_, [128, 8, 512], BF, ds=True)
                un = sbt(as_, [128, 8, 512], BF, dj=True)
                ksum = sbt(as_, [128, 4, 8], F32)
                ksum_b = sbt(as_, [128, 4, 8], BF)
                Gsb = sbt(as_, [128, 32, 8], F32)
                M8 = sbt(as_, [128, 32, 8], F32)
                SEL = sbt(as_, [128, 32, 8], F32)
                mbias = sbt(as_, [128, 4, 64], F32)
                MTsb = sbt(as_, [64, 512], BF)
                pT = [sbt(as_, [128, 512], BF) for _ in range(3)]
                rden = sbt(as_, [128, 512], F32)
                sqb = [sbt(as_, [128, 512], BF) for _ in range(2)]
                rt = sbt(as_, [128, 512], F32)
                rstd = sbt(as_, [128, 512], F32)
                att = sbt(as_, [128, 4, 512], F32)
                WS = Stream(as_, [128, 1024], 4, chunk=1024)
                T.op(DVE, lambda: nc.vector.memset(ksum.t[:], 0.0), writes=[ksum])
                T.op(DVE, lambda: nc.vector.memset(Kx.t[:], 0.0), writes=[Kx])
                T.op(DVE, lambda: nc.vector.memset(Qx.t[:], 0.0), writes=[Qx])
                T.op(DVE, lambda: nc.vector.memset(Qm.t[:], 0.0), writes=[Qm])
                Qx_ds2 = T.newds()
                cast_dma(Kx, Kx.t[0:12, :], kaug_d, S, Kx.ds)
                for tg in range(4):
                    WS.start([win_d[n] for n in range(16)])
                for tg in range(4):
                    c0 = tg * 512
                    WS.get(tg * 16)
                    T.dma(POOL, Qx.t[8:12, :, :], qaug_d[:, :, c0:c0 + 512], Qx.ds, writes=[Qx])
                    prenorm(c0, G_MIXPRE, un, 0, sqb, banks[6], rt, rstd)
                    for n in range(8):
                        sl = WS.get(tg * 16 + n)
                        pb = banks[n % 2]
                        T.pre(PE, reads=[sl, un], writes=[pb])
                        for kc in range(8):
                            ins = MM(pb.t[:, :], lhsT=sl.t[:, kc * 128:(kc + 1) * 128], rhs=un.t[:, kc, :],
                                                   start=(kc == 0), stop=(kc == 7))
                        T.post(PE, ins, reads=[sl, un], writes=[pb])
                        if n < 4:
                            T.op(ACT, lambda pb=pb, n=n: nc.scalar.copy(out=Kpk.t[:, n, c0:c0 + 512], in_=pb.t[:, :]),
                                 reads=[pb], writes=[Kpk])
                            T.op(DVE, lambda pb=pb, n=n: nc.vector.tensor_reduce(
                                out=ksum.t[:, n, 2 * tg:2 * tg + 2], in_=pb.t[:, :].rearrange("p (a b) -> p a b", b=256),
                                axis=AX.X, op=ALU.add), reads=[pb], writes=[ksum])
                        else:
                            for e in range(2):
                                T.op(ACT, lambda pb=pb, n=n, e=e: nc.scalar.mul(out=Qm.t[64 * e:64 * e + 64, 2 * (n - 4) + e, :],
                                                                               in_=pb.t[64 * e:64 * e + 64, :], mul=0.125),
                                     reads=[pb], writes=[Qm])
                    for c in range(4):
                        sl = WS.get(tg * 16 + 8 + c)
                        pb = banks[2 + c % 2]
                        T.pre(PE, reads=[sl, un], writes=[pb])
                        for tt in range(4):
                            for kc in range(8):
                                ins = MM(pb.t[:, tt * 128:(tt + 1) * 128], lhsT=un.t[:, kc, tt * 128:(tt + 1) * 128],
                                                       rhs=sl.t[:, kc * 128:(kc + 1) * 128], start=(kc == 0), stop=(kc == 7))
                        T.post(PE, ins, reads=[sl, un], writes=[pb])
                        T.op(DVE, lambda pb=pb, c=c: nc.vector.tensor_copy(
                            out=V.t[:, 4 * tg:4 * tg + 4, c * 128:(c + 1) * 128],
                            in_=pb.t[:, :].rearrange("p (a b) -> p a b", b=128)), reads=[pb], writes=[V])
                    for k in range(4):
                        sl = WS.get(tg * 16 + 12 + k)
                        pb = banks[4 + k % 2]
                        T.pre(PE, reads=[sl, un], writes=[pb])
                        for kc in range(8):
                            ins = MM(pb.t[:, :], lhsT=sl.t[:, kc * 128:(kc + 1) * 128], rhs=un.t[:, kc, :],
                                                   start=(kc == 0), stop=(kc == 7))
                        T.post(PE, ins, reads=[sl, un], writes=[pb])
                        T.op(ACT, lambda pb=pb, k=k: nc.scalar.copy(out=usT.t[:, k, c0:c0 + 512], in_=pb.t[:, :]),
                             reads=[pb], writes=[usT])
                    if tg < 2:
                        T.op(DVE, lambda: nc.vector.memset(mbias.t[:], 0.0), writes=[mbias])
                    else:
                        T.op(DVE, lambda: nc.vector.tensor_copy(out=ksum_b.t[:], in_=ksum.t[:]), reads=[ksum], writes=[ksum_b])
                        gb = banks[7]
                        T.pre(PE, reads=[Qm, ksum_b], writes=[gb])
                        for qt in range(4):
                            for h in range(8):
                                e, hp = h % 2, h // 2
                                ins = MM(gb.t[:, (qt * 8 + h) * 8:(qt * 8 + h) * 8 + 8],
                                                       lhsT=Qm.t[:, h, qt * 128:(qt + 1) * 128],
                                                       rhs=ksum_b.t[:, hp, :], start=True, stop=True)
                        T.post(PE, ins, reads=[Qm, ksum_b], writes=[gb])
                        T.op(DVE, lambda: nc.vector.memset(Gsb.t[:], -1e30), writes=[Gsb])
                        for half2 in range(2):
                            nb = 2 * tg + half2
                            T.op(DVE, lambda half2=half2, nb=nb: nc.vector.tensor_copy(
                                out=Gsb.t[:, 16 * half2:16 * half2 + 16, 0:nb],
                                in_=gb.t[:, 128 * half2:128 * half2 + 128].rearrange("p (a b) -> p a b", b=8)[:, :, 0:nb]),
                                reads=[gb], writes=[Gsb])
                        for i in range(32):
                            T.op(DVE, lambda i=i: nc.vector.max(out=M8.t[:, i, :], in_=Gsb.t[:, i, :]), reads=[Gsb], writes=[M8])
                        T.op(DVE, lambda: nc.vector.tensor_tensor(out=SEL.t[:], in0=Gsb.t[:], in1=M8.t[:, :, 2:3].to_broadcast([128, 32, 8]),
                                                                  op=ALU.is_ge), reads=[Gsb, M8], writes=[SEL])
                        T.op(DVE, lambda: nc.vector.tensor_scalar(out=mbias.t[:].rearrange("p a (h n) -> p (a h) n", n=8), in0=SEL.t[:],
                                                                  scalar1=1.0, scalar2=BIG, op0=ALU.subtract, op1=ALU.mult),
                             reads=[SEL], writes=[mbias])
                        for half2 in range(2):
                            nb = 2 * tg + half2
                            T.op(DVE, lambda half2=half2, nb=nb: nc.vector.memset(
                                mbias.t[:, 2 * half2:2 * half2 + 2, :].rearrange("p a (h n) -> p a h n", n=8)[:, :, :, nb:nb + 1], 0.0),
                                writes=[mbias])
                    mb = banks[7]
                    T.pre(PE, reads=[mbias, ident_f], writes=[mb])
                    for qt in range(4):
                        ins = MT(mb.t[0:64, qt * 128:(qt + 1) * 128], mbias.t[:, qt, :], ident_f.t[:, :])
                    T.post(PE, ins, reads=[mbias, ident_f], writes=[mb])
                    T.op(ACT, lambda: nc.scalar.copy(out=MTsb.t[:, :], in_=mb.t[0:64, :]), reads=[mb], writes=[MTsb])
                    for h in range(8):
                        T.dma(SP, Qx.t[0:8, h, :], MTsb.t[8 * h:8 * h + 8, :], Qx_ds2, reads=[MTsb], writes=[Qx])
                    nkt = 4 * tg + 4
                    for hp in range(4):
                        numP, denP = banks[2 + hp % 2], banks[4 + hp % 2]
                        for e in range(2):
                            h = 2 * hp + e
                            rows = slice(64 * e, 64 * e + 64)
                            pend = None
                            for kt in range(nkt):
                                j = kt - 4 * tg
                                ql = 128 * j if j > 0 else 0
                                sP = banks[kt % 2]
                                rd = [Kpk, Qm, Kx, Qx, ident_b, trineg]
                                T.pre(PE, reads=rd, writes=[sP])
                                MM(sP.t[:, ql:512], lhsT=Kpk.t[:, hp, kt * 128:(kt + 1) * 128],
                                                 rhs=Qm.t[:, h, ql:512], start=True, stop=False)
                                ins = MM(sP.t[:, ql:512], lhsT=Kx.t[:, kt * 128:(kt + 1) * 128],
                                                       rhs=Qx.t[:, h, ql:512], start=False, stop=(j < 0))
                                if j >= 0:
                                    ins = MM(sP.t[:, ql:ql + 128], lhsT=ident_b.t[:, :], rhs=trineg.t[:, :],
                                                           start=False, stop=True)
                                T.post(PE, ins, reads=rd, writes=[sP])
                                p = pT[kt % 3]
                                T.op(ACT, lambda p=p, sP=sP, ql=ql: nc.scalar.activation(out=p.t[:, ql:512], in_=sP.t[:, ql:512], func=AF.Exp),
                                     reads=[sP], writes=[p])
                                if pend is not None:
                                    pend()

                                def pend(p=p, kt=kt, ql=ql):
                                    T.pre(PE, reads=[p, V, ones_b], writes=[numP, denP])
                                    MM(numP.t[rows, ql:512], lhsT=V.t[:, kt, h * 64:(h + 1) * 64], rhs=p.t[:, ql:512],
                                                     start=(kt == 0), stop=(kt == nkt - 1))
                                    ins2 = MM(denP.t[rows, ql:512], lhsT=ones_b.t[:, 0:64], rhs=p.t[:, ql:512],
                                                            start=(kt == 0), stop=(kt == nkt - 1))
                                    T.post(PE, ins2, reads=[p, V, ones_b], writes=[numP, denP])
                            pend()
                        T.op(DVE, lambda: nc.vector.reciprocal(out=rden.t[:, :], in_=denP.t[:, :]), reads=[denP], writes=[rden])
                        T.op(DVE, lambda: nc.vector.tensor_tensor(out=att.t[:, hp, :], in0=numP.t[:, :], in1=rden.t[:, :],
                                                                  op=ALU.mult), reads=[numP, rden], writes=[att])
                    sq_and_sum(sqb, lambda i: att.t[:, i, :], [att], 4, banks[6])
                    rstd_from(banks[6], rt, rstd, 512.0)
                    for hp in range(4):
                        T.op(DVE, lambda hp=hp: nc.vector.scalar_tensor_tensor(
                            out=matt.t[:, hp, c0:c0 + 512], in0=att.t[:, hp, :], scalar=gcol(G_ATT + hp), in1=rstd.t[:, :],
                            op0=ALU.mult, op1=ALU.mult), reads=[att, rstd, gv], writes=[matt])
            T.barrier()
            if STOP >= 3:
             with contextlib.ExitStack() as ss_:
                CS = sbt(ss_, [128, 2, 16, 128], BF)
                BB = sbt(ss_, [128, 16, 2, 128], BF)
                CC = sbt(ss_, [128, 16, 3, 64], BF)
                DDm = sbt(ss_, [128, 512], BF, ds=True)
                wglu = sbt(ss_, [128, 2048], BF, ds=True)
                mag_s = sbt(ss_, [128, 16], F32)
                ECS = sbt(ss_, [128, 2, 16], F32)
                Hc = sbt(ss_, [128, 2, 16], F32)
                Hm = sbt(ss_, [128, 2, 16], F32)
                magT = sbt(ss_, [128, 16, 128], F32)
                cast_dma(DDm, DDm.t, dlay_d, 512, DDm.ds)
                cast_dma(wglu, wglu.t, wglu_d, 2048, wglu.ds)

                def sincos(stk, ang, n, sinb, cosb, tmpF, tmpI):
                    T.op(DVE, lambda: nc.vector.tensor_scalar(out=tmpI.t[:, :n], in0=ang.t[:, :n], scalar1=1.0 / (2 * PI), scalar2=None,
                                                              op0=ALU.mult), reads=[ang], writes=[tmpI])
                    T.op(DVE, lambda: nc.vector.tensor_copy(out=tmpF.t[:, :n], in_=tmpI.t[:, :n]), reads=[tmpI], writes=[tmpF])
                    T.op(DVE, lambda: nc.vector.scalar_tensor_tensor(out=tmpF.t[:, :n], in0=tmpF.t[:, :n], scalar=-2 * PI, in1=ang.t[:, :n],
                                                                     op0=ALU.mult, op1=ALU.add), reads=[tmpF, ang], writes=[tmpF])
                    T.op(DVE, lambda: nc.vector.tensor_scalar(out=tmpF.t[:, :n], in0=tmpF.t[:, :n], scalar1=-PI, scalar2=PI,
                                                              op0=ALU.max, op1=ALU.min), reads=[tmpF], writes=[tmpF])
                    T.op(ACT, lambda: nc.scalar.activation(out=sinb.t[:, :n], in_=tmpF.t[:, :n], func=AF.Sin), reads=[tmpF], writes=[sinb])
                    T.op(ACT, lambda: nc.scalar.activation(out=tmpF.t[:, :n], in_=tmpF.t[:, :n], func=AF.Abs), reads=[tmpF, sinb], writes=[tmpF])
                    T.op(DVE, lambda: nc.vector.tensor_scalar(out=tmpF.t[:, :n], in0=tmpF.t[:, :n], scalar1=-1.0, scalar2=PI / 2,
                                                              op0=ALU.mult, op1=ALU.add), reads=[tmpF], writes=[tmpF])
                    T.op(ACT, lambda: nc.scalar.activation(out=cosb.t[:, :n], in_=tmpF.t[:, :n], func=AF.Sin), reads=[tmpF], writes=[cosb])

                TT = lambda o, a, b, op, rd, wr: T.op(DVE, lambda: nc.vector.tensor_tensor(out=o, in0=a, in1=b, op=op), reads=rd, writes=wr)
                with contextlib.ExitStack() as st:
                    lamst = sbt(st, [128, 48], F32, ds=True)
                    iota = sbt(st, [128, 128], F32, ds=True)
                    clay = sbt(st, [128, 2 * 1024], F32, ds=True)
                    T.dma(SP, lamst.t[:], lamst_d, lamst.ds, writes=[lamst])
                    T.dma(SP, iota.t[:], cst_d[:, 256:384], iota.ds, writes=[iota])
                    T.dma(SP, clay.t[:], clay_d, clay.ds, writes=[clay])
                    W = [sbt(st, [128, 2048], F32) for _ in range(3)]
                    WI = sbt(st, [128, 2048], I32)
                    sS = sbt(st, [128, 2048], F32)
                    sC = sbt(st, [128, 2048], F32)
                    dts, th_s, e1 = W[0], W[1], W[2]
                    T.op(ACT, lambda: nc.scalar.activation(out=dts.t[:, :16], in_=lamst.t[:, 32:48], func=AF.Exp), reads=[lamst], writes=[dts])
                    TT(e1.t[:, :16], lamst.t[:, 0:16], dts.t[:, :16], ALU.mult, [lamst, dts], [e1])
                    T.op(ACT, lambda: nc.scalar.activation(out=mag_s.t[:, :], in_=e1.t[:, :16], func=AF.Exp), reads=[e1], writes=[mag_s])
                    TT(th_s.t[:, :16], lamst.t[:, 16:32], dts.t[:, :16], ALU.mult, [lamst, dts], [th_s])
                    for j in range(16):
                        T.op(DVE, lambda j=j: nc.vector.tensor_scalar(out=magT.t[:, j, :], in0=iota.t[:, :], scalar1=0.0,
                                                                      scalar2=mag_s.t[:, j:j + 1], op0=ALU.mult, op1=ALU.add),
                             reads=[iota, mag_s], writes=[magT])
                    T.op(DVE, lambda: nc.vector.memset(magT.t[:, :, 0:1], 0.0), writes=[magT])
                    PH = W[2]
                    for j in range(16):
                        T.op(DVE, lambda j=j: nc.vector.tensor_scalar(out=PH.t[:, j * 128:(j + 1) * 128], in0=iota.t[:, :],
                                                                      scalar1=th_s.t[:, j:j + 1], scalar2=None, op0=ALU.mult),
                             reads=[iota, th_s], writes=[PH])
                    sincos(st, PH, 2048, sS, sC, W[0], WI)
                    j3 = lambda ap: ap.rearrange("p (j t) -> p j t", t=128)
                    T.op(DVE, lambda: nc.vector.tensor_copy(out=CS.t[:, 0, :, :], in_=j3(sC.t[:, :])), reads=[sC], writes=[CS])
                    T.op(DVE, lambda: nc.vector.tensor_copy(out=CS.t[:, 1, :, :], in_=j3(sS.t[:, :])), reads=[sS], writes=[CS])
                    T.op(DVE, lambda: nc.vector.tensor_copy(out=ECS.t[:, 0, :], in_=j3(sC.t[:, :])[:, :, 127]), reads=[sC], writes=[ECS])
                    T.op(DVE, lambda: nc.vector.tensor_copy(out=ECS.t[:, 1, :], in_=j3(sS.t[:, :])[:, :, 127]), reads=[sS], writes=[ECS])
                    c3 = lambda ap: ap.rearrange("p (j c) -> p j c", c=64)
                    T.op(DVE, lambda: nc.vector.tensor_copy(out=CC.t[:, :, 0, :], in_=c3(clay.t[:, 0:1024])), reads=[clay], writes=[CC])
                    T.op(DVE, lambda: nc.vector.tensor_scalar(out=CC.t[:, :, 1, :], in0=c3(clay.t[:, 0:1024]), scalar1=-1.0, scalar2=None,
                                                              op0=ALU.mult), reads=[clay], writes=[CC])
                    T.op(DVE, lambda: nc.vector.tensor_scalar(out=CC.t[:, :, 2, :], in0=c3(clay.t[:, 1024:2048]), scalar1=-1.0, scalar2=None,
                                                              op0=ALU.mult), reads=[clay], writes=[CC])
                T.barrier()
                with contextlib.ExitStack() as st:
                    NH = 1024
                    lamh = sbt(st, [128, 3 * NH], F32, ds=True)
                    blh = sbt(st, [128, 2 * NH], F32, ds=True)
                    W = [sbt(st, [128, NH], F32) for _ in range(7)]
                    WI = sbt(st, [128, NH], I32)
                    sS = sbt(st, [128, NH], F32)
                    sC = sbt(st, [128, NH], F32)
                    for hf in range(2):
                        for q in range(3):
                            T.dma(SP, lamh.t[:, q * NH:(q + 1) * NH], lambc_d[:, q * 2048 + hf * NH:q * 2048 + (hf + 1) * NH], lamh.ds, writes=[lamh])
                        for q in range(2):
                            T.dma(SP, blh.t[:, q * NH:(q + 1) * NH], blay_d[:, q * 2048 + hf * NH:q * 2048 + (hf + 1) * NH], blh.ds, writes=[blh])
                        lr, li, ldt = lamh.t[:, 0:NH], lamh.t[:, NH:2 * NH], lamh.t[:, 2 * NH:3 * NH]
                        dtb, thb, magb = W[0], W[1], W[2]
                        T.op(ACT, lambda: nc.scalar.activation(out=dtb.t[:, :], in_=ldt, func=AF.Exp), reads=[lamh], writes=[dtb])
                        TT(magb.t[:, :], lr, dtb.t[:, :], ALU.mult, [lamh, dtb], [magb])
                        T.op(ACT, lambda: nc.scalar.activation(out=magb.t[:, :], in_=magb.t[:, :], func=AF.Exp), reads=[magb], writes=[magb])
                        TT(thb.t[:, :], li, dtb.t[:, :], ALU.mult, [lamh, dtb], [thb])
                        sincos(st, thb, NH, sS, sC, W[4], WI)
                        are, aim, den, fre, fim, tmp = W[3], W[4], W[5], W[6], W[0], W[1]
                        TT(are.t[:, :], magb.t[:, :], sC.t[:, :], ALU.mult, [magb, sC], [are])
                        TT(aim.t[:, :], magb.t[:, :], sS.t[:, :], ALU.mult, [magb, sS], [aim])
                        T.op(DVE, lambda: nc.vector.tensor_scalar(out=are.t[:, :], in0=are.t[:, :], scalar1=-1.0, scalar2=None, op0=ALU.add),
                             reads=[are], writes=[are])
                        TT(den.t[:, :], lr, lr, ALU.mult, [lamh], [den])
                        TT(tmp.t[:, :], li, li, ALU.mult, [lamh], [tmp])
                        TT(den.t[:, :], den.t[:, :], tmp.t[:, :], ALU.add, [den, tmp], [den])
                        T.op(DVE, lambda: nc.vector.reciprocal(out=den.t[:, :], in_=den.t[:, :]), reads=[den], writes=[den])
                        TT(fre.t[:, :], are.t[:, :], lr, ALU.mult, [are, lamh], [fre])
                        TT(tmp.t[:, :], aim.t[:, :], li, ALU.mult, [aim, lamh], [tmp])
                        TT(fre.t[:, :], fre.t[:, :], tmp.t[:, :], ALU.add, [fre, tmp], [fre])
                        TT(fre.t[:, :], fre.t[:, :], den.t[:, :], ALU.mult, [fre, den], [fre])
                        TT(fim.t[:, :], aim.t[:, :], lr, ALU.mult, [aim, lamh], [fim])
                        TT(tmp.t[:, :], are.t[:, :], li, ALU.mult, [are, lamh], [tmp])
                        TT(fim.t[:, :], fim.t[:, :], tmp.t[:, :], ALU.subtract, [fim, tmp], [fim])
                        TT(fim.t[:, :], fim.t[:, :], den.t[:, :], ALU.mult, [fim, den], [fim])
                        bre, bim = blh.t[:, 0:NH], blh.t[:, NH:2 * NH]
                        t2, t3 = W[2], W[3]
                        v3 = lambda ap: ap.rearrange("p (j c) -> p j c", c=128)
                        js = slice(8 * hf, 8 * hf + 8)
                        TT(t2.t[:, :], fre.t[:, :], bre, ALU.mult, [fre, blh], [t2])
                        TT(t3.t[:, :], fim.t[:, :], bim, ALU.mult, [fim, blh], [t3])
                        TT(BB.t[:, js, 0, :], v3(t2.t[:, :]), v3(t3.t[:, :]), ALU.subtract, [t2, t3], [BB])
                        TT(t2.t[:, :], fre.t[:, :], bim, ALU.mult, [fre, blh], [t2])
                        TT(t3.t[:, :], fim.t[:, :], bre, ALU.mult, [fim, blh], [t3])
                        TT(BB.t[:, js, 1, :], v3(t2.t[:, :]), v3(t3.t[:, :]), ALU.add, [t2, t3], [BB])
                T.barrier()
                TTq = sbt(ss_, [128, 4, 4, 128], BF)
                XTq = sbt(ss_, [128, 2, 4, 128], BF)
                Gqs = [sbt(ss_, [128, 2, 4, 128], BF) for _ in range(2)]
                Pq = [sbt(ss_, [128, 4, 4, 128], BF) for _ in range(2)]
                ctmp = sbt(ss_, [128, 4, 4], F32)
                ygf = sbt(ss_, [128, 4, 512], F32)
                ygb = sbt(ss_, [128, 4, 512], BF)
                sig = sbt(ss_, [128, 512], F32)
                mssm = sbt(ss_, [128, 4, 512], BF)
                fT = sbt(ss_, [128, 8, 512], F32)
                sqb = [sbt(ss_, [128, 512], BF) for _ in range(2)]
                rt = sbt(ss_, [128, 512], F32)
                rstd = sbt(ss_, [128, 512], F32)
                WO = Stream(ss_, [128, 1024], 3, chunk=1024)
                for tg in range(4):
                    WO.start([wout_d[dt] for dt in range(8)])
                it = 0
                for tg in range(4):
                    c0 = tg * 512
                    for cc in range(4):
                        c = 4 * tg + cc
                        t0 = c0 + 128 * cc
                        for k in range(4):
                            XA, XB = banks[2 * (it % 2)], banks[2 * (it % 2) + 1]
                            P = Pq[it % 2]
                            Gq = Gqs[it % 2]
                            it += 1
                            T.pre(PE, reads=[BB, usT], writes=[XA, XB])
                            for jl in range(4):
                                j, e = 4 * k + jl, jl // 2
                                rows = slice(64 * e, 64 * e + 64)
                                MM(XA.t[:, jl * 128:(jl + 1) * 128], lhsT=BB.t[:, j, 0, :], rhs=usT.t[:, k, t0:t0 + 128],
                                                 start=True, stop=True)
                                ins = MM(XB.t[:, jl * 128:(jl + 1) * 128], lhsT=BB.t[:, j, 1, :], rhs=usT.t[:, k, t0:t0 + 128],
                                                       start=True, stop=True)
                            T.post(PE, ins, reads=[BB, usT], writes=[XA, XB])
                            q4 = slice(4 * k, 4 * k + 4)
                            xa = XA.t[:, :].rearrange("p (a b) -> p a b", b=128)
                            xb = XB.t[:, :].rearrange("p (a b) -> p a b", b=128)
                            cosq, sinq = CS.t[:, 0, q4, :], CS.t[:, 1, q4, :]
                            T.op(DVE, lambda: nc.vector.tensor_tensor(out=TTq.t[:, 0], in0=xa, in1=cosq, op=ALU.mult), reads=[XA, CS], writes=[TTq])
                            T.op(DVE, lambda: nc.vector.tensor_tensor(out=TTq.t[:, 1], in0=xb, in1=sinq, op=ALU.mult), reads=[XB, CS], writes=[TTq])
                            T.op(DVE, lambda: nc.vector.tensor_tensor(out=TTq.t[:, 2], in0=xb, in1=cosq, op=ALU.mult), reads=[XB, CS], writes=[TTq])
                            T.op(DVE, lambda: nc.vector.tensor_tensor(out=TTq.t[:, 3], in0=xa, in1=sinq, op=ALU.mult), reads=[XA, CS], writes=[TTq])
                            T.op(DVE, lambda: nc.vector.tensor_tensor(out=XTq.t[:, 0], in0=TTq.t[:, 0], in1=TTq.t[:, 1], op=ALU.add),
                                 reads=[TTq], writes=[XTq])
                            T.op(DVE, lambda: nc.vector.tensor_tensor(out=XTq.t[:, 1], in0=TTq.t[:, 2], in1=TTq.t[:, 3], op=ALU.subtract),
                                 reads=[TTq], writes=[XTq])
                            if c > 0:
                                T.op(DVE, lambda: nc.vector.tensor_tensor(out=XTq.t[:, :, :, 0], in0=XTq.t[:, :, :, 0], in1=Hm.t[:, :, q4], op=ALU.add),
                                     reads=[XTq, Hm], writes=[XTq])
                            for r in range(2):
                                T.op(DVE, lambda r=r: nc.vector.tensor_tensor_scan(
                                    out=Gq.t[:, r, :, :].rearrange("p a b -> p (a b)"), data0=magT.t[:, q4, :].rearrange("p a b -> p (a b)"),
                                    data1=XTq.t[:, r, :, :].rearrange("p a b -> p (a b)"), initial=0.0, op0=ALU.mult, op1=ALU.add),
                                    reads=[XTq, magT], writes=[Gq])
                            T.op(POOL, lambda: nc.gpsimd.tensor_tensor(out=P.t[:, 0:2], in0=Gq.t[:, 0:2], in1=CS.t[:, 0:2, q4, :], op=ALU.mult),
                                 reads=[Gq, CS], writes=[P])
                            T.op(POOL, lambda: nc.gpsimd.tensor_tensor(out=P.t[:, 2], in0=Gq.t[:, 1], in1=cosq, op=ALU.mult), reads=[Gq, CS], writes=[P])
                            T.op(POOL, lambda: nc.gpsimd.tensor_tensor(out=P.t[:, 3], in0=Gq.t[:, 0], in1=sinq, op=ALU.mult), reads=[Gq, CS], writes=[P])
                            T.op(POOL, lambda: nc.gpsimd.tensor_tensor(out=Hc.t[:, 0, q4], in0=P.t[:, 0, :, 127], in1=P.t[:, 1, :, 127], op=ALU.subtract),
                                 reads=[P], writes=[Hc])
                            T.op(POOL, lambda: nc.gpsimd.tensor_tensor(out=Hc.t[:, 1, q4], in0=P.t[:, 2, :, 127], in1=P.t[:, 3, :, 127], op=ALU.add),
                                 reads=[P], writes=[Hc])
                            for r in range(2):
                                T.op(POOL, lambda r=r: nc.gpsimd.tensor_tensor(out=Hm.t[:, r, q4], in0=Hc.t[:, r, q4], in1=mag_s.t[:, q4], op=ALU.mult),
                                     reads=[Hc, mag_s], writes=[Hm])
                            Y = banks[4 + k]
                            rd = [CC, P, DDm, usT]
                            T.pre(PE, reads=rd, writes=[Y])
                            for e in range(2):
                                rows = slice(64 * e, 64 * e + 64)
                                yo = Y.t[rows, cc * 128:(cc + 1) * 128]
                                first = True
                                for jl in (2 * e, 2 * e + 1):
                                    j = 4 * k + jl
                                    for (ci, pi_) in ((0, 0), (1, 1), (2, 2), (2, 3)):
                                        MM(yo, lhsT=CC.t[:, j, ci, :], rhs=P.t[:, pi_, jl, :], start=first, stop=False)
                                        first = False
                                ins = MM(yo, lhsT=DDm.t[:, k * 128 + 64 * e:k * 128 + 64 * e + 64],
                                                       rhs=usT.t[:, k, t0:t0 + 128], start=False, stop=True)
                            T.post(PE, ins, reads=rd, writes=[Y])
                    for k in range(4):
                        Y = banks[4 + k]
                        T.op(ACT, lambda k=k, Y=Y: nc.scalar.activation(out=ygf.t[:, k, :], in_=Y.t[:, :], func=AF.Gelu_apprx_tanh),
                             reads=[Y], writes=[ygf])
                        T.op(DVE, lambda k=k: nc.vector.tensor_copy(out=ygb.t[:, k, :], in_=ygf.t[:, k, :]), reads=[ygf], writes=[ygb])
                    for ko in range(4):
                        GP = banks[ko % 2]
                        T.pre(PE, reads=[wglu, ygb], writes=[GP])
                        for ki in range(4):
                            ins = MM(GP.t[:, :], lhsT=wglu.t[:, ki * 512 + ko * 128:ki * 512 + (ko + 1) * 128], rhs=ygb.t[:, ki, :],
                                                   start=(ki == 0), stop=(ki == 3))
                        T.post(PE, ins, reads=[wglu, ygb], writes=[GP])
                        T.op(ACT, lambda GP=GP, ko=ko: nc.scalar.activation(out=sig.t[:, :], in_=GP.t[:, :], func=AF.Sigmoid,
                                                                          bias=gcol(G_BGLU + ko)), reads=[GP, gv], writes=[sig])
                        T.op(DVE, lambda ko=ko: nc.vector.tensor_tensor(out=ygf.t[:, ko, :], in0=ygf.t[:, ko, :], in1=sig.t[:, :], op=ALU.mult),
                             reads=[ygf, sig], writes=[ygf])
                    sq_and_sum(sqb, lambda i: ygf.t[:, i, :], [ygf], 4, banks[2])
                    rstd_from(banks[2], rt, rstd, 512.0)
                    for ko in range(4):
                        T.op(DVE, lambda ko=ko: nc.vector.scalar_tensor_tensor(
                            out=mssm.t[:, ko, :], in0=ygf.t[:, ko, :], scalar=gcol(G_SSM + ko), in1=rstd.t[:, :],
                            op0=ALU.mult, op1=ALU.mult), reads=[ygf, rstd, gv], writes=[mssm])
                    down_project(WO, tg * 8, 8,
                                 lambda m, ti, c0=c0: (matt.t[:, m, c0:c0 + 512] if m < 4 else mssm.t[:, m - 4, :]), [matt, mssm],
                                 [fT], [c0], G_MIXPOST, 1.0, [banks[0], banks[1]], [banks[3]], sqb, rt, rstd)
            T.barrier()

        if STOP >= 4:
            ffn(G_F2PRE, G_F2POST, w2gu_d, w2d_d)
        T.barrier()
        T.live = True
        T.cut = 10 ** 9
        T.barrier()
        od = T.newds()
        T.dma(SP, out_d, xT.t[:], od, reads=[xT])
        SP.e.wait_ge(od.sem, od.cnt)
    return nc


def _shared_layouts(inp):
    f = np.float32
    A = lambda k: np.ascontiguousarray(np.asarray(inp[k], dtype=f)[0])
    o = {}
    gv = np.zeros((128, 64), f)

    def put(c0, v):
        n = v.shape[0] // 128
        gv[:, c0:c0 + n] = v.reshape(n, 128).T
    put(G_F1PRE, A("ffn1_pre_g")); put(G_F1POST, A("ffn1_post_g")); put(G_MIXPRE, A("mix_pre_g"))
    put(G_ATT, A("attn_out_g")); put(G_SSM, A("ssm_out_g")); put(G_MIXPOST, A("mix_post_g"))
    put(G_F2PRE, A("ffn2_pre_g")); put(G_F2POST, A("ffn2_post_g")); put(G_BGLU, A("b_glu"))
    o["gv"] = gv

    def gu(wg, wu):
        a = wg.reshape(8, 128, NFT, 128).transpose(2, 1, 0, 3)
        b = wu.reshape(8, 128, NFT, 128).transpose(2, 1, 0, 3)
        return np.ascontiguousarray(np.concatenate([a, b], axis=2).reshape(NFT, 128, 2048))

    def dn(wd):
        return np.ascontiguousarray(wd.reshape(NFT, 128, 8, 128).transpose(2, 1, 0, 3).reshape(8, 128, DFF))
    o["w1gu"] = gu(A("ffn1_w_gate"), A("ffn1_w_up")); o["w1d"] = dn(A("ffn1_w_down"))
    o["w2gu"] = gu(A("ffn2_w_gate"), A("ffn2_w_up")); o["w2d"] = dn(A("ffn2_w_down"))
    win = A("w_in")
    order = [512 + 128 * i for i in range(4)] + [128 * i for i in range(4)] + [1024 + 128 * i for i in range(8)]
    o["win"] = np.ascontiguousarray(np.stack([win[:, c:c + 128].reshape(8, 128, 128).transpose(1, 0, 2).reshape(128, 1024) for c in order]))
    o["wglu"] = np.ascontiguousarray(A("w_glu").reshape(4, 128, 512).transpose(1, 0, 2).reshape(128, 2048))
    wo = A("w_out")
    o["wout"] = np.ascontiguousarray(wo.reshape(8, 128, 8, 128).transpose(2, 1, 0, 3).reshape(8, 128, 1024))
    lr, li, ld = A("lam_re"), A("lam_im"), A("log_dt")
    st = lambda a: a.reshape(16, 2, 64).transpose(1, 2, 0).reshape(128, 16)
    ldf = np.repeat(ld[:, None], 64, axis=1)
    o["lamst"] = np.ascontiguousarray(np.concatenate([st(lr), st(li), st(ldf)], axis=1))
    bc = np.concatenate([lr.reshape(-1), li.reshape(-1), ldf.reshape(-1)])
    o["lambc"] = np.ascontiguousarray(np.broadcast_to(bc[None, :], (128, 6144)))
    bre, bim = A("b_re"), A("b_im")
    bl = np.zeros((2, 128, 16, 128), f)
    cre, cim = A("c_re"), A("c_im")
    cl = np.zeros((2, 128, 16, 64), f)
    for j in range(16):
        q = j % 4
        for g2 in range(2):
            g = 2 * j + g2
            r0 = 32 * q + 16 * g2
            bl[0, r0:r0 + 16, j, 64 * g2:64 * g2 + 64] = bre[g].T
            bl[1, r0:r0 + 16, j, 64 * g2:64 * g2 + 64] = bim[g].T
            cc0 = 32 * (j % 2) + 16 * g2
            cl[0, 64 * g2:64 * g2 + 64, j, cc0:cc0 + 16] = cre[g].T
            cl[1, 64 * g2:64 * g2 + 64, j, cc0:cc0 + 16] = cim[g].T
    o["blay"] = np.ascontiguousarray(bl.transpose(1, 0, 2, 3).reshape(128, 4096))
    o["clay"] = np.ascontiguousarray(cl.transpose(1, 0, 2, 3).reshape(128, 2048))
    dsk = A("d_skip").reshape(512)
    dl = np.zeros((128, 4, 128), f)
    for k in range(4):
        dl[np.arange(128), k, np.arange(128)] = dsk[k * 128:(k + 1) * 128]
    o["dlay"] = dl.reshape(128, 512)
    pos = np.arange(S)
    ka = np.zeros((12, S), f)
    ka[pos // 256, pos] = 1.0
    ka[8] = 1.0; ka[9] = 1.0; ka[10] = 128.0 * (pos // 128); ka[11] = pos % 128
    o["kaugc"] = ka
    qa = np.zeros((4, 8, S), f)
    for h in range(8):
        sl = 2.0 ** (-(h + 1))
        qa[0, h] = -sl * 128.0 * (pos // 128); qa[1, h] = -sl * (pos % 128); qa[2, h] = sl; qa[3, h] = sl
    o["qaugc"] = qa
    cst = np.zeros((128, 384), f)
    cst[:, 0:128] = np.eye(128, dtype=f)
    kk, qq = np.meshgrid(np.arange(128), np.arange(128), indexing="ij")
    cst[:, 128:256] = np.where(kk > qq, -BIG, 0.0)
    cst[:, 256:384] = np.arange(1, 129, dtype=f)[None, :]
    o["cst"] = cst
    return o


_NC = None


def kernel(**inputs):
    global _NC
    x = np.asarray(inputs["x"], dtype=np.float32)
    nb = x.shape[0]
    sh = _shared_layouts(inputs)
    in_maps = []
    for b in range(nb):
        m = dict(sh)
        m["xT"] = np.ascontiguousarray(x[b].T.reshape(8, 128, S).transpose(1, 0, 2))
        in_maps.append(m)
    if _NC is None:
        _NC = build()
    res = run_bass_kernel_spmd(_NC, in_maps, core_ids=list(range(nb)))
    out = np.empty_like(x)
    for b in range(nb):
        yT = np.asarray(res.results[b]["yT"], dtype=np.float32)
        out[b] = yT.transpose(1, 0, 2).reshape(1024, S).T
    return out
```

```python
import contextlib
import math
import numpy as np
import concourse.bass as bass
import concourse.mybir as mybir
from concourse.bass_utils import run_bass_kernel_spmd

F32 = mybir.dt.float32
BF = mybir.dt.bfloat16
I32 = mybir.dt.int32
AF = mybir.ActivationFunctionType
ALU = mybir.AluOpType
AX = mybir.AxisListType
PI = math.pi
S = 2048
DFF = 2816
NFT = 22
EPS = 1e-6
BIG = 30000.0

G_F1PRE, G_F1POST, G_MIXPRE, G_ATT, G_SSM, G_MIXPOST, G_F2PRE, G_F2POST, G_BGLU = 0, 8, 16, 24, 28, 32, 40, 48, 56


class Eng:
    def __init__(self, nc, e, name, es):
        self.e, self.name = e, name
        self.sem = es.enter_context(nc.semaphore("s_" + name))
        self.cnt = 0
        self.seen = {}


class DS:
    def __init__(self, nc, name, es):
        self.sem = es.enter_context(nc.semaphore(name))
        self.cnt = 0
        self.name = name


class Buf:
    def __init__(self, t, ds=None, dj=False, psum=False):
        self.psum = psum
        self.dj = dj
        self.t = t
        self.w = None
        self.r = {}
        self.ds = ds


class Trk:
    def __init__(self, nc, es):
        self.nc = nc
        self.es = es
        self.PE = Eng(nc, nc.tensor, "pe", es)
        self.ACT = Eng(nc, nc.scalar, "act", es)
        self.DVE = Eng(nc, nc.vector, "dve", es)
        self.POOL = Eng(nc, nc.gpsimd, "pool", es)
        self.SP = Eng(nc, nc.sync, "sp", es)
        self.engs = [self.PE, self.ACT, self.DVE, self.POOL, self.SP]
        self.dss = []
        self.nds = 0
        self.n = 0
        self.live = True
        self.cut = CUT

    def newds(self):
        self.nds += 1
        d = DS(self.nc, "d%d" % self.nds, self.es)
        self.dss.append(d)
        return d

    def wait(self, E, tok):
        src, val = tok
        if src is E and E is self.PE:
            return
        k = id(src)
        if E.seen.get(k, 0) >= val:
            return
        E.e.wait_ge(src.sem, val)
        E.seen[k] = val

    def pre(self, E, reads=(), writes=()):
        if self.n >= self.cut:
            self.live = False
        if not self.live:
            return
        for b in reads:
            if b.w is not None:
                self.wait(E, b.w)
            if b.psum:
                for src, val in list(b.r.values()):
                    if src is not E:
                        self.wait(E, (src, val))
        for b in writes:
            if b.w is not None and (b.w[0] is not E or not b.dj):
                self.wait(E, b.w)
            for src, val in list(b.r.values()):
                self.wait(E, (src, val))

    def _reg(self, tok, reads, writes):
        for b in reads:
            b.r[id(tok[0])] = tok
        for b in writes:
            b.w = tok
            b.r = {}

    def post(self, E, inst, reads=(), writes=()):
        if not self.live:
            return
        self.n += 1
        E.cnt += 1
        inst.then_inc(E.sem, 1)
        self._reg((E, E.cnt), reads, writes)

    def op(self, E, fn, reads=(), writes=()):
        self.pre(E, reads, writes)
        if not self.live:
            return
        inst = fn()
        self.post(E, inst, reads, writes)

    def dma(self, Q, out, in_, ds, reads=(), writes=(), **kw):
        self.pre(Q, reads, writes)
        if not self.live:
            return
        self.n += 1
        inst = Q.e.dma_start(out=out, in_=in_, **kw)
        ds.cnt += 16
        inst.then_inc(ds.sem, 16)
        self._reg((ds, ds.cnt), reads, writes)

    def mark(self, name):
        MARKS.append((name, self.n))

    def barrier(self):
        if not self.live:
            return
        for E in self.engs:
            for Fe in self.engs:
                if Fe is not E and Fe.cnt > 0:
                    self.wait(E, (Fe, Fe.cnt))
            for d in self.dss:
                if d.cnt > 0:
                    self.wait(E, (d, d.cnt))


import os
STOP = int(os.environ.get('KSTOP', '99'))
CUT = int(os.environ.get('KCUT', str(10 ** 9)))
MARKS = []


def build():
    nc = bass.Bass("TRN2", target_bir_lowering=False)

    def D(name, shape, kind="ExternalInput", dt=F32):
        return nc.dram_tensor(name, shape, dt, kind=kind).ap()

    xT_d = D("xT", [128, 8, S])
    gv_d = D("gv", [128, 64])
    w1gu_d = D("w1gu", [NFT, 128, 2048])
    w1d_d = D("w1d", [8, 128, DFF])
    w2gu_d = D("w2gu", [NFT, 128, 2048])
    w2d_d = D("w2d", [8, 128, DFF])
    win_d = D("win", [16, 128, 1024])
    wglu_d = D("wglu", [128, 2048])
    wout_d = D("wout", [8, 128, 1024])
    lamst_d = D("lamst", [128, 48])
    lambc_d = D("lambc", [128, 3 * 2048])
    blay_d = D("blay", [128, 2 * 2048])
    clay_d = D("clay", [128, 2 * 1024])
    dlay_d = D("dlay", [128, 512])
    kaug_d = D("kaugc", [12, S])
    qaug_d = D("qaugc", [4, 8, S])
    cst_d = D("cst", [128, 3 * 128])
    out_d = D("yT", [128, 8, S], kind="ExternalOutput")

    es = contextlib.ExitStack()
    with es:
        T = Trk(nc, es)
        PE, ACT, DVE, POOL, SP = T.PE, T.ACT, T.DVE, T.POOL, T.SP
        cnt = [0]

        def sbt(stack, shape, dt, ds=False, dj=False):
            cnt[0] += 1
            t = stack.enter_context(nc.sbuf_tensor("t%d" % cnt[0], shape, dt))
            return Buf(t, T.newds() if ds else None, dj)

        xT = sbt(es, [128, 8, S], F32, ds=True)
        gv = sbt(es, [128, 64], F32, ds=True)
        ident_f = sbt(es, [128, 128], F32, ds=True)
        ident_b = sbt(es, [128, 128], BF, ds=True)
        trineg = sbt(es, [128, 128], BF, ds=True)
        ones_b = sbt(es, [128, 128], BF)
        banks = []
        for i in range(8):
            banks.append(Buf(es.enter_context(nc.psum_tensor("bank%d" % i, [128, 512], F32)), psum=True))

        T.dma(SP, xT.t[:], xT_d, xT.ds, writes=[xT])
        T.dma(SP, gv.t[:], gv_d, gv.ds, writes=[gv])
        T.dma(SP, ident_f.t[:], cst_d[:, 0:128], ident_f.ds, writes=[ident_f])
        T.dma(POOL, ident_b.t[:], cst_d[:, 0:128], ident_b.ds, writes=[ident_b])
        T.dma(POOL, trineg.t[:], cst_d[:, 128:256], trineg.ds, writes=[trineg])
        T.op(DVE, lambda: nc.vector.memset(ones_b.t[:], 1.0), writes=[ones_b])

        def gcol(c):
            return gv.t[:, c:c + 1]

        def MM(*a, **k):
            return nc.tensor.matmul(*a, **k) if T.live else None

        def MT(*a, **k):
            return nc.tensor.transpose(*a, **k) if T.live else None

        def cast_dma(buf, out_ap, in_ap, ncols, ds, chunk=512):
            nchunk = (ncols + chunk - 1) // chunk
            w = (ncols + nchunk - 1) // nchunk
            for a in range(0, ncols, w):
                b = min(ncols, a + w)
                T.dma(POOL, out_ap[:, a:b], in_ap[:, a:b], ds, writes=[buf])

        class Stream:
            def __init__(self, stack, shape, nslots, chunk=512):
                self.chunk = chunk
                self.slots = [sbt(stack, shape, BF, ds=True) for _ in range(nslots)]
                self.n = nslots
                self.jobs = []
                self.issued = 0
                self.base = 0

            def start(self, jobs):
                self.jobs = self.jobs + list(jobs)

            def _issue(self, k):
                sl = self.slots[k % self.n]
                cast_dma(sl, sl.t, self.jobs[k], sl.t.shape[1], sl.ds, chunk=self.chunk)

            def get(self, i):
                hi = min(i + self.n - 1, len(self.jobs) - 1)
                while self.issued <= hi:
                    self._issue(self.issued)
                    self.issued += 1
                return self.slots[i % self.n]

        def sq_and_sum(stack_bufs, src_ap_fn, src_bufs, ntiles, ssb, ncols=512):
            sqb = stack_bufs
            for i in range(ntiles):
                q = sqb[i % len(sqb)]
                T.op(ACT, lambda q=q, i=i: nc.scalar.activation(out=q.t[:, :ncols], in_=src_ap_fn(i), func=AF.Square),
                     reads=src_bufs, writes=[q])
                T.op(PE, lambda q=q, i=i: MM(ssb.t[:, :ncols], lhsT=ones_b.t[:], rhs=q.t[:, :ncols],
                                                          start=(i == 0), stop=(i == ntiles - 1)),
                     reads=[q, ones_b], writes=[ssb])

        def rstd_from(ssb, rt, rstd, dim, ncols=512):
            T.op(DVE, lambda: nc.vector.tensor_scalar(out=rt.t[:, :ncols], in0=ssb.t[:, :ncols], scalar1=1.0 / dim,
                                                      scalar2=EPS, op0=ALU.mult, op1=ALU.add),
                 reads=[ssb], writes=[rt])
            T.op(ACT, lambda: nc.scalar.activation(out=rt.t[:, :ncols], in_=rt.t[:, :ncols], func=AF.Sqrt),
                 reads=[rt], writes=[rt])
            T.op(DVE, lambda: nc.vector.reciprocal(out=rstd.t[:, :ncols], in_=rt.t[:, :ncols]),
                 reads=[rt], writes=[rstd])

        def prenorm(c0, g0, outb, ocol0, sqb, ssb, rt, rstd):
            sq_and_sum(sqb, lambda i: xT.t[:, i, c0:c0 + 512], [xT], 8, ssb)
            rstd_from(ssb, rt, rstd, 1024.0)
            for kc in range(8):
                T.op(DVE, lambda kc=kc: nc.vector.scalar_tensor_tensor(
                    out=outb.t[:, kc, ocol0:ocol0 + 512], in0=xT.t[:, kc, c0:c0 + 512], scalar=gcol(g0 + kc),
                    in1=rstd.t[:, :], op0=ALU.mult, op1=ALU.mult), reads=[xT, rstd, gv], writes=[outb])

        def down_project(stream, job0, nk, rhs_fn, rhs_bufs, fTs, c0s, gpost, half_scale, ybanks, ssbs, sqb, rt, rstd):
            pend = None
            step = 0
            ng = len(c0s)
            for dt in range(8):
                sl = stream.get(job0 + dt)
                for ti in range(ng):
                    yb = ybanks[step % 2]
                    q = sqb[step % 2]
                    step += 1
                    fT, ssb = fTs[ti], ssbs[ti]
                    T.pre(PE, reads=[sl] + rhs_bufs, writes=[yb])
                    for k in range(nk):
                        ins = MM(yb.t[:, :], lhsT=sl.t[:, k * 128:(k + 1) * 128], rhs=rhs_fn(k, ti),
                                               start=(k == 0), stop=(k == nk - 1))
                    T.post(PE, ins, reads=[sl] + rhs_bufs, writes=[yb])
                    if pend is not None:
                        pend()
                    T.op(ACT, lambda q=q, yb=yb: nc.scalar.activation(out=q.t[:, :], in_=yb.t[:, :], func=AF.Square),
                         reads=[yb], writes=[q])
                    T.op(DVE, lambda yb=yb, dt=dt, fT=fT: nc.vector.tensor_copy(out=fT.t[:, dt, :], in_=yb.t[:, :]),
                         reads=[yb], writes=[fT])

                    def pend(q=q, dt=dt, ssb=ssb):
                        T.op(PE, lambda: MM(ssb.t[:, :], lhsT=ones_b.t[:], rhs=q.t[:, :],
                                                          start=(dt == 0), stop=(dt == 7)),
                             reads=[q, ones_b], writes=[ssb])
            pend()
            T.mark('dp_finalize')
            for ti in range(ng):
                fT, c0 = fTs[ti], c0s[ti]
                rstd_from(ssbs[ti], rt, rstd, 1024.0)
                for dt in range(8):
                    T.op(DVE, lambda dt=dt, fT=fT: nc.vector.scalar_tensor_tensor(
                        out=fT.t[:, dt, :], in0=fT.t[:, dt, :], scalar=gcol(gpost + dt), in1=rstd.t[:, :],
                        op0=ALU.mult, op1=ALU.mult), reads=[fT, rstd, gv], writes=[fT])
                    T.op(DVE, lambda dt=dt, fT=fT, c0=c0: nc.vector.scalar_tensor_tensor(
                        out=xT.t[:, dt, c0:c0 + 512], in0=fT.t[:, dt, :], scalar=half_scale, in1=xT.t[:, dt, c0:c0 + 512],
                        op0=ALU.mult, op1=ALU.add), reads=[fT, xT], writes=[xT])

        def ffn(gpre, gpost, wgu_d, wd_d):
            with contextlib.ExitStack() as fs:
                xn = sbt(fs, [128, 8, 1024], BF, dj=True)
                actT = sbt(fs, [128, NFT, 1024], BF, dj=True)
                fTs = [sbt(fs, [128, 8, 512], F32) for _ in range(2)]
                sg = [sbt(fs, [128, 512], BF) for _ in range(2)]
                sqb = [sbt(fs, [128, 512], BF) for _ in range(2)]
                rt = sbt(fs, [128, 512], F32)
                rstd = sbt(fs, [128, 512], F32)
                SA = Stream(fs, [128, 2048], 4, chunk=1024)
                SB = Stream(fs, [128, DFF], 3, chunk=1408)
                for half in range(2):
                    SA.start([wgu_d[ft] for ft in range(NFT)])
                    SB.start([wd_d[dt] for dt in range(8)])
                rsn = [sbt(fs, [128, 512], F32) for _ in range(2)]

                def stats(hh, ssbanks):
                    for tg in range(2):
                        cc = hh + tg * 512
                        sq_and_sum(sqb, lambda i, cc=cc: xT.t[:, i, cc:cc + 512], [xT], 8, ssbanks[tg])
                        rstd_from(ssbanks[tg], rt, rsn[tg], 1024.0)

                def apply(hh):
                    for tg in range(2):
                        for kc in range(8):
                            T.op(DVE, lambda kc=kc, tg=tg: nc.vector.scalar_tensor_tensor(
                                out=xn.t[:, kc, tg * 512:(tg + 1) * 512], in0=xT.t[:, kc, hh + tg * 512:hh + (tg + 1) * 512],
                                scalar=gcol(gpre + kc), in1=rsn[tg].t[:, :], op0=ALU.mult, op1=ALU.mult),
                                reads=[xT, rsn[tg], gv], writes=[xn])

                SA.get(0)
                stats(0, [banks[6], banks[7]])
                apply(0)
                for half in range(2):
                    h0 = half * 1024
                    for ft in range(NFT):
                        sl = SA.get(half * NFT + ft)
                        if half == 0 and ft == 10:
                            stats(1024, [banks[4], banks[5]])
                        for tg in range(2):
                            hg, hu = banks[2 * tg], banks[2 * tg + 1]
                            T.pre(PE, reads=[sl, xn], writes=[hg, hu])
                            for kc in range(8):
                                MM(hg.t[:, :], lhsT=sl.t[:, kc * 128:(kc + 1) * 128],
                                   rhs=xn.t[:, kc, tg * 512:(tg + 1) * 512], start=(kc == 0), stop=(kc == 7))
                            for kc in range(8):
                                ins = MM(hu.t[:, :], lhsT=sl.t[:, 1024 + kc * 128:1024 + (kc + 1) * 128],
                                         rhs=xn.t[:, kc, tg * 512:(tg + 1) * 512], start=(kc == 0), stop=(kc == 7))
                            T.post(PE, ins, reads=[sl, xn], writes=[hg, hu])
                            s_ = sg[tg]
                            T.op(ACT, lambda s_=s_, hg=hg: nc.scalar.activation(out=s_.t[:, :], in_=hg.t[:, :], func=AF.Silu),
                                 reads=[hg], writes=[s_])
                            T.op(DVE, lambda s_=s_, hu=hu, ft=ft, tg=tg: nc.vector.tensor_tensor(
                                out=actT.t[:, ft, tg * 512:(tg + 1) * 512], in0=hu.t[:, :], in1=s_.t[:, :], op=ALU.mult),
                                reads=[hu, s_], writes=[actT])
                    if half == 0:
                        apply(1024)
                    down_project(SB, half * 8, NFT, lambda k, ti: actT.t[:, k, ti * 512:(ti + 1) * 512], [actT],
                                 fTs, [h0, h0 + 512], gpost, 0.5, [banks[4], banks[5]], [banks[6], banks[7]], sqb, rt, rstd)

        T.barrier()
        if STOP >= 1:
            ffn(G_F1PRE, G_F1POST, w1gu_d, w1d_d)
        T.barrier()

        with contextlib.ExitStack() as ms:
          if STOP >= 2:
            usT = sbt(ms, [128, 4, S], BF, dj=True)
            matt = sbt(ms, [128, 4, S], BF, dj=True)
            with contextlib.ExitStack() as as_:
                Kpk = sbt(as_, [128, 4, S], BF, dj=True)
                Kx = sbt(as_, [128, S], BF, ds=True)
                V = sbt(as_, [128, 16, 512], BF, dj=True)
                Qm = sbt(as_, [128, 8, 512], BF)
                Qx = sbt(as_, [128, 8, 512], BF, ds=True)
                un = sbt(as_, [128, 8, 512], BF, dj=True)
                ksum = sbt(as_, [128, 4, 8], F32)
                ksum_b = sbt(as_, [128, 4, 8], BF)
                Gsb = sbt(as_, [128, 32, 8], F32)
                M8 = sbt(as_, [128, 32, 8], F32)
                SEL = sbt(as_, [128, 32, 8], F32)
                mbias = sbt(as_, [128, 4, 64], F32)
                MTsb = sbt(as_, [64, 512], BF)
                pT = [sbt(as_, [128, 512], BF) for _ in range(3)]
                rden = sbt(as_, [128, 512], F32)
                sqb = [sbt(as_, [128, 512], BF) for _ in range(2)]
                rt = sbt(as_, [128, 512], F32)
                rstd = sbt(as_, [128, 512], F32)
                att = sbt(as_, [128, 4, 512], F32)
                WS = Stream(as_, [128, 1024], 4, chunk=1024)
                T.op(DVE, lambda: nc.vector.memset(ksum.t[:], 0.0), writes=[ksum])
                T.op(DVE, lambda: nc.vector.memset(Kx.t[:], 0.0), writes=[Kx])
                T.op(DVE, lambda: nc.vector.memset(Qx.t[:], 0.0), writes=[Qx])
                T.op(DVE, lambda: nc.vector.memset(Qm.t[:], 0.0), writes=[Qm])
                Qx_ds2 = T.newds()
                cast_dma(Kx, Kx.t[0:12, :], kaug_d, S, Kx.ds)
                for tg in range(4):
                    WS.start([win_d[n] for n in range(16)])
                for tg in range(4):
                    c0 = tg * 512
                    WS.get(tg * 16)
                    T.dma(POOL, Qx.t[8:12, :, :], qaug_d[:, :, c0:c0 + 512], Qx.ds, writes=[Qx])
                    prenorm(c0, G_MIXPRE, un, 0, sqb, banks[6], rt, rstd)
                    for n in range(8):
                        sl = WS.get(tg * 16 + n)
                        pb = banks[n % 2]
                        T.pre(PE, reads=[sl, un], writes=[pb])
                        for kc in range(8):
                            ins = MM(pb.t[:, :], lhsT=sl.t[:, kc * 128:(kc + 1) * 128], rhs=un.t[:, kc, :],
                                                   start=(kc == 0), stop=(kc == 7))
                        T.post(PE, ins, reads=[sl, un], writes=[pb])
                        if n < 4:
                            T.op(ACT, lambda pb=pb, n=n: nc.scalar.copy(out=Kpk.t[:, n, c0:c0 + 512], in_=pb.t[:, :]),
                                 reads=[pb], writes=[Kpk])
                            T.op(DVE, lambda pb=pb, n=n: nc.vector.tensor_reduce(
                                out=ksum.t[:, n, 2 * tg:2 * tg + 2], in_=pb.t[:, :].rearrange("p (a b) -> p a b", b=256),
                                axis=AX.X, op=ALU.add), reads=[pb], writes=[ksum])
                        else:
                            for e in range(2):
                                T.op(ACT, lambda pb=pb, n=n, e=e: nc.scalar.mul(out=Qm.t[64 * e:64 * e + 64, 2 * (n - 4) + e, :],
                                                                               in_=pb.t[64 * e:64 * e + 64, :], mul=0.125),
                                     reads=[pb], writes=[Qm])
                    if tg < 2:
                        T.op(DVE, lambda: nc.vector.memset(mbias.t[:], 0.0), writes=[mbias])
                    else:
                        T.op(DVE, lambda: nc.vector.tensor_copy(out=ksum_b.t[:], in_=ksum.t[:]), reads=[ksum], writes=[ksum_b])
                        gb = banks[7]
                        T.pre(PE, reads=[Qm, ksum_b], writes=[gb])
                        for qt in range(4):
                            for h in range(8):
                                e, hp = h % 2, h // 2
                                ins = MM(gb.t[:, (qt * 8 + h) * 8:(qt * 8 + h) * 8 + 8],
                                                       lhsT=Qm.t[:, h, qt * 128:(qt + 1) * 128],
                                                       rhs=ksum_b.t[:, hp, :], start=True, stop=True)
                        T.post(PE, ins, reads=[Qm, ksum_b], writes=[gb])
                        T.op(DVE, lambda: nc.vector.memset(Gsb.t[:], -1e30), writes=[Gsb])
                        for half2 in range(2):
                            nb = 2 * tg + half2
                            T.op(DVE, lambda half2=half2, nb=nb: nc.vector.tensor_copy(
                                out=Gsb.t[:, 16 * half2:16 * half2 + 16, 0:nb],
                                in_=gb.t[:, 128 * half2:128 * half2 + 128].rearrange("p (a b) -> p a b", b=8)[:, :, 0:nb]),
                                reads=[gb], writes=[Gsb])
                        for i in range(32):
                            T.op(DVE, lambda i=i: nc.vector.max(out=M8.t[:, i, :], in_=Gsb.t[:, i, :]), reads=[Gsb], writes=[M8])
                        T.op(DVE, lambda: nc.vector.tensor_tensor(out=SEL.t[:], in0=Gsb.t[:], in1=M8.t[:, :, 2:3].to_broadcast([128, 32, 8]),
                                                                  op=ALU.is_ge), reads=[Gsb, M8], writes=[SEL])
                        T.op(DVE, lambda: nc.vector.tensor_scalar(out=mbias.t[:].rearrange("p a (h n) -> p (a h) n", n=8), in0=SEL.t[:],
                                                                  scalar1=1.0, scalar2=BIG, op0=ALU.subtract, op1=ALU.mult),
                             reads=[SEL], writes=[mbias])
                        for half2 in range(2):
                            nb = 2 * tg + half2
                            T.op(DVE, lambda half2=half2, nb=nb: nc.vector.memset(
                                mbias.t[:, 2 * half2:2 * half2 + 2, :].rearrange("p a (h n) -> p a h n", n=8)[:, :, :, nb:nb + 1], 0.0),
                                writes=[mbias])
                    for c in range(4):
                        sl = WS.get(tg * 16 + 8 + c)
                        pb = banks[2 + c % 2]
                        T.pre(PE, reads=[sl, un], writes=[pb])
                        for tt in range(4):
                            for kc in range(8):
                                ins = MM(pb.t[:, tt * 128:(tt + 1) * 128], lhsT=un.t[:, kc, tt * 128:(tt + 1) * 128],
                                                       rhs=sl.t[:, kc * 128:(kc + 1) * 128], start=(kc == 0), stop=(kc == 7))
                        T.post(PE, ins, reads=[sl, un], writes=[pb])
                        T.op(DVE, lambda pb=pb, c=c: nc.vector.tensor_copy(
                            out=V.t[:, 4 * tg:4 * tg + 4, c * 128:(c + 1) * 128],
                            in_=pb.t[:, :].rearrange("p (a b) -> p a b", b=128)), reads=[pb], writes=[V])
                    for k in range(4):
                        sl = WS.get(tg * 16 + 12 + k)
                        pb = banks[4 + k % 2]
                        T.pre(PE, reads=[sl, un], writes=[pb])
                        for kc in range(8):
                            ins = MM(pb.t[:, :], lhsT=sl.t[:, kc * 128:(kc + 1) * 128], rhs=un.t[:, kc, :],
                                                   start=(kc == 0), stop=(kc == 7))
                        T.post(PE, ins, reads=[sl, un], writes=[pb])
                        T.op(ACT, lambda pb=pb, k=k: nc.scalar.copy(out=usT.t[:, k, c0:c0 + 512], in_=pb.t[:, :]),
                             reads=[pb], writes=[usT])
                    mb = banks[7]
                    T.pre(PE, reads=[mbias, ident_f], writes=[mb])
                    for qt in range(4):
                        ins = MT(mb.t[0:64, qt * 128:(qt + 1) * 128], mbias.t[:, qt, :], ident_f.t[:, :])
                    T.post(PE, ins, reads=[mbias, ident_f], writes=[mb])
                    T.op(ACT, lambda: nc.scalar.copy(out=MTsb.t[:, :], in_=mb.t[0:64, :]), reads=[mb], writes=[MTsb])
                    for h in range(8):
                        T.dma(SP, Qx.t[0:8, h, :], MTsb.t[8 * h:8 * h + 8, :], Qx_ds2, reads=[MTsb], writes=[Qx])
                    nkt = 4 * tg + 4
                    for hp in range(4):
                        numP, denP = banks[2 + hp % 2], banks[4 + hp % 2]
                        for e in range(2):
                            h = 2 * hp + e
                            rows = slice(64 * e, 64 * e + 64)
                            pend = None
                            for kt in range(nkt):
                                j = kt - 4 * tg
                                ql = 128 * j if j > 0 else 0
                                sP = banks[kt % 2]
                                rd = [Kpk, Qm, Kx, Qx, ident_b, trineg]
                                T.pre(PE, reads=rd, writes=[sP])
                                MM(sP.t[:, ql:512], lhsT=Kpk.t[:, hp, kt * 128:(kt + 1) * 128],
                                                 rhs=Qm.t[:, h, ql:512], start=True, stop=False)
                                ins = MM(sP.t[:, ql:512], lhsT=Kx.t[:, kt * 128:(kt + 1) * 128],
                                                       rhs=Qx.t[:, h, ql:512], start=False, stop=(j < 0))
                                if j >= 0:
                                    ins = MM(sP.t[:, ql:ql + 128], lhsT=ident_b.t[:, :], rhs=trineg.t[:, :],
                                                           start=False, stop=True)
                                T.post(PE, ins, reads=rd, writes=[sP])
                                p = pT[kt % 3]
                                T.op(ACT, lambda p=p, sP=sP, ql=ql: nc.scalar.activation(out=p.t[:, ql:512], in_=sP.t[:, ql:512], func=AF.Exp),
                                     reads=[sP], writes=[p])
                                if pend is not None:
                                    pend()

                                def pend(p=p, kt=kt, ql=ql):
                                    T.pre(PE, reads=[p, V, ones_b], writes=[numP, denP])
                                    MM(numP.t[rows, ql:512], lhsT=V.t[:, kt, h * 64:(h + 1) * 64], rhs=p.t[:, ql:512],
                                                     start=(kt == 0), stop=(kt == nkt - 1))
                                    ins2 = MM(denP.t[rows, ql:512], lhsT=ones_b.t[:, 0:64], rhs=p.t[:, ql:512],
                                                            start=(kt == 0), stop=(kt == nkt - 1))
                                    T.post(PE, ins2, reads=[p, V, ones_b], writes=[numP, denP])
                            pend()
                        T.op(DVE, lambda: nc.vector.reciprocal(out=rden.t[:, :], in_=denP.t[:, :]), reads=[denP], writes=[rden])
                        T.op(DVE, lambda: nc.vector.tensor_tensor(out=att.t[:, hp, :], in0=numP.t[:, :], in1=rden.t[:, :],
                                                                  op=ALU.mult), reads=[numP, rden], writes=[att])
                    sq_and_sum(sqb, lambda i: att.t[:, i, :], [att], 4, banks[6])
                    rstd_from(banks[6], rt, rstd, 512.0)
                    for hp in range(4):
                        T.op(DVE, lambda hp=hp: nc.vector.scalar_tensor_tensor(
                            out=matt.t[:, hp, c0:c0 + 512], in0=att.t[:, hp, :], scalar=gcol(G_ATT + hp), in1=rstd.t[:, :],
                            op0=ALU.mult, op1=ALU.mult), reads=[att, rstd, gv], writes=[matt])
            T.barrier()
            if STOP >= 3:
             with contextlib.ExitStack() as ss_:
                CS = sbt(ss_, [128, 2, 16, 128], BF)
                BB = sbt(ss_, [128, 16, 2, 128], BF)
                CC = sbt(ss_, [128, 16, 3, 64], BF)
                DDm = sbt(ss_, [128, 512], BF, ds=True)
                wglu = sbt(ss_, [128, 2048], BF, ds=True)
                mag_s = sbt(ss_, [128, 16], F32)
                ECS = sbt(ss_, [128, 2, 16], F32)
                Hc = sbt(ss_, [128, 2, 16], F32)
                Hm = sbt(ss_, [128, 2, 16], F32)
                magT = sbt(ss_, [128, 16, 128], F32)
                cast_dma(DDm, DDm.t, dlay_d, 512, DDm.ds)
                cast_dma(wglu, wglu.t, wglu_d, 2048, wglu.ds)

                def sincos(stk, ang, n, sinb, cosb, tmpF, tmpI):
                    T.op(DVE, lambda: nc.vector.tensor_scalar(out=tmpI.t[:, :n], in0=ang.t[:, :n], scalar1=1.0 / (2 * PI), scalar2=None,
                                                              op0=ALU.mult), reads=[ang], writes=[tmpI])
                    T.op(DVE, lambda: nc.vector.tensor_copy(out=tmpF.t[:, :n], in_=tmpI.t[:, :n]), reads=[tmpI], writes=[tmpF])
                    T.op(DVE, lambda: nc.vector.scalar_tensor_tensor(out=tmpF.t[:, :n], in0=tmpF.t[:, :n], scalar=-2 * PI, in1=ang.t[:, :n],
                                                                     op0=ALU.mult, op1=ALU.add), reads=[tmpF, ang], writes=[tmpF])
                    T.op(DVE, lambda: nc.vector.tensor_scalar(out=tmpF.t[:, :n], in0=tmpF.t[:, :n], scalar1=-PI, scalar2=PI,
                                                              op0=ALU.max, op1=ALU.min), reads=[tmpF], writes=[tmpF])
                    T.op(ACT, lambda: nc.scalar.activation(out=sinb.t[:, :n], in_=tmpF.t[:, :n], func=AF.Sin), reads=[tmpF], writes=[sinb])
                    T.op(ACT, lambda: nc.scalar.activation(out=tmpF.t[:, :n], in_=tmpF.t[:, :n], func=AF.Abs), reads=[tmpF, sinb], writes=[tmpF])
                    T.op(DVE, lambda: nc.vector.tensor_scalar(out=tmpF.t[:, :n], in0=tmpF.t[:, :n], scalar1=-1.0, scalar2=PI / 2,
                                                              op0=ALU.mult, op1=ALU.add), reads=[tmpF], writes=[tmpF])
                    T.op(ACT, lambda: nc.scalar.activation(out=cosb.t[:, :n], in_=tmpF.t[:, :n], func=AF.Sin), reads=[tmpF], writes=[cosb])

                TT = lambda o, a, b, op, rd, wr: T.op(DVE, lambda: nc.vector.tensor_tensor(out=o, in0=a, in1=b, op=op), reads=rd, writes=wr)
                with contextlib.ExitStack() as st:
                    lamst = sbt(st, [128, 48], F32, ds=True)
                    iota = sbt(st, [128, 128], F32, ds=True)
                    clay = sbt(st, [128, 2 * 1024], F32, ds=True)
                    T.dma(SP, lamst.t[:], lamst_d, lamst.ds, writes=[lamst])
                    T.dma(SP, iota.t[:], cst_d[:, 256:384], iota.ds, writes=[iota])
                    T.dma(SP, clay.t[:], clay_d, clay.ds, writes=[clay])
                    W = [sbt(st, [128, 2048], F32) for _ in range(3)]
                    WI = sbt(st, [128, 2048], I32)
                    sS = sbt(st, [128, 2048], F32)
                    sC = sbt(st, [128, 2048], F32)
                    dts, th_s, e1 = W[0], W[1], W[2]
                    T.op(ACT, lambda: nc.scalar.activation(out=dts.t[:, :16], in_=lamst.t[:, 32:48], func=AF.Exp), reads=[lamst], writes=[dts])
                    TT(e1.t[:, :16], lamst.t[:, 0:16], dts.t[:, :16], ALU.mult, [lamst, dts], [e1])
                    T.op(ACT, lambda: nc.scalar.activation(out=mag_s.t[:, :], in_=e1.t[:, :16], func=AF.Exp), reads=[e1], writes=[mag_s])
                    TT(th_s.t[:, :16], lamst.t[:, 16:32], dts.t[:, :16], ALU.mult, [lamst, dts], [th_s])
                    for j in range(16):
                        T.op(DVE, lambda j=j: nc.vector.tensor_scalar(out=magT.t[:, j, :], in0=iota.t[:, :], scalar1=0.0,
                                                                      scalar2=mag_s.t[:, j:j + 1], op0=ALU.mult, op1=ALU.add),
                             reads=[iota, mag_s], writes=[magT])
                    T.op(DVE, lambda: nc.vector.memset(magT.t[:, :, 0:1], 0.0), writes=[magT])
                    PH = W[2]
                    for j in range(16):
                        T.op(DVE, lambda j=j: nc.vector.tensor_scalar(out=PH.t[:, j * 128:(j + 1) * 128], in0=iota.t[:, :],
                                                                      scalar1=th_s.t[:, j:j + 1], scalar2=None, op0=ALU.mult),
                             reads=[iota, th_s], writes=[PH])
                    sincos(st, PH, 2048, sS, sC, W[0], WI)
                    j3 = lambda ap: ap.rearrange("p (j t) -> p j t", t=128)
                    T.op(DVE, lambda: nc.vector.tensor_copy(out=CS.t[:, 0, :, :], in_=j3(sC.t[:, :])), reads=[sC], writes=[CS])
                    T.op(DVE, lambda: nc.vector.tensor_copy(out=CS.t[:, 1, :, :], in_=j3(sS.t[:, :])), reads=[sS], writes=[CS])
                    T.op(DVE, lambda: nc.vector.tensor_copy(out=ECS.t[:, 0, :], in_=j3(sC.t[:, :])[:, :, 127]), reads=[sC], writes=[ECS])
                    T.op(DVE, lambda: nc.vector.tensor_copy(out=ECS.t[:, 1, :], in_=j3(sS.t[:, :])[:, :, 127]), reads=[sS], writes=[ECS])
                    c3 = lambda ap: ap.rearrange("p (j c) -> p j c", c=64)
                    T.op(DVE, lambda: nc.vector.tensor_copy(out=CC.t[:, :, 0, :], in_=c3(clay.t[:, 0:1024])), reads=[clay], writes=[CC])
                    T.op(DVE, lambda: nc.vector.tensor_scalar(out=CC.t[:, :, 1, :], in0=c3(clay.t[:, 0:1024]), scalar1=-1.0, scalar2=None,
                                                              op0=ALU.mult), reads=[clay], writes=[CC])
                    T.op(DVE, lambda: nc.vector.tensor_scalar(out=CC.t[:, :, 2, :], in0=c3(clay.t[:, 1024:2048]), scalar1=-1.0, scalar2=None,
                                                              op0=ALU.mult), reads=[clay], writes=[CC])
                T.barrier()
                with contextlib.ExitStack() as st:
                    NH = 1024
                    lamh = sbt(st, [128, 3 * NH], F32, ds=True)
                    blh = sbt(st, [128, 2 * NH], F32, ds=True)
                    W = [sbt(st, [128, NH], F32) for _ in range(7)]
                    WI = sbt(st, [128, NH], I32)
                    sS = sbt(st, [128, NH], F32)
                    sC = sbt(st, [128, NH], F32)
                    for hf in range(2):
                        for q in range(3):
                            T.dma(SP, lamh.t[:, q * NH:(q + 1) * NH], lambc_d[:, q * 2048 + hf * NH:q * 2048 + (hf + 1) * NH], lamh.ds, writes=[lamh])
                        for q in range(2):
                            T.dma(SP, blh.t[:, q * NH:(q + 1) * NH], blay_d[:, q * 2048 + hf * NH:q * 2048 + (hf + 1) * NH], blh.ds, writes=[blh])
                        lr, li, ldt = lamh.t[:, 0:NH], lamh.t[:, NH:2 * NH], lamh.t[:, 2 * NH:3 * NH]
                        dtb, thb, magb = W[0], W[1], W[2]
                        T.op(ACT, lambda: nc.scalar.activation(out=dtb.t[:, :], in_=ldt, func=AF.Exp), reads=[lamh], writes=[dtb])
                        TT(magb.t[:, :], lr, dtb.t[:, :], ALU.mult, [lamh, dtb], [magb])
                        T.op(ACT, lambda: nc.scalar.activation(out=magb.t[:, :], in_=magb.t[:, :], func=AF.Exp), reads=[magb], writes=[magb])
                        TT(thb.t[:, :], li, dtb.t[:, :], ALU.mult, [lamh, dtb], [thb])
                        sincos(st, thb, NH, sS, sC, W[4], WI)
                        are, aim, den, fre, fim, tmp = W[3], W[4], W[5], W[6], W[0], W[1]
                        TT(are.t[:, :], magb.t[:, :], sC.t[:, :], ALU.mult, [magb, sC], [are])
                        TT(aim.t[:, :], magb.t[:, :], sS.t[:, :], ALU.mult, [magb, sS], [aim])
                        T.op(DVE, lambda: nc.vector.tensor_scalar(out=are.t[:, :], in0=are.t[:, :], scalar1=-1.0, scalar2=None, op0=ALU.add),
                             reads=[are], writes=[are])
                        TT(den.t[:, :], lr, lr, ALU.mult, [lamh], [den])
                        TT(tmp.t[:, :], li, li, ALU.mult, [lamh], [tmp])
                        TT(den.t[:, :], den.t[:, :], tmp.t[:, :], ALU.add, [den, tmp], [den])
                        T.op(DVE, lambda: nc.vector.reciprocal(out=den.t[:, :], in_=den.t[:, :]), reads=[den], writes=[den])
                        TT(fre.t[:, :], are.t[:, :], lr, ALU.mult, [are, lamh], [fre])
                        TT(tmp.t[:, :], aim.t[:, :], li, ALU.mult, [aim, lamh], [tmp])
                        TT(fre.t[:, :], fre.t[:, :], tmp.t[:, :], ALU.add, [fre, tmp], [fre])
                        TT(fre.t[:, :], fre.t[:, :], den.t[:, :], ALU.mult, [fre, den], [fre])
                        TT(fim.t[:, :], aim.t[:, :], lr, ALU.mult, [aim, lamh], [fim])
                        TT(tmp.t[:, :], are.t[:, :], li, ALU.mult, [are, lamh], [tmp])
                        TT(fim.t[:, :], fim.t[:, :], tmp.t[:, :], ALU.subtract, [fim, tmp], [fim])
                        TT(fim.t[:, :], fim.t[:, :], den.t[:, :], ALU.mult, [fim, den], [fim])
                        bre, bim = blh.t[:, 0:NH], blh.t[:, NH:2 * NH]
                        t2, t3 = W[2], W[3]
                        v3 = lambda ap: ap.rearrange("p (j c) -> p j c", c=128)
                        js = slice(8 * hf, 8 * hf + 8)
                        TT(t2.t[:, :], fre.t[:, :], bre, ALU.mult, [fre, blh], [t2])
                        TT(t3.t[:, :], fim.t[:, :], bim, ALU.mult, [fim, blh], [t3])
                        TT(BB.t[:, js, 0, :], v3(t2.t[:, :]), v3(t3.t[:, :]), ALU.subtract, [t2, t3], [BB])
                        TT(t2.t[:, :], fre.t[:, :], bim, ALU.mult, [fre, blh], [t2])
                        TT(t3.t[:, :], fim.t[:, :], bre, ALU.mult, [fim, blh], [t3])
                        TT(BB.t[:, js, 1, :], v3(t2.t[:, :]), v3(t3.t[:, :]), ALU.add, [t2, t3], [BB])
                T.barrier()
                TTq = sbt(ss_, [128, 4, 4, 128], BF)
                XTq = sbt(ss_, [128, 2, 4, 128], BF)
                Gq = sbt(ss_, [128, 2, 4, 128], BF)
                Pq = [sbt(ss_, [128, 4, 4, 128], BF) for _ in range(2)]
                ctmp = sbt(ss_, [128, 4, 4], F32)
                ygf = sbt(ss_, [128, 4, 512], F32)
                ygb = sbt(ss_, [128, 4, 512], BF)
                sig = sbt(ss_, [128, 512], F32)
                mssm = sbt(ss_, [128, 4, 512], BF)
                fT = sbt(ss_, [128, 8, 512], F32)
                sqb = [sbt(ss_, [128, 512], BF) for _ in range(2)]
                rt = sbt(ss_, [128, 512], F32)
                rstd = sbt(ss_, [128, 512], F32)
                WO = Stream(ss_, [128, 1024], 3, chunk=1024)
                for tg in range(4):
                    WO.start([wout_d[dt] for dt in range(8)])
                it = 0
                for tg in range(4):
                    c0 = tg * 512
                    for cc in range(4):
                        c = 4 * tg + cc
                        t0 = c0 + 128 * cc
                        for k in range(4):
                            XA, XB = banks[2 * (it % 2)], banks[2 * (it % 2) + 1]
                            P = Pq[it % 2]
                            it += 1
                            T.pre(PE, reads=[BB, usT], writes=[XA, XB])
                            for jl in range(4):
                                j, e = 4 * k + jl, jl // 2
                                rows = slice(64 * e, 64 * e + 64)
                                MM(XA.t[:, jl * 128:(jl + 1) * 128], lhsT=BB.t[:, j, 0, :], rhs=usT.t[:, k, t0:t0 + 128],
                                                 start=True, stop=True)
                                ins = MM(XB.t[:, jl * 128:(jl + 1) * 128], lhsT=BB.t[:, j, 1, :], rhs=usT.t[:, k, t0:t0 + 128],
                                                       start=True, stop=True)
                            T.post(PE, ins, reads=[BB, usT], writes=[XA, XB])
                            q4 = slice(4 * k, 4 * k + 4)
                            xa = XA.t[:, :].rearrange("p (a b) -> p a b", b=128)
                            xb = XB.t[:, :].rearrange("p (a b) -> p a b", b=128)
                            cosq, sinq = CS.t[:, 0, q4, :], CS.t[:, 1, q4, :]
                            T.op(DVE, lambda: nc.vector.tensor_tensor(out=TTq.t[:, 0], in0=xa, in1=cosq, op=ALU.mult), reads=[XA, CS], writes=[TTq])
                            T.op(DVE, lambda: nc.vector.tensor_tensor(out=TTq.t[:, 1], in0=xb, in1=sinq, op=ALU.mult), reads=[XB, CS], writes=[TTq])
                            T.op(DVE, lambda: nc.vector.tensor_tensor(out=TTq.t[:, 2], in0=xb, in1=cosq, op=ALU.mult), reads=[XB, CS], writes=[TTq])
                            T.op(DVE, lambda: nc.vector.tensor_tensor(out=TTq.t[:, 3], in0=xa, in1=sinq, op=ALU.mult), reads=[XA, CS], writes=[TTq])
                            T.op(DVE, lambda: nc.vector.tensor_tensor(out=XTq.t[:, 0], in0=TTq.t[:, 0], in1=TTq.t[:, 1], op=ALU.add),
                                 reads=[TTq], writes=[XTq])
                            T.op(DVE, lambda: nc.vector.tensor_tensor(out=XTq.t[:, 1], in0=TTq.t[:, 2], in1=TTq.t[:, 3], op=ALU.subtract),
                                 reads=[TTq], writes=[XTq])
                            if c > 0:
                                T.op(DVE, lambda: nc.vector.tensor_tensor(out=XTq.t[:, :, :, 0], in0=XTq.t[:, :, :, 0], in1=Hm.t[:, :, q4], op=ALU.add),
                                     reads=[XTq, Hm], writes=[XTq])
                            for r in range(2):
                                T.op(DVE, lambda r=r: nc.vector.tensor_tensor_scan(
                                    out=Gq.t[:, r, :, :].rearrange("p a b -> p (a b)"), data0=magT.t[:, q4, :].rearrange("p a b -> p (a b)"),
                                    data1=XTq.t[:, r, :, :].rearrange("p a b -> p (a b)"), initial=0.0, op0=ALU.mult, op1=ALU.add),
                                    reads=[XTq, magT], writes=[Gq])
                            T.op(DVE, lambda: nc.vector.tensor_tensor(out=P.t[:, 0:2], in0=Gq.t[:, 0:2], in1=CS.t[:, 0:2, q4, :], op=ALU.mult),
                                 reads=[Gq, CS], writes=[P])
                            T.op(DVE, lambda: nc.vector.tensor_tensor(out=P.t[:, 2], in0=Gq.t[:, 1], in1=cosq, op=ALU.mult), reads=[Gq, CS], writes=[P])
                            T.op(DVE, lambda: nc.vector.tensor_tensor(out=P.t[:, 3], in0=Gq.t[:, 0], in1=sinq, op=ALU.mult), reads=[Gq, CS], writes=[P])
                            T.op(DVE, lambda: nc.vector.tensor_tensor(out=Hc.t[:, 0, q4], in0=P.t[:, 0, :, 127], in1=P.t[:, 1, :, 127], op=ALU.subtract),
                                 reads=[P], writes=[Hc])
                            T.op(DVE, lambda: nc.vector.tensor_tensor(out=Hc.t[:, 1, q4], in0=P.t[:, 2, :, 127], in1=P.t[:, 3, :, 127], op=ALU.add),
                                 reads=[P], writes=[Hc])
                            for r in range(2):
                                T.op(DVE, lambda r=r: nc.vector.tensor_tensor(out=Hm.t[:, r, q4], in0=Hc.t[:, r, q4], in1=mag_s.t[:, q4], op=ALU.mult),
                                     reads=[Hc, mag_s], writes=[Hm])
                            Y = banks[4 + k]
                            rd = [CC, P, DDm, usT]
                            T.pre(PE, reads=rd, writes=[Y])
                            for e in range(2):
                                rows = slice(64 * e, 64 * e + 64)
                                yo = Y.t[rows, cc * 128:(cc + 1) * 128]
                                first = True
                                for jl in (2 * e, 2 * e + 1):
                                    j = 4 * k + jl
                                    for (ci, pi_) in ((0, 0), (1, 1), (2, 2), (2, 3)):
                                        MM(yo, lhsT=CC.t[:, j, ci, :], rhs=P.t[:, pi_, jl, :], start=first, stop=False)
                                        first = False
                                ins = MM(yo, lhsT=DDm.t[:, k * 128 + 64 * e:k * 128 + 64 * e + 64],
                                                       rhs=usT.t[:, k, t0:t0 + 128], start=False, stop=True)
                            T.post(PE, ins, reads=rd, writes=[Y])
                    for k in range(4):
                        Y = banks[4 + k]
                        T.op(ACT, lambda k=k, Y=Y: nc.scalar.activation(out=ygf.t[:, k, :], in_=Y.t[:, :], func=AF.Gelu_apprx_tanh),
                             reads=[Y], writes=[ygf])
                        T.op(DVE, lambda k=k: nc.vector.tensor_copy(out=ygb.t[:, k, :], in_=ygf.t[:, k, :]), reads=[ygf], writes=[ygb])
                    for ko in range(4):
                        GP = banks[ko % 2]
                        T.pre(PE, reads=[wglu, ygb], writes=[GP])
                        for ki in range(4):
                            ins = MM(GP.t[:, :], lhsT=wglu.t[:, ki * 512 + ko * 128:ki * 512 + (ko + 1) * 128], rhs=ygb.t[:, ki, :],
                                                   start=(ki == 0), stop=(ki == 3))
                        T.post(PE, ins, reads=[wglu, ygb], writes=[GP])
                        T.op(ACT, lambda GP=GP, ko=ko: nc.scalar.activation(out=sig.t[:, :], in_=GP.t[:, :], func=AF.Sigmoid,
                                                                          bias=gcol(G_BGLU + ko)), reads=[GP, gv], writes=[sig])
                        T.op(DVE, lambda ko=ko: nc.vector.tensor_tensor(out=ygf.t[:, ko, :], in0=ygf.t[:, ko, :], in1=sig.t[:, :], op=ALU.mult),
                             reads=[ygf, sig], writes=[ygf])
                    sq_and_sum(sqb, lambda i: ygf.t[:, i, :], [ygf], 4, banks[2])
                    rstd_from(banks[2], rt, rstd, 512.0)
                    for ko in range(4):
                        T.op(DVE, lambda ko=ko: nc.vector.scalar_tensor_tensor(
                            out=mssm.t[:, ko, :], in0=ygf.t[:, ko, :], scalar=gcol(G_SSM + ko), in1=rstd.t[:, :],
                            op0=ALU.mult, op1=ALU.mult), reads=[ygf, rstd, gv], writes=[mssm])
                    down_project(WO, tg * 8, 8,
                                 lambda m, ti, c0=c0: (matt.t[:, m, c0:c0 + 512] if m < 4 else mssm.t[:, m - 4, :]), [matt, mssm],
                                 [fT], [c0], G_MIXPOST, 1.0, [banks[0], banks[1]], [banks[3]], sqb, rt, rstd)
            T.barrier()

        if STOP >= 4:
            ffn(G_F2PRE, G_F2POST, w2gu_d, w2d_d)
        T.barrier()
        T.live = True
        T.cut = 10 ** 9
        T.barrier()
        od = T.newds()
        T.dma(SP, out_d, xT.t[:], od, reads=[xT])
        SP.e.wait_ge(od.sem, od.cnt)
    return nc


def _shared_layouts(inp):
    f = np.float32
    A = lambda k: np.ascontiguousarray(np.asarray(inp[k], dtype=f)[0])
    o = {}
    gv = np.zeros((128, 64), f)

    def put(c0, v):
        n = v.shape[0] // 128
        gv[:, c0:c0 + n] = v.reshape(n, 128).T
    put(G_F1PRE, A("ffn1_pre_g")); put(G_F1POST, A("ffn1_post_g")); put(G_MIXPRE, A("mix_pre_g"))
    put(G_ATT, A("attn_out_g")); put(G_SSM, A("ssm_out_g")); put(G_MIXPOST, A("mix_post_g"))
    put(G_F2PRE, A("ffn2_pre_g")); put(G_F2POST, A("ffn2_post_g")); put(G_BGLU, A("b_glu"))
    o["gv"] = gv

    def gu(wg, wu):
        a = wg.reshape(8, 128, NFT, 128).transpose(2, 1, 0, 3)
        b = wu.reshape(8, 128, NFT, 128).transpose(2, 1, 0, 3)
        return np.ascontiguousarray(np.concatenate([a, b], axis=2).reshape(NFT, 128, 2048))

    def dn(wd):
        return np.ascontiguousarray(wd.reshape(NFT, 128, 8, 128).transpose(2, 1, 0, 3).reshape(8, 128, DFF))
    o["w1gu"] = gu(A("ffn1_w_gate"), A("ffn1_w_up")); o["w1d"] = dn(A("ffn1_w_down"))
    o["w2gu"] = gu(A("ffn2_w_gate"), A("ffn2_w_up")); o["w2d"] = dn(A("ffn2_w_down"))
    win = A("w_in")
    order = [512 + 128 * i for i in range(4)] + [128 * i for i in range(4)] + [1024 + 128 * i for i in range(8)]
    o["win"] = np.ascontiguousarray(np.stack([win[:, c:c + 128].reshape(8, 128, 128).transpose(1, 0, 2).reshape(128, 1024) for c in order]))
    o["wglu"] = np.ascontiguousarray(A("w_glu").reshape(4, 128, 512).transpose(1, 0, 2).reshape(128, 2048))
    wo = A("w_out")
    o["wout"] = np.ascontiguousarray(wo.reshape(8, 128, 8, 128).transpose(2, 1, 0, 3).reshape(8, 128, 1024))
    lr, li, ld = A("lam_re"), A("lam_im"), A("log_dt")
    st = lambda a: a.reshape(16, 2, 64).transpose(1, 2, 0).reshape(128, 16)
    ldf = np.repeat(ld[:, None], 64, axis=1)
    o["lamst"] = np.ascontiguousarray(np.concatenate([st(lr), st(li), st(ldf)], axis=1))
    bc = np.concatenate([lr.reshape(-1), li.reshape(-1), ldf.reshape(-1)])
    o["lambc"] = np.ascontiguousarray(np.broadcast_to(bc[None, :], (128, 6144)))
    bre, bim = A("b_re"), A("b_im")
    bl = np.zeros((2, 128, 16, 128), f)
    cre, cim = A("c_re"), A("c_im")
    cl = np.zeros((2, 128, 16, 64), f)
    for j in range(16):
        q = j % 4
        for g2 in range(2):
            g = 2 * j + g2
            r0 = 32 * q + 16 * g2
            bl[0, r0:r0 + 16, j, 64 * g2:64 * g2 + 64] = bre[g].T
            bl[1, r0:r0 + 16, j, 64 * g2:64 * g2 + 64] = bim[g].T
            cc0 = 32 * (j % 2) + 16 * g2
            cl[0, 64 * g2:64 * g2 + 64, j, cc0:cc0 + 16] = cre[g].T
            cl[1, 64 * g2:64 * g2 + 64, j, cc0:cc0 + 16] = cim[g].T
    o["blay"] = np.ascontiguousarray(bl.transpose(1, 0, 2, 3).reshape(128, 4096))
    o["clay"] = np.ascontiguousarray(cl.transpose(1, 0, 2, 3).reshape(128, 2048))
    dsk = A("d_skip").reshape(512)
    dl = np.zeros((128, 4, 128), f)
    for k in range(4):
        dl[np.arange(128), k, np.arange(128)] = dsk[k * 128:(k + 1) * 128]
    o["dlay"] = dl.reshape(128, 512)
    pos = np.arange(S)
    ka = np.zeros((12, S), f)
    ka[pos // 256, pos] = 1.0
    ka[8] = 1.0; ka[9] = 1.0; ka[10] = 128.0 * (pos // 128); ka[11] = pos % 128
    o["kaugc"] = ka
    qa = np.zeros((4, 8, S), f)
    for h in range(8):
        sl = 2.0 ** (-(h + 1))
        qa[0, h] = -sl * 128.0 * (pos // 128); qa[1, h] = -sl * (pos % 128); qa[2, h] = sl; qa[3, h] = sl
    o["qaugc"] = qa
    cst = np.zeros((128, 384), f)
    cst[:, 0:128] = np.eye(128, dtype=f)
    kk, qq = np.meshgrid(np.arange(128), np.arange(128), indexing="ij")
    cst[:, 128:256] = np.where(kk > qq, -BIG, 0.0)
    cst[:, 256:384] = np.arange(1, 129, dtype=f)[None, :]
    o["cst"] = cst
    return o


_NC = None


def kernel(**inputs):
    global _NC
    x = np.asarray(inputs["x"], dtype=np.float32)
    nb = x.shape[0]
    sh = _shared_layouts(inputs)
    in_maps = []
    for b in range(nb):
        m = dict(sh)
        m["xT"] = np.ascontiguousarray(x[b].T.reshape(8, 128, S).transpose(1, 0, 2))
        in_maps.append(m)
    if _NC is None:
        _NC = build()
    res = run_bass_kernel_spmd(_NC, in_maps, core_ids=list(range(nb)))
    out = np.empty_like(x)
    for b in range(nb):
        yT = np.asarray(res.results[b]["yT"], dtype=np.float32)
        out[b] = yT.transpose(1, 0, 2).reshape(1024, S).T
    return out
```

```python
import contextlib
import math
import numpy as np
import concourse.bass as bass
import concourse.mybir as mybir
from concourse.bass_utils import run_bass_kernel_spmd

F32 = mybir.dt.float32
BF = mybir.dt.bfloat16
I32 = mybir.dt.int32
AF = mybir.ActivationFunctionType
ALU = mybir.AluOpType
AX = mybir.AxisListType
PI = math.pi
S = 2048
DFF = 2816
NFT = 22
EPS = 1e-6
BIG = 30000.0

G_F1PRE, G_F1POST, G_MIXPRE, G_ATT, G_SSM, G_MIXPOST, G_F2PRE, G_F2POST, G_BGLU = 0, 8, 16, 24, 28, 32, 40, 48, 56


class Eng:
    def __init__(self, nc, e, name, es):
        self.e, self.name = e, name
        self.sem = es.enter_context(nc.semaphore("s_" + name))
        self.cnt = 0
        self.seen = {}


class DS:
    def __init__(self, nc, name, es):
        self.sem = es.enter_context(nc.semaphore(name))
        self.cnt = 0
        self.name = name


class Buf:
    def __init__(self, t, ds=None, dj=False, psum=False):
        self.psum = psum
        self.dj = dj
        self.t = t
        self.w = None
        self.r = {}
        self.ds = ds


class Trk:
    def __init__(self, nc, es):
        self.nc = nc
        self.es = es
        self.PE = Eng(nc, nc.tensor, "pe", es)
        self.ACT = Eng(nc, nc.scalar, "act", es)
        self.DVE = Eng(nc, nc.vector, "dve", es)
        self.POOL = Eng(nc, nc.gpsimd, "pool", es)
        self.SP = Eng(nc, nc.sync, "sp", es)
        self.engs = [self.PE, self.ACT, self.DVE, self.POOL, self.SP]
        self.dss = []
        self.nds = 0
        self.n = 0
        self.live = True
        self.cut = CUT

    def newds(self):
        self.nds += 1
        d = DS(self.nc, "d%d" % self.nds, self.es)
        self.dss.append(d)
        return d

    def wait(self, E, tok):
        src, val = tok
        if src is E and E is self.PE:
            return
        k = id(src)
        if E.seen.get(k, 0) >= val:
            return
        E.e.wait_ge(src.sem, val)
        E.seen[k] = val

    def pre(self, E, reads=(), writes=()):
        if self.n >= self.cut:
            self.live = False
        if not self.live:
            return
        for b in reads:
            if b.w is not None:
                self.wait(E, b.w)
            if b.psum:
                for src, val in list(b.r.values()):
                    if src is not E:
                        self.wait(E, (src, val))
        for b in writes:
            if b.w is not None and (b.w[0] is not E or not b.dj):
                self.wait(E, b.w)
            for src, val in list(b.r.values()):
                self.wait(E, (src, val))

    def _reg(self, tok, reads, writes):
        for b in reads:
            b.r[id(tok[0])] = tok
        for b in writes:
            b.w = tok
            b.r = {}

    def post(self, E, inst, reads=(), writes=()):
        if not self.live:
            return
        self.n += 1
        E.cnt += 1
        inst.then_inc(E.sem, 1)
        self._reg((E, E.cnt), reads, writes)

    def op(self, E, fn, reads=(), writes=()):
        self.pre(E, reads, writes)
        if not self.live:
            return
        inst = fn()
        self.post(E, inst, reads, writes)

    def dma(self, Q, out, in_, ds, reads=(), writes=(), **kw):
        self.pre(Q, reads, writes)
        if not self.live:
            return
        self.n += 1
        inst = Q.e.dma_start(out=out, in_=in_, **kw)
        ds.cnt += 16
        inst.then_inc(ds.sem, 16)
        self._reg((ds, ds.cnt), reads, writes)

    def mark(self, name):
        MARKS.append((name, self.n))

    def barrier(self):
        if not self.live:
            return
        for E in self.engs:
            for Fe in self.engs:
                if Fe is not E and Fe.cnt > 0:
                    self.wait(E, (Fe, Fe.cnt))
            for d in self.dss:
                if d.cnt > 0:
                    self.wait(E, (d, d.cnt))


import os
STOP = int(os.environ.get('KSTOP', '99'))
CUT = int(os.environ.get('KCUT', str(10 ** 9)))
MARKS = []


def build():
    nc = bass.Bass("TRN2", target_bir_lowering=False)

    def D(name, shape, kind="ExternalInput", dt=F32):
        return nc.dram_tensor(name, shape, dt, kind=kind).ap()

    xT_d = D("xT", [128, 8, S])
    gv_d = D("gv", [128, 64])
    w1gu_d = D("w1gu", [NFT, 128, 2048])
    w1d_d = D("w1d", [8, 128, DFF])
    w2gu_d = D("w2gu", [NFT, 128, 2048])
    w2d_d = D("w2d", [8, 128, DFF])
    win_d = D("win", [16, 128, 1024])
    wglu_d = D("wglu", [128, 2048])
    wout_d = D("wout", [8, 128, 1024])
    lamst_d = D("lamst", [128, 48])
    lambc_d = D("lambc", [128, 3 * 2048])
    blay_d = D("blay", [128, 2 * 2048])
    clay_d = D("clay", [128, 2 * 1024])
    dlay_d = D("dlay", [128, 512])
    kaug_d = D("kaugc", [12, S])
    qaug_d = D("qaugc", [4, 8, S])
    cst_d = D("cst", [128, 3 * 128])
    out_d = D("yT", [128, 8, S], kind="ExternalOutput")

    es = contextlib.ExitStack()
    with es:
        T = Trk(nc, es)
        PE, ACT, DVE, POOL, SP = T.PE, T.ACT, T.DVE, T.POOL, T.SP
        cnt = [0]

        def sbt(stack, shape, dt, ds=False, dj=False):
            cnt[0] += 1
            t = stack.enter_context(nc.sbuf_tensor("t%d" % cnt[0], shape, dt))
            return Buf(t, T.newds() if ds else None, dj)

        xT = sbt(es, [128, 8, S], F32, ds=True)
        gv = sbt(es, [128, 64], F32, ds=True)
        ident_f = sbt(es, [128, 128], F32, ds=True)
        ident_b = sbt(es, [128, 128], BF, ds=True)
        trineg = sbt(es, [128, 128], BF, ds=True)
        ones_b = sbt(es, [128, 128], BF)
        banks = []
        for i in range(8):
            banks.append(Buf(es.enter_context(nc.psum_tensor("bank%d" % i, [128, 512], F32)), psum=True))

        T.dma(SP, xT.t[:], xT_d, xT.ds, writes=[xT])
        T.dma(SP, gv.t[:], gv_d, gv.ds, writes=[gv])
        T.dma(SP, ident_f.t[:], cst_d[:, 0:128], ident_f.ds, writes=[ident_f])
        T.dma(POOL, ident_b.t[:], cst_d[:, 0:128], ident_b.ds, writes=[ident_b])
        T.dma(POOL, trineg.t[:], cst_d[:, 128:256], trineg.ds, writes=[trineg])
        T.op(DVE, lambda: nc.vector.memset(ones_b.t[:], 1.0), writes=[ones_b])

        def gcol(c):
            return gv.t[:, c:c + 1]

        def MM(*a, **k):
            return nc.tensor.matmul(*a, **k) if T.live else None

        def MT(*a, **k):
            return nc.tensor.transpose(*a, **k) if T.live else None

        def cast_dma(buf, out_ap, in_ap, ncols, ds, chunk=512):
            nchunk = (ncols + chunk - 1) // chunk
            w = (ncols + nchunk - 1) // nchunk
            for a in range(0, ncols, w):
                b = min(ncols, a + w)
                T.dma(POOL, out_ap[:, a:b], in_ap[:, a:b], ds, writes=[buf])

        class Stream:
            def __init__(self, stack, shape, nslots, chunk=512):
                self.chunk = chunk
                self.slots = [sbt(stack, shape, BF, ds=True) for _ in range(nslots)]
                self.n = nslots
                self.jobs = []
                self.issued = 0
                self.base = 0

            def start(self, jobs):
                self.jobs = self.jobs + list(jobs)

            def _issue(self, k):
                sl = self.slots[k % self.n]
                cast_dma(sl, sl.t, self.jobs[k], sl.t.shape[1], sl.ds, chunk=self.chunk)

            def get(self, i):
                hi = min(i + self.n - 1, len(self.jobs) - 1)
                while self.issued <= hi:
                    self._issue(self.issued)
                    self.issued += 1
                return self.slots[i % self.n]

        def sq_and_sum(stack_bufs, src_ap_fn, src_bufs, ntiles, ssb, ncols=512):
            sqb = stack_bufs
            for i in range(ntiles):
                q = sqb[i % len(sqb)]
                T.op(ACT, lambda q=q, i=i: nc.scalar.activation(out=q.t[:, :ncols], in_=src_ap_fn(i), func=AF.Square),
                     reads=src_bufs, writes=[q])
                T.op(PE, lambda q=q, i=i: MM(ssb.t[:, :ncols], lhsT=ones_b.t[:], rhs=q.t[:, :ncols],
                                                          start=(i == 0), stop=(i == ntiles - 1)),
                     reads=[q, ones_b], writes=[ssb])

        def rstd_from(ssb, rt, rstd, dim, ncols=512):
            T.op(DVE, lambda: nc.vector.tensor_scalar(out=rt.t[:, :ncols], in0=ssb.t[:, :ncols], scalar1=1.0 / dim,
                                                      scalar2=EPS, op0=ALU.mult, op1=ALU.add),
                 reads=[ssb], writes=[rt])
            T.op(ACT, lambda: nc.scalar.activation(out=rt.t[:, :ncols], in_=rt.t[:, :ncols], func=AF.Sqrt),
                 reads=[rt], writes=[rt])
            T.op(DVE, lambda: nc.vector.reciprocal(out=rstd.t[:, :ncols], in_=rt.t[:, :ncols]),
                 reads=[rt], writes=[rstd])

        def prenorm(c0, g0, outb, ocol0, sqb, ssb, rt, rstd):
            sq_and_sum(sqb, lambda i: xT.t[:, i, c0:c0 + 512], [xT], 8, ssb)
            rstd_from(ssb, rt, rstd, 1024.0)
            for kc in range(8):
                T.op(DVE, lambda kc=kc: nc.vector.scalar_tensor_tensor(
                    out=outb.t[:, kc, ocol0:ocol0 + 512], in0=xT.t[:, kc, c0:c0 + 512], scalar=gcol(g0 + kc),
                    in1=rstd.t[:, :], op0=ALU.mult, op1=ALU.mult), reads=[xT, rstd, gv], writes=[outb])

        def down_project(stream, job0, nk, rhs_fn, rhs_bufs, fTs, c0s, gpost, half_scale, ybanks, ssbs, sqb, rt, rstd):
            pend = None
            step = 0
            ng = len(c0s)
            for dt in range(8):
                sl = stream.get(job0 + dt)
                for ti in range(ng):
                    yb = ybanks[step % 2]
                    q = sqb[step % 2]
                    step += 1
                    fT, ssb = fTs[ti], ssbs[ti]
                    T.pre(PE, reads=[sl] + rhs_bufs, writes=[yb])
                    for k in range(nk):
                        ins = MM(yb.t[:, :], lhsT=sl.t[:, k * 128:(k + 1) * 128], rhs=rhs_fn(k, ti),
                                               start=(k == 0), stop=(k == nk - 1))
                    T.post(PE, ins, reads=[sl] + rhs_bufs, writes=[yb])
                    if pend is not None:
                        pend()
                    T.op(ACT, lambda q=q, yb=yb: nc.scalar.activation(out=q.t[:, :], in_=yb.t[:, :], func=AF.Square),
                         reads=[yb], writes=[q])
                    T.op(DVE, lambda yb=yb, dt=dt, fT=fT: nc.vector.tensor_copy(out=fT.t[:, dt, :], in_=yb.t[:, :]),
                         reads=[yb], writes=[fT])

                    def pend(q=q, dt=dt, ssb=ssb):
                        T.op(PE, lambda: MM(ssb.t[:, :], lhsT=ones_b.t[:], rhs=q.t[:, :],
                                                          start=(dt == 0), stop=(dt == 7)),
                             reads=[q, ones_b], writes=[ssb])
            pend()
            T.mark('dp_finalize')
            for ti in range(ng):
                fT, c0 = fTs[ti], c0s[ti]
                rstd_from(ssbs[ti], rt, rstd, 1024.0)
                for dt in range(8):
                    T.op(DVE, lambda dt=dt, fT=fT: nc.vector.scalar_tensor_tensor(
                        out=fT.t[:, dt, :], in0=fT.t[:, dt, :], scalar=gcol(gpost + dt), in1=rstd.t[:, :],
                        op0=ALU.mult, op1=ALU.mult), reads=[fT, rstd, gv], writes=[fT])
                    T.op(DVE, lambda dt=dt, fT=fT, c0=c0: nc.vector.scalar_tensor_tensor(
                        out=xT.t[:, dt, c0:c0 + 512], in0=fT.t[:, dt, :], scalar=half_scale, in1=xT.t[:, dt, c0:c0 + 512],
                        op0=ALU.mult, op1=ALU.add), reads=[fT, xT], writes=[xT])

        def ffn(gpre, gpost, wgu_d, wd_d):
            with contextlib.ExitStack() as fs:
                xn = sbt(fs, [128, 8, 1024], BF, dj=True)
                actT = sbt(fs, [128, NFT, 1024], BF, dj=True)
                fTs = [sbt(fs, [128, 8, 512], F32) for _ in range(2)]
                sg = [sbt(fs, [128, 512], BF) for _ in range(2)]
                sqb = [sbt(fs, [128, 512], BF) for _ in range(2)]
                rt = sbt(fs, [128, 512], F32)
                rstd = sbt(fs, [128, 512], F32)
                SA = Stream(fs, [128, 2048], 4, chunk=1024)
                SB = Stream(fs, [128, DFF], 3, chunk=1408)
                for half in range(2):
                    SA.start([wgu_d[ft] for ft in range(NFT)])
                    SB.start([wd_d[dt] for dt in range(8)])
                rsn = [sbt(fs, [128, 512], F32) for _ in range(2)]

                def stats(hh, ssbanks):
                    for tg in range(2):
                        cc = hh + tg * 512
                        sq_and_sum(sqb, lambda i, cc=cc: xT.t[:, i, cc:cc + 512], [xT], 8, ssbanks[tg])
                        rstd_from(ssbanks[tg], rt, rsn[tg], 1024.0)

                def apply(hh):
                    for tg in range(2):
                        for kc in range(8):
                            T.op(DVE, lambda kc=kc, tg=tg: nc.vector.scalar_tensor_tensor(
                                out=xn.t[:, kc, tg * 512:(tg + 1) * 512], in0=xT.t[:, kc, hh + tg * 512:hh + (tg + 1) * 512],
                                scalar=gcol(gpre + kc), in1=rsn[tg].t[:, :], op0=ALU.mult, op1=ALU.mult),
                                reads=[xT, rsn[tg], gv], writes=[xn])

                SA.get(0)
                stats(0, [banks[6], banks[7]])
                apply(0)
                for half in range(2):
                    h0 = half * 1024
                    for ft in range(NFT):
                        sl = SA.get(half * NFT + ft)
                        if half == 0 and ft == 10:
                            stats(1024, [banks[4], banks[5]])
                        for tg in range(2):
                            hg, hu = banks[2 * tg], banks[2 * tg + 1]
                            T.pre(PE, reads=[sl, xn], writes=[hg, hu])
                            for kc in range(8):
                                MM(hg.t[:, :], lhsT=sl.t[:, kc * 128:(kc + 1) * 128],
                                   rhs=xn.t[:, kc, tg * 512:(tg + 1) * 512], start=(kc == 0), stop=(kc == 7))
                            for kc in range(8):
                                ins = MM(hu.t[:, :], lhsT=sl.t[:, 1024 + kc * 128:1024 + (kc + 1) * 128],
                                         rhs=xn.t[:, kc, tg * 512:(tg + 1) * 512], start=(kc == 0), stop=(kc == 7))
                            T.post(PE, ins, reads=[sl, xn], writes=[hg, hu])
                            s_ = sg[tg]
                            T.op(ACT, lambda s_=s_, hg=hg: nc.scalar.activation(out=s_.t[:, :], in_=hg.t[:, :], func=AF.Silu),
                                 reads=[hg], writes=[s_])
                            T.op(DVE, lambda s_=s_, hu=hu, ft=ft, tg=tg: nc.vector.tensor_tensor(
                                out=actT.t[:, ft, tg * 512:(tg + 1) * 512], in0=hu.t[:, :], in1=s_.t[:, :], op=ALU.mult),
                                reads=[hu, s_], writes=[actT])
                    if half == 0:
                        apply(1024)
                    down_project(SB, half * 8, NFT, lambda k, ti: actT.t[:, k, ti * 512:(ti + 1) * 512], [actT],
                                 fTs, [h0, h0 + 512], gpost, 0.5, [banks[4], banks[5]], [banks[6], banks[7]], sqb, rt, rstd)

        T.barrier()
        if STOP >= 1:
            ffn(G_F1PRE, G_F1POST, w1gu_d, w1d_d)
        T.barrier()

        with contextlib.ExitStack() as ms:
          if STOP >= 2:
            usT = sbt(ms, [128, 4, S], BF, dj=True)
            matt = sbt(ms, [128, 4, S], BF, dj=True)
            with contextlib.ExitStack() as as_:
                Kpk = sbt(as_, [128, 4, S], BF, dj=True)
                Kx = sbt(as_, [128, S], BF, ds=True)
                V = sbt(as_, [128, 16, 512], BF, dj=True)
                Qm = sbt(as_, [128, 8, 512], BF)
                Qx = sbt(as_, [128, 8, 512], BF, ds=True)
                un = sbt(as_, [128, 8, 512], BF, dj=True)
                ksum = sbt(as_, [128, 4, 8], F32)
                ksum_b = sbt(as_, [128, 4, 8], BF)
                Gsb = sbt(as_, [128, 32, 8], F32)
                M8 = sbt(as_, [128, 32, 8], F32)
                SEL = sbt(as_, [128, 32, 8], F32)
                mbias = sbt(as_, [128, 4, 64], F32)
                MTsb = sbt(as_, [64, 512], BF)
                pT = [sbt(as_, [128, 512], BF) for _ in range(3)]
                rden = sbt(as_, [128, 512], F32)
                sqb = [sbt(as_, [128, 512], BF) for _ in range(2)]
                rt = sbt(as_, [128, 512], F32)
                rstd = sbt(as_, [128, 512], F32)
                att = sbt(as_, [128, 4, 512], F32)
                WS = Stream(as_, [128, 1024], 4, chunk=1024)
                T.op(DVE, lambda: nc.vector.memset(ksum.t[:], 0.0), writes=[ksum])
                T.op(DVE, lambda: nc.vector.memset(Kx.t[:], 0.0), writes=[Kx])
                T.op(DVE, lambda: nc.vector.memset(Qx.t[:], 0.0), writes=[Qx])
                T.op(DVE, lambda: nc.vector.memset(Qm.t[:], 0.0), writes=[Qm])
                Qx_ds2 = T.newds()
                cast_dma(Kx, Kx.t[0:12, :], kaug_d, S, Kx.ds)
                for tg in range(4):
                    WS.start([win_d[n] for n in range(16)])
                for tg in range(4):
                    c0 = tg * 512
                    WS.get(tg * 16)
                    T.dma(POOL, Qx.t[8:12, :, :], qaug_d[:, :, c0:c0 + 512], Qx.ds, writes=[Qx])
                    prenorm(c0, G_MIXPRE, un, 0, sqb, banks[6], rt, rstd)
                    for n in range(8):
                        sl = WS.get(tg * 16 + n)
                        pb = banks[n % 2]
                        T.pre(PE, reads=[sl, un], writes=[pb])
                        for kc in range(8):
                            ins = MM(pb.t[:, :], lhsT=sl.t[:, kc * 128:(kc + 1) * 128], rhs=un.t[:, kc, :],
                                                   start=(kc == 0), stop=(kc == 7))
                        T.post(PE, ins, reads=[sl, un], writes=[pb])
                        if n < 4:
                            T.op(ACT, lambda pb=pb, n=n: nc.scalar.copy(out=Kpk.t[:, n, c0:c0 + 512], in_=pb.t[:, :]),
                                 reads=[pb], writes=[Kpk])
                            T.op(DVE, lambda pb=pb, n=n: nc.vector.tensor_reduce(
                                out=ksum.t[:, n, 2 * tg:2 * tg + 2], in_=pb.t[:, :].rearrange("p (a b) -> p a b", b=256),
                                axis=AX.X, op=ALU.add), reads=[pb], writes=[ksum])
                        else:
                            for e in range(2):
                                T.op(ACT, lambda pb=pb, n=n, e=e: nc.scalar.mul(out=Qm.t[64 * e:64 * e + 64, 2 * (n - 4) + e, :],
                                                                               in_=pb.t[64 * e:64 * e + 64, :], mul=0.125),
                                     reads=[pb], writes=[Qm])
                    if tg < 2:
                        T.op(DVE, lambda: nc.vector.memset(mbias.t[:], 0.0), writes=[mbias])
                    else:
                        T.op(DVE, lambda: nc.vector.tensor_copy(out=ksum_b.t[:], in_=ksum.t[:]), reads=[ksum], writes=[ksum_b])
                        gb = banks[7]
                        T.pre(PE, reads=[Qm, ksum_b], writes=[gb])
                        for qt in range(4):
                            for h in range(8):
                                e, hp = h % 2, h // 2
                                ins = MM(gb.t[:, (qt * 8 + h) * 8:(qt * 8 + h) * 8 + 8],
                                                       lhsT=Qm.t[:, h, qt * 128:(qt + 1) * 128],
                                                       rhs=ksum_b.t[:, hp, :], start=True, stop=True)
                        T.post(PE, ins, reads=[Qm, ksum_b], writes=[gb])
                        T.op(DVE, lambda: nc.vector.memset(Gsb.t[:], -1e30), writes=[Gsb])
                        for half2 in range(2):
                            nb = 2 * tg + half2
                            T.op(DVE, lambda half2=half2, nb=nb: nc.vector.tensor_copy(
                                out=Gsb.t[:, 16 * half2:16 * half2 + 16, 0:nb],
                                in_=gb.t[:, 128 * half2:128 * half2 + 128].rearrange("p (a b) -> p a b", b=8)[:, :, 0:nb]),
                                reads=[gb], writes=[Gsb])
                        for i in range(32):
                            T.op(DVE, lambda i=i: nc.vector.max(out=M8.t[:, i, :], in_=Gsb.t[:, i, :]), reads=[Gsb], writes=[M8])
                        T.op(DVE, lambda: nc.vector.tensor_tensor(out=SEL.t[:], in0=Gsb.t[:], in1=M8.t[:, :, 2:3].to_broadcast([128, 32, 8]),
                                                                  op=ALU.is_ge), reads=[Gsb, M8], writes=[SEL])
                        T.op(DVE, lambda: nc.vector.tensor_scalar(out=mbias.t[:].rearrange("p a (h n) -> p (a h) n", n=8), in0=SEL.t[:],
                                                                  scalar1=1.0, scalar2=BIG, op0=ALU.subtract, op1=ALU.mult),
                             reads=[SEL], writes=[mbias])
                        for half2 in range(2):
                            nb = 2 * tg + half2
                            T.op(DVE, lambda half2=half2, nb=nb: nc.vector.memset(
                                mbias.t[:, 2 * half2:2 * half2 + 2, :].rearrange("p a (h n) -> p a h n", n=8)[:, :, :, nb:nb + 1], 0.0),
                                writes=[mbias])
                    for c in range(4):
                        sl = WS.get(tg * 16 + 8 + c)
                        pb = banks[2 + c % 2]
                        T.pre(PE, reads=[sl, un], writes=[pb])
                        for tt in range(4):
                            for kc in range(8):
                                ins = MM(pb.t[:, tt * 128:(tt + 1) * 128], lhsT=un.t[:, kc, tt * 128:(tt + 1) * 128],
                                                       rhs=sl.t[:, kc * 128:(kc + 1) * 128], start=(kc == 0), stop=(kc == 7))
                        T.post(PE, ins, reads=[sl, un], writes=[pb])
                        T.op(DVE, lambda pb=pb, c=c: nc.vector.tensor_copy(
                            out=V.t[:, 4 * tg:4 * tg + 4, c * 128:(c + 1) * 128],
                            in_=pb.t[:, :].rearrange("p (a b) -> p a b", b=128)), reads=[pb], writes=[V])
                    for k in range(4):
                        sl = WS.get(tg * 16 + 12 + k)
                        pb = banks[4 + k % 2]
                        T.pre(PE, reads=[sl, un], writes=[pb])
                        for kc in range(8):
                            ins = MM(pb.t[:, :], lhsT=sl.t[:, kc * 128:(kc + 1) * 128], rhs=un.t[:, kc, :],
                                                   start=(kc == 0), stop=(kc == 7))
                        T.post(PE, ins, reads=[sl, un], writes=[pb])
                        T.op(ACT, lambda pb=pb, k=k: nc.scalar.copy(out=usT.t[:, k, c0:c0 + 512], in_=pb.t[:, :]),
                             reads=[pb], writes=[usT])
                    mb = banks[7]
                    T.pre(PE, reads=[mbias, ident_f], writes=[mb])
                    for qt in range(4):
                        ins = MT(mb.t[0:64, qt * 128:(qt + 1) * 128], mbias.t[:, qt, :], ident_f.t[:, :])
                    T.post(PE, ins, reads=[mbias, ident_f], writes=[mb])
                    T.op(ACT, lambda: nc.scalar.copy(out=MTsb.t[:, :], in_=mb.t[0:64, :]), reads=[mb], writes=[MTsb])
                    for h in range(8):
                        T.dma(SP, Qx.t[0:8, h, :], MTsb.t[8 * h:8 * h + 8, :], Qx_ds2, reads=[MTsb], writes=[Qx])
                    nkt = 4 * tg + 4
                    for hp in range(4):
                        numP, denP = banks[2 + hp % 2], banks[4 + hp % 2]
                        for e in range(2):
                            h = 2 * hp + e
                            rows = slice(64 * e, 64 * e + 64)
                            pend = None
                            for kt in range(nkt):
                                j = kt - 4 * tg
                                ql = 128 * j if j > 0 else 0
                                sP = banks[kt % 2]
                                rd = [Kpk, Qm, Kx, Qx, ident_b, trineg]
                                T.pre(PE, reads=rd, writes=[sP])
                                MM(sP.t[:, ql:512], lhsT=Kpk.t[:, hp, kt * 128:(kt + 1) * 128],
                                                 rhs=Qm.t[:, h, ql:512], start=True, stop=False)
                                ins = MM(sP.t[:, ql:512], lhsT=Kx.t[:, kt * 128:(kt + 1) * 128],
                                                       rhs=Qx.t[:, h, ql:512], start=False, stop=(j < 0))
                                if j >= 0:
                                    ins = MM(sP.t[:, ql:ql + 128], lhsT=ident_b.t[:, :], rhs=trineg.t[:, :],
                                                           start=False, stop=True)
                                T.post(PE, ins, reads=rd, writes=[sP])
                                p = pT[kt % 3]
                                T.op(ACT, lambda p=p, sP=sP, ql=ql: nc.scalar.activation(out=p.t[:, ql:512], in_=sP.t[:, ql:512], func=AF.Exp),
                                     reads=[sP], writes=[p])
                                if pend is not None:
                                    pend()

                                def pend(p=p, kt=kt, ql=ql):
                                    T.pre(PE, reads=[p, V, ones_b], writes=[numP, denP])
                                    MM(numP.t[rows, ql:512], lhsT=V.t[:, kt, h * 64:(h + 1) * 64], rhs=p.t[:, ql:512],
                                                     start=(kt == 0), stop=(kt == nkt - 1))
                                    ins2 = MM(denP.t[rows, ql:512], lhsT=ones_b.t[:, 0:64], rhs=p.t[:, ql:512],
                                                            start=(kt == 0), stop=(kt == nkt - 1))
                                    T.post(PE, ins2, reads=[p, V, ones_b], writes=[numP, denP])
                            pend()
                        T.op(DVE, lambda: nc.vector.reciprocal(out=rden.t[:, :], in_=denP.t[:, :]), reads=[denP], writes=[rden])
                        T.op(DVE, lambda: nc.vector.tensor_tensor(out=att.t[:, hp, :], in0=numP.t[:, :], in1=rden.t[:, :],
                                                                  op=ALU.mult), reads=[numP, rden], writes=[att])
                    sq_and_sum(sqb, lambda i: att.t[:, i, :], [att], 4, banks[6])
                    rstd_from(banks[6], rt, rstd, 512.0)
                    for hp in range(4):
                        T.op(DVE, lambda hp=hp: nc.vector.scalar_tensor_tensor(
                            out=matt.t[:, hp, c0:c0 + 512], in0=att.t[:, hp, :], scalar=gcol(G_ATT + hp), in1=rstd.t[:, :],
                            op0=ALU.mult, op1=ALU.mult), reads=[att, rstd, gv], writes=[matt])
            T.barrier()
            if STOP >= 3:
             with contextlib.ExitStack() as ss_:
                CS = sbt(ss_, [128, 2, 16, 128], BF)
                BB = sbt(ss_, [128, 16, 2, 128], BF)
                CC = sbt(ss_, [128, 16, 3, 64], BF)
                DDm = sbt(ss_, [128, 512], BF, ds=True)
                wglu = sbt(ss_, [128, 2048], BF, ds=True)
                mag_s = sbt(ss_, [128, 16], F32)
                ECS = sbt(ss_, [128, 2, 16], F32)
                Hc = sbt(ss_, [128, 2, 16], F32)
                Hm = sbt(ss_, [128, 2, 16], F32)
                magT = sbt(ss_, [128, 16, 128], F32)
                cast_dma(DDm, DDm.t, dlay_d, 512, DDm.ds)
                cast_dma(wglu, wglu.t, wglu_d, 2048, wglu.ds)

                def sincos(stk, ang, n, sinb, cosb, tmpF, tmpI):
                    T.op(DVE, lambda: nc.vector.tensor_scalar(out=tmpI.t[:, :n], in0=ang.t[:, :n], scalar1=1.0 / (2 * PI), scalar2=None,
                                                              op0=ALU.mult), reads=[ang], writes=[tmpI])
                    T.op(DVE, lambda: nc.vector.tensor_copy(out=tmpF.t[:, :n], in_=tmpI.t[:, :n]), reads=[tmpI], writes=[tmpF])
                    T.op(DVE, lambda: nc.vector.scalar_tensor_tensor(out=tmpF.t[:, :n], in0=tmpF.t[:, :n], scalar=-2 * PI, in1=ang.t[:, :n],
                                                                     op0=ALU.mult, op1=ALU.add), reads=[tmpF, ang], writes=[tmpF])
                    T.op(DVE, lambda: nc.vector.tensor_scalar(out=tmpF.t[:, :n], in0=tmpF.t[:, :n], scalar1=-PI, scalar2=PI,
                                                              op0=ALU.max, op1=ALU.min), reads=[tmpF], writes=[tmpF])
                    T.op(ACT, lambda: nc.scalar.activation(out=sinb.t[:, :n], in_=tmpF.t[:, :n], func=AF.Sin), reads=[tmpF], writes=[sinb])
                    T.op(ACT, lambda: nc.scalar.activation(out=tmpF.t[:, :n], in_=tmpF.t[:, :n], func=AF.Abs), reads=[tmpF, sinb], writes=[tmpF])
                    T.op(DVE, lambda: nc.vector.tensor_scalar(out=tmpF.t[:, :n], in0=tmpF.t[:, :n], scalar1=-1.0, scalar2=PI / 2,
                                                              op0=ALU.mult, op1=ALU.add), reads=[tmpF], writes=[tmpF])
                    T.op(ACT, lambda: nc.scalar.activation(out=cosb.t[:, :n], in_=tmpF.t[:, :n], func=AF.Sin), reads=[tmpF], writes=[cosb])

                TT = lambda o, a, b, op, rd, wr: T.op(DVE, lambda: nc.vector.tensor_tensor(out=o, in0=a, in1=b, op=op), reads=rd, writes=wr)
                with contextlib.ExitStack() as st:
                    lamst = sbt(st, [128, 48], F32, ds=True)
                    iota = sbt(st, [128, 128], F32, ds=True)
                    clay = sbt(st, [128, 2 * 1024], F32, ds=True)
                    T.dma(SP, lamst.t[:], lamst_d, lamst.ds, writes=[lamst])
                    T.dma(SP, iota.t[:], cst_d[:, 256:384], iota.ds, writes=[iota])
                    T.dma(SP, clay.t[:], clay_d, clay.ds, writes=[clay])
                    W = [sbt(st, [128, 2048], F32) for _ in range(3)]
                    WI = sbt(st, [128, 2048], I32)
                    sS = sbt(st, [128, 2048], F32)
                    sC = sbt(st, [128, 2048], F32)
                    dts, th_s, e1 = W[0], W[1], W[2]
                    T.op(ACT, lambda: nc.scalar.activation(out=dts.t[:, :16], in_=lamst.t[:, 32:48], func=AF.Exp), reads=[lamst], writes=[dts])
                    TT(e1.t[:, :16], lamst.t[:, 0:16], dts.t[:, :16], ALU.mult, [lamst, dts], [e1])
                    T.op(ACT, lambda: nc.scalar.activation(out=mag_s.t[:, :], in_=e1.t[:, :16], func=AF.Exp), reads=[e1], writes=[mag_s])
                    TT(th_s.t[:, :16], lamst.t[:, 16:32], dts.t[:, :16], ALU.mult, [lamst, dts], [th_s])
                    for j in range(16):
                        T.op(DVE, lambda j=j: nc.vector.tensor_scalar(out=magT.t[:, j, :], in0=iota.t[:, :], scalar1=0.0,
                                                                      scalar2=mag_s.t[:, j:j + 1], op0=ALU.mult, op1=ALU.add),
                             reads=[iota, mag_s], writes=[magT])
                    T.op(DVE, lambda: nc.vector.memset(magT.t[:, :, 0:1], 0.0), writes=[magT])
                    PH = W[2]
                    for j in range(16):
                        T.op(DVE, lambda j=j: nc.vector.tensor_scalar(out=PH.t[:, j * 128:(j + 1) * 128], in0=iota.t[:, :],
                                                                      scalar1=th_s.t[:, j:j + 1], scalar2=None, op0=ALU.mult),
                             reads=[iota, th_s], writes=[PH])
                    sincos(st, PH, 2048, sS, sC, W[0], WI)
                    j3 = lambda ap: ap.rearrange("p (j t) -> p j t", t=128)
                    T.op(DVE, lambda: nc.vector.tensor_copy(out=CS.t[:, 0, :, :], in_=j3(sC.t[:, :])), reads=[sC], writes=[CS])
                    T.op(DVE, lambda: nc.vector.tensor_copy(out=CS.t[:, 1, :, :], in_=j3(sS.t[:, :])), reads=[sS], writes=[CS])
                    T.op(DVE, lambda: nc.vector.tensor_copy(out=ECS.t[:, 0, :], in_=j3(sC.t[:, :])[:, :, 127]), reads=[sC], writes=[ECS])
                    T.op(DVE, lambda: nc.vector.tensor_copy(out=ECS.t[:, 1, :], in_=j3(sS.t[:, :])[:, :, 127]), reads=[sS], writes=[ECS])
                    c3 = lambda ap: ap.rearrange("p (j c) -> p j c", c=64)
                    T.op(DVE, lambda: nc.vector.tensor_copy(out=CC.t[:, :, 0, :], in_=c3(clay.t[:, 0:1024])), reads=[clay], writes=[CC])
                    T.op(DVE, lambda: nc.vector.tensor_scalar(out=CC.t[:, :, 1, :], in0=c3(clay.t[:, 0:1024]), scalar1=-1.0, scalar2=None,
                                                              op0=ALU.mult), reads=[clay], writes=[CC])
                    T.op(DVE, lambda: nc.vector.tensor_scalar(out=CC.t[:, :, 2, :], in0=c3(clay.t[:, 1024:2048]), scalar1=-1.0, scalar2=None,
                                                              op0=ALU.mult), reads=[clay], writes=[CC])
                T.barrier()
                with contextlib.ExitStack() as st:
                    NH = 1024
                    lamh = sbt(st, [128, 3 * NH], F32, ds=True)
                    blh = sbt(st, [128, 2 * NH], F32, ds=True)
                    W = [sbt(st, [128, NH], F32) for _ in range(7)]
                    WI = sbt(st, [128, NH], I32)
                    sS = sbt(st, [128, NH], F32)
                    sC = sbt(st, [128, NH], F32)
                    for hf in range(2):
                        for q in range(3):
                            T.dma(SP, lamh.t[:, q * NH:(q + 1) * NH], lambc_d[:, q * 2048 + hf * NH:q * 2048 + (hf + 1) * NH], lamh.ds, writes=[lamh])
                        for q in range(2):
                            T.dma(SP, blh.t[:, q * NH:(q + 1) * NH], blay_d[:, q * 2048 + hf * NH:q * 2048 + (hf + 1) * NH], blh.ds, writes=[blh])
                        lr, li, ldt = lamh.t[:, 0:NH], lamh.t[:, NH:2 * NH], lamh.t[:, 2 * NH:3 * NH]
                        dtb, thb, magb = W[0], W[1], W[2]
                        T.op(ACT, lambda: nc.scalar.activation(out=dtb.t[:, :], in_=ldt, func=AF.Exp), reads=[lamh], writes=[dtb])
                        TT(magb.t[:, :], lr, dtb.t[:, :], ALU.mult, [lamh, dtb], [magb])
                        T.op(ACT, lambda: nc.scalar.activation(out=magb.t[:, :], in_=magb.t[:, :], func=AF.Exp), reads=[magb], writes=[magb])
                        TT(thb.t[:, :], li, dtb.t[:, :], ALU.mult, [lamh, dtb], [thb])
                        sincos(st, thb, NH, sS, sC, W[4], WI)
                        are, aim, den, fre, fim, tmp = W[3], W[4], W[5], W[6], W[0], W[1]
                        TT(are.t[:, :], magb.t[:, :], sC.t[:, :], ALU.mult, [magb, sC], [are])
                        TT(aim.t[:, :], magb.t[:, :], sS.t[:, :], ALU.mult, [magb, sS], [aim])
                        T.op(DVE, lambda: nc.vector.tensor_scalar(out=are.t[:, :], in0=are.t[:, :], scalar1=-1.0, scalar2=None, op0=ALU.add),
                             reads=[are], writes=[are])
                        TT(den.t[:, :], lr, lr, ALU.mult, [lamh], [den])
                        TT(tmp.t[:, :], li, li, ALU.mult, [lamh], [tmp])
                        TT(den.t[:, :], den.t[:, :], tmp.t[:, :], ALU.add, [den, tmp], [den])
                        T.op(DVE, lambda: nc.vector.reciprocal(out=den.t[:, :], in_=den.t[:, :]), reads=[den], writes=[den])
                        TT(fre.t[:, :], are.t[:, :], lr, ALU.mult, [are, lamh], [fre])
                        TT(tmp.t[:, :], aim.t[:, :], li, ALU.mult, [aim, lamh], [tmp])
                        TT(fre.t[:, :], fre.t[:, :], tmp.t[:, :], ALU.add, [fre, tmp], [fre])
                        TT(fre.t[:, :], fre.t[:, :], den.t[:, :], ALU.mult, [fre, den], [fre])
                        TT(fim.t[:, :], aim.t[:, :], lr, ALU.mult, [aim, lamh], [fim])
                        TT(tmp.t[:, :], are.t[:, :], li, ALU.mult, [are, lamh], [tmp])
                        TT(fim.t[:, :], fim.t[:, :], tmp.t[:, :], ALU.subtract, [fim, tmp], [fim])
                        TT(fim.t[:, :], fim.t[:, :], den.t[:, :], ALU.mult, [fim, den], [fim])
                        bre, bim = blh.t[:, 0:NH], blh.t[:, NH:2 * NH]
                        t2, t3 = W[2], W[3]
                        v3 = lambda ap: ap.rearrange("p (j c) -> p j c", c=128)
                        js = slice(8 * hf, 8 * hf + 8)
                        TT(t2.t[:, :], fre.t[:, :], bre, ALU.mult, [fre, blh], [t2])
                        TT(t3.t[:, :], fim.t[:, :], bim, ALU.mult, [fim, blh], [t3])
                        TT(BB.t[:, js, 0, :], v3(t2.t[:, :]), v3(t3.t[:, :]), ALU.subtract, [t2, t3], [BB])
                        TT(t2.t[:, :], fre.t[:, :], bim, ALU.mult, [fre, blh], [t2])
                        TT(t3.t[:, :], fim.t[:, :], bre, ALU.mult, [fim, blh], [t3])
                        TT(BB.t[:, js, 1, :], v3(t2.t[:, :]), v3(t3.t[:, :]), ALU.add, [t2, t3], [BB])
                T.barrier()
                TTq = sbt(ss_, [128, 4, 4, 128], BF)
                XTq = sbt(ss_, [128, 2, 4, 128], BF)
                Gq = sbt(ss_, [128, 2, 4, 128], BF)
                Pq = [sbt(ss_, [128, 4, 4, 128], BF) for _ in range(2)]
                ctmp = sbt(ss_, [128, 4, 4], F32)
                ygf = sbt(ss_, [128, 4, 512], F32)
                ygb = sbt(ss_, [128, 4, 512], BF)
                sig = sbt(ss_, [128, 512], F32)
                mssm = sbt(ss_, [128, 4, 512], BF)
                fT = sbt(ss_, [128, 8, 512], F32)
                sqb = [sbt(ss_, [128, 512], BF) for _ in range(2)]
                rt = sbt(ss_, [128, 512], F32)
                rstd = sbt(ss_, [128, 512], F32)
                WO = Stream(ss_, [128, 1024], 3, chunk=1024)
                for tg in range(4):
                    WO.start([wout_d[dt] for dt in range(8)])
                TTqs = [TTq, sbt(ss_, [128, 4, 4, 128], BF)]
                XTqs = [XTq, sbt(ss_, [128, 2, 4, 128], BF)]
                Gqs = [Gq, sbt(ss_, [128, 2, 4, 128], BF)]
                Hc.dj = True
                Hm.dj = True

                def quad_thunks(c, cc, t0, k, par):
                    XA, XB = banks[2 * par], banks[2 * par + 1]
                    P, TT_, XT_, G_ = Pq[par], TTqs[par], XTqs[par], Gqs[par]
                    q4 = slice(4 * k, 4 * k + 4)
                    xa = XA.t[:, :].rearrange("p (a b) -> p a b", b=128)
                    xb = XB.t[:, :].rearrange("p (a b) -> p a b", b=128)
                    cosq, sinq = CS.t[:, 0, q4, :], CS.t[:, 1, q4, :]
                    th = []

                    def pe_x():
                        T.pre(PE, reads=[BB, usT], writes=[XA, XB])
                        for jl in range(4):
                            j = 4 * k + jl
                            MM(XA.t[:, jl * 128:(jl + 1) * 128], lhsT=BB.t[:, j, 0, :], rhs=usT.t[:, k, t0:t0 + 128], start=True, stop=True)
                            ins = MM(XB.t[:, jl * 128:(jl + 1) * 128], lhsT=BB.t[:, j, 1, :], rhs=usT.t[:, k, t0:t0 + 128], start=True, stop=True)
                        T.post(PE, ins, reads=[BB, usT], writes=[XA, XB])
                    th.append(pe_x)

                    def tt(out, in0, in1, op, rd, wr):
                        th.append(lambda: T.op(DVE, lambda: nc.vector.tensor_tensor(out=out, in0=in0, in1=in1, op=op), reads=rd, writes=wr))
                    tt(TT_.t[:, 0], xa, cosq, ALU.mult, [XA, CS], [TT_])
                    tt(TT_.t[:, 1], xb, sinq, ALU.mult, [XB, CS], [TT_])
                    tt(TT_.t[:, 2], xb, cosq, ALU.mult, [XB, CS], [TT_])
                    tt(TT_.t[:, 3], xa, sinq, ALU.mult, [XA, CS], [TT_])
                    tt(XT_.t[:, 0], TT_.t[:, 0], TT_.t[:, 1], ALU.add, [TT_], [XT_])
                    tt(XT_.t[:, 1], TT_.t[:, 2], TT_.t[:, 3], ALU.subtract, [TT_], [XT_])
                    if c > 0:
                        tt(XT_.t[:, :, :, 0], XT_.t[:, :, :, 0], Hm.t[:, :, q4], ALU.add, [XT_, Hm], [XT_])
                    for r in range(2):
                        th.append(lambda r=r: T.op(DVE, lambda: nc.vector.tensor_tensor_scan(
                            out=G_.t[:, r, :, :].rearrange("p a b -> p (a b)"), data0=magT.t[:, q4, :].rearrange("p a b -> p (a b)"),
                            data1=XT_.t[:, r, :, :].rearrange("p a b -> p (a b)"), initial=0.0, op0=ALU.mult, op1=ALU.add),
                            reads=[XT_, magT], writes=[G_]))
                    tt(P.t[:, 0:2], G_.t[:, 0:2], CS.t[:, 0:2, q4, :], ALU.mult, [G_, CS], [P])
                    tt(P.t[:, 2], G_.t[:, 1], cosq, ALU.mult, [G_, CS], [P])
                    tt(P.t[:, 3], G_.t[:, 0], sinq, ALU.mult, [G_, CS], [P])
                    tt(Hc.t[:, 0, q4], P.t[:, 0, :, 127], P.t[:, 1, :, 127], ALU.subtract, [P], [Hc])
                    tt(Hc.t[:, 1, q4], P.t[:, 2, :, 127], P.t[:, 3, :, 127], ALU.add, [P], [Hc])
                    for r in range(2):
                        tt(Hm.t[:, r, q4], Hc.t[:, r, q4], mag_s.t[:, q4], ALU.mult, [Hc, mag_s], [Hm])

                    def pe_c():
                        Y = banks[4 + k]
                        rd = [CC, P, DDm, usT]
                        T.pre(PE, reads=rd, writes=[Y])
                        for e in range(2):
                            rows = slice(64 * e, 64 * e + 64)
                            yo = Y.t[rows, cc * 128:(cc + 1) * 128]
                            first = True
                            for jl in (2 * e, 2 * e + 1):
                                j = 4 * k + jl
                                for (ci, pi_) in ((0, 0), (1, 1), (2, 2), (2, 3)):
                                    MM(yo, lhsT=CC.t[:, j, ci, :], rhs=P.t[:, pi_, jl, :], start=first, stop=False)
                                    first = False
                            ins = MM(yo, lhsT=DDm.t[:, k * 128 + 64 * e:k * 128 + 64 * e + 64], rhs=usT.t[:, k, t0:t0 + 128], start=False, stop=True)
                        T.post(PE, ins, reads=rd, writes=[Y])
                    th.append(pe_c)
                    return th

                for tg in range(4):
                    c0 = tg * 512
                    for cc in range(4):
                        c = 4 * tg + cc
                        t0 = c0 + 128 * cc
                        for kp in (0, 2):
                            A_ = quad_thunks(c, cc, t0, kp, 0)
                            B_ = quad_thunks(c, cc, t0, kp + 1, 1)
                            for fa, fb in zip(A_, B_):
                                fa()
                                fb()
                    for k in range(4):
                        Y = banks[4 + k]
                        T.op(ACT, lambda k=k, Y=Y: nc.scalar.activation(out=ygf.t[:, k, :], in_=Y.t[:, :], func=AF.Gelu_apprx_tanh),
                             reads=[Y], writes=[ygf])
                        T.op(DVE, lambda k=k: nc.vector.tensor_copy(out=ygb.t[:, k, :], in_=ygf.t[:, k, :]), reads=[ygf], writes=[ygb])
                    for ko in range(4):
                        GP = banks[ko % 2]
                        T.pre(PE, reads=[wglu, ygb], writes=[GP])
                        for ki in range(4):
                            ins = MM(GP.t[:, :], lhsT=wglu.t[:, ki * 512 + ko * 128:ki * 512 + (ko + 1) * 128], rhs=ygb.t[:, ki, :],
                                                   start=(ki == 0), stop=(ki == 3))
                        T.post(PE, ins, reads=[wglu, ygb], writes=[GP])
                        T.op(ACT, lambda GP=GP, ko=ko: nc.scalar.activation(out=sig.t[:, :], in_=GP.t[:, :], func=AF.Sigmoid,
                                                                          bias=gcol(G_BGLU + ko)), reads=[GP, gv], writes=[sig])
                        T.op(DVE, lambda ko=ko: nc.vector.tensor_tensor(out=ygf.t[:, ko, :], in0=ygf.t[:, ko, :], in1=sig.t[:, :], op=ALU.mult),
                             reads=[ygf, sig], writes=[ygf])
                    sq_and_sum(sqb, lambda i: ygf.t[:, i, :], [ygf], 4, banks[2])
                    rstd_from(banks[2], rt, rstd, 512.0)
                    for ko in range(4):
                        T.op(DVE, lambda ko=ko: nc.vector.scalar_tensor_tensor(
                            out=mssm.t[:, ko, :], in0=ygf.t[:, ko, :], scalar=gcol(G_SSM + ko), in1=rstd.t[:, :],
                            op0=ALU.mult, op1=ALU.mult), reads=[ygf, rstd, gv], writes=[mssm])
                    down_project(WO, tg * 8, 8,
                                 lambda m, ti, c0=c0: (matt.t[:, m, c0:c0 + 512] if m < 4 else mssm.t[:, m - 4, :]), [matt, mssm],
                                 [fT], [c0], G_MIXPOST, 1.0, [banks[0], banks[1]], [banks[3]], sqb, rt, rstd)
            T.barrier()

        if STOP >= 4:
            ffn(G_F2PRE, G_F2POST, w2gu_d, w2d_d)
        T.barrier()
        T.live = True
        T.cut = 10 ** 9
        T.barrier()
        od = T.newds()
        T.dma(SP, out_d, xT.t[:], od, reads=[xT])
        SP.e.wait_ge(od.sem, od.cnt)
    return nc


def _shared_layouts(inp):
    f = np.float32
    A = lambda k: np.ascontiguousarray(np.asarray(inp[k], dtype=f)[0])
    o = {}
    gv = np.zeros((128, 64), f)

    def put(c0, v):
        n = v.shape[0] // 128
        gv[:, c0:c0 + n] = v.reshape(n, 128).T
    put(G_F1PRE, A("ffn1_pre_g")); put(G_F1POST, A("ffn1_post_g")); put(G_MIXPRE, A("mix_pre_g"))
    put(G_ATT, A("attn_out_g")); put(G_SSM, A("ssm_out_g")); put(G_MIXPOST, A("mix_post_g"))
    put(G_F2PRE, A("ffn2_pre_g")); put(G_F2POST, A("ffn2_post_g")); put(G_BGLU, A("b_glu"))
    o["gv"] = gv

    def gu(wg, wu):
        a = wg.reshape(8, 128, NFT, 128).transpose(2, 1, 0, 3)
        b = wu.reshape(8, 128, NFT, 128).transpose(2, 1, 0, 3)
        return np.ascontiguousarray(np.concatenate([a, b], axis=2).reshape(NFT, 128, 2048))

    def dn(wd):
        return np.ascontiguousarray(wd.reshape(NFT, 128, 8, 128).transpose(2, 1, 0, 3).reshape(8, 128, DFF))
    o["w1gu"] = gu(A("ffn1_w_gate"), A("ffn1_w_up")); o["w1d"] = dn(A("ffn1_w_down"))
    o["w2gu"] = gu(A("ffn2_w_gate"), A("ffn2_w_up")); o["w2d"] = dn(A("ffn2_w_down"))
    win = A("w_in")
    order = [512 + 128 * i for i in range(4)] + [128 * i for i in range(4)] + [1024 + 128 * i for i in range(8)]
    o["win"] = np.ascontiguousarray(np.stack([win[:, c:c + 128].reshape(8, 128, 128).transpose(1, 0, 2).reshape(128, 1024) for c in order]))
    o["wglu"] = np.ascontiguousarray(A("w_glu").reshape(4, 128, 512).transpose(1, 0, 2).reshape(128, 2048))
    wo = A("w_out")
    o["wout"] = np.ascontiguousarray(wo.reshape(8, 128, 8, 128).transpose(2, 1, 0, 3).reshape(8, 128, 1024))
    lr, li, ld = A("lam_re"), A("lam_im"), A("log_dt")
    st = lambda a: a.reshape(16, 2, 64).transpose(1, 2, 0).reshape(128, 16)
    ldf = np.repeat(ld[:, None], 64, axis=1)
    o["lamst"] = np.ascontiguousarray(np.concatenate([st(lr), st(li), st(ldf)], axis=1))
    bc = np.concatenate([lr.reshape(-1), li.reshape(-1), ldf.reshape(-1)])
    o["lambc"] = np.ascontiguousarray(np.broadcast_to(bc[None, :], (128, 6144)))
    bre, bim = A("b_re"), A("b_im")
    bl = np.zeros((2, 128, 16, 128), f)
    cre, cim = A("c_re"), A("c_im")
    cl = np.zeros((2, 128, 16, 64), f)
    for j in range(16):
        q = j % 4
        for g2 in range(2):
            g = 2 * j + g2
            r0 = 32 * q + 16 * g2
            bl[0, r0:r0 + 16, j, 64 * g2:64 * g2 + 64] = bre[g].T
            bl[1, r0:r0 + 16, j, 64 * g2:64 * g2 + 64] = bim[g].T
            cc0 = 32 * (j % 2) + 16 * g2
            cl[0, 64 * g2:64 * g2 + 64, j, cc0:cc0 + 16] = cre[g].T
            cl[1, 64 * g2:64 * g2 + 64, j, cc0:cc0 + 16] = cim[g].T
    o["blay"] = np.ascontiguousarray(bl.transpose(1, 0, 2, 3).reshape(128, 4096))
    o["clay"] = np.ascontiguousarray(cl.transpose(1, 0, 2, 3).reshape(128, 2048))
    dsk = A("d_skip").reshape(512)
    dl = np.zeros((128, 4, 128), f)
    for k in range(4):
        dl[np.arange(128), k, np.arange(128)] = dsk[k * 128:(k + 1) * 128]
    o["dlay"] = dl.reshape(128, 512)
    pos = np.arange(S)
    ka = np.zeros((12, S), f)
    ka[pos // 256, pos] = 1.0
    ka[8] = 1.0; ka[9] = 1.0; ka[10] = 128.0 * (pos // 128); ka[11] = pos % 128
    o["kaugc"] = ka
    qa = np.zeros((4, 8, S), f)
    for h in range(8):
        sl = 2.0 ** (-(h + 1))
        qa[0, h] = -sl * 128.0 * (pos // 128); qa[1, h] = -sl * (pos % 128); qa[2, h] = sl; qa[3, h] = sl
    o["qaugc"] = qa
    cst = np.zeros((128, 384), f)
    cst[:, 0:128] = np.eye(128, dtype=f)
    kk, qq = np.meshgrid(np.arange(128), np.arange(128), indexing="ij")
    cst[:, 128:256] = np.where(kk > qq, -BIG, 0.0)
    cst[:, 256:384] = np.arange(1, 129, dtype=f)[None, :]
    o["cst"] = cst
    return o


_NC = None


def kernel(**inputs):
    global _NC
    x = np.asarray(inputs["x"], dtype=np.float32)
    nb = x.shape[0]
    sh = _shared_layouts(inputs)
    in_maps = []
    for b in range(nb):
        m = dict(sh)
        m["xT"] = np.ascontiguousarray(x[b].T.reshape(8, 128, S).transpose(1, 0, 2))
        in_maps.append(m)
    if _NC is None:
        _NC = build()
    res = run_bass_kernel_spmd(_NC, in_maps, core_ids=list(range(nb)))
    out = np.empty_like(x)
    for b in range(nb):
        yT = np.asarray(res.results[b]["yT"], dtype=np.float32)
        out[b] = yT.transpose(1, 0, 2).reshape(1024, S).T
    return out
```

```python
import contextlib
import math
import numpy as np
import concourse.bass as bass
import concourse.mybir as mybir
from concourse.bass_utils import run_bass_kernel_spmd

F32 = mybir.dt.float32
BF = mybir.dt.bfloat16
I32 = mybir.dt.int32
AF = mybir.ActivationFunctionType
ALU = mybir.AluOpType
AX = mybir.AxisListType
PI = math.pi
S = 2048
DFF = 2816
NFT = 22
EPS = 1e-6
BIG = 30000.0

G_F1PRE, G_F1POST, G_MIXPRE, G_ATT, G_SSM, G_MIXPOST, G_F2PRE, G_F2POST, G_BGLU = 0, 8, 16, 24, 28, 32, 40, 48, 56


class Eng:
    def __init__(self, nc, e, name, es):
        self.e, self.name = e, name
        self.sem = es.enter_context(nc.semaphore("s_" + name))
        self.cnt = 0
        self.seen = {}


class DS:
    def __init__(self, nc, name, es):
        self.sem = es.enter_context(nc.semaphore(name))
        self.cnt = 0
        self.name = name


class Buf:
    def __init__(self, t, ds=None, dj=False, psum=False):
        self.psum = psum
        self.dj = dj
        self.t = t
        self.w = None
        self.r = {}
        self.ds = ds


class Trk:
    def __init__(self, nc, es):
        self.nc = nc
        self.es = es
        self.PE = Eng(nc, nc.tensor, "pe", es)
        self.ACT = Eng(nc, nc.scalar, "act", es)
        self.DVE = Eng(nc, nc.vector, "dve", es)
        self.POOL = Eng(nc, nc.gpsimd, "pool", es)
        self.SP = Eng(nc, nc.sync, "sp", es)
        self.engs = [self.PE, self.ACT, self.DVE, self.POOL, self.SP]
        self.dss = []
        self.nds = 0
        self.n = 0
        self.live = True
        self.cut = CUT

    def newds(self):
        self.nds += 1
        d = DS(self.nc, "d%d" % self.nds, self.es)
        self.dss.append(d)
        return d

    def wait(self, E, tok):
        src, val = tok
        if src is E and E is self.PE:
            return
        k = id(src)
        if E.seen.get(k, 0) >= val:
            return
        E.e.wait_ge(src.sem, val)
        E.seen[k] = val

    def pre(self, E, reads=(), writes=()):
        if self.n >= self.cut:
            self.live = False
        if not self.live:
            return
        for b in reads:
            if b.w is not None:
                self.wait(E, b.w)
            if b.psum:
                for src, val in list(b.r.values()):
                    if src is not E:
                        self.wait(E, (src, val))
        for b in writes:
            if b.w is not None and (b.w[0] is not E or not b.dj):
                self.wait(E, b.w)
            for src, val in list(b.r.values()):
                self.wait(E, (src, val))

    def _reg(self, tok, reads, writes):
        for b in reads:
            b.r[id(tok[0])] = tok
        for b in writes:
            b.w = tok
            b.r = {}

    def post(self, E, inst, reads=(), writes=()):
        if not self.live:
            return
        self.n += 1
        E.cnt += 1
        inst.then_inc(E.sem, 1)
        self._reg((E, E.cnt), reads, writes)

    def op(self, E, fn, reads=(), writes=()):
        self.pre(E, reads, writes)
        if not self.live:
            return
        inst = fn()
        self.post(E, inst, reads, writes)

    def dma(self, Q, out, in_, ds, reads=(), writes=(), **kw):
        self.pre(Q, reads, writes)
        if not self.live:
            return
        self.n += 1
        inst = Q.e.dma_start(out=out, in_=in_, **kw)
        ds.cnt += 16
        inst.then_inc(ds.sem, 16)
        self._reg((ds, ds.cnt), reads, writes)

    def mark(self, name):
        MARKS.append((name, self.n))

    def barrier(self):
        if not self.live:
            return
        for E in self.engs:
            for Fe in self.engs:
                if Fe is not E and Fe.cnt > 0:
                    self.wait(E, (Fe, Fe.cnt))
            for d in self.dss:
                if d.cnt > 0:
                    self.wait(E, (d, d.cnt))


import os
STOP = int(os.environ.get('KSTOP', '99'))
CUT = int(os.environ.get('KCUT', str(10 ** 9)))
MARKS = []


def build():
    nc = bass.Bass("TRN2", target_bir_lowering=False)

    def D(name, shape, kind="ExternalInput", dt=F32):
        return nc.dram_tensor(name, shape, dt, kind=kind).ap()

    xT_d = D("xT", [128, 8, S])
    gv_d = D("gv", [128, 64])
    w1gu_d = D("w1gu", [NFT, 128, 2048])
    w1d_d = D("w1d", [8, 128, DFF])
    w2gu_d = D("w2gu", [NFT, 128, 2048])
    w2d_d = D("w2d", [8, 128, DFF])
    win_d = D("win", [16, 128, 1024])
    wglu_d = D("wglu", [128, 2048])
    wout_d = D("wout", [8, 128, 1024])
    lamst_d = D("lamst", [128, 48])
    lambc_d = D("lambc", [128, 3 * 2048])
    blay_d = D("blay", [128, 2 * 2048])
    clay_d = D("clay", [128, 2 * 1024])
    dlay_d = D("dlay", [128, 512])
    kaug_d = D("kaugc", [12, S])
    qaug_d = D("qaugc", [4, 8, S])
    cst_d = D("cst", [128, 3 * 128])
    out_d = D("yT", [128, 8, S], kind="ExternalOutput")

    es = contextlib.ExitStack()
    with es:
        T = Trk(nc, es)
        PE, ACT, DVE, POOL, SP = T.PE, T.ACT, T.DVE, T.POOL, T.SP
        cnt = [0]

        def sbt(stack, shape, dt, ds=False, dj=False):
            cnt[0] += 1
            t = stack.enter_context(nc.sbuf_tensor("t%d" % cnt[0], shape, dt))
            return Buf(t, T.newds() if ds else None, dj)

        xT = sbt(es, [128, 8, S], F32, ds=True)
        gv = sbt(es, [128, 64], F32, ds=True)
        ident_f = sbt(es, [128, 128], F32, ds=True)
        ident_b = sbt(es, [128, 128], BF, ds=True)
        trineg = sbt(es, [128, 128], BF, ds=True)
        ones_b = sbt(es, [128, 128], BF)
        banks = []
        for i in range(8):
            banks.append(Buf(es.enter_context(nc.psum_tensor("bank%d" % i, [128, 512], F32)), psum=True))

        T.dma(SP, xT.t[:], xT_d, xT.ds, writes=[xT])
        T.dma(SP, gv.t[:], gv_d, gv.ds, writes=[gv])
        T.dma(SP, ident_f.t[:], cst_d[:, 0:128], ident_f.ds, writes=[ident_f])
        T.dma(POOL, ident_b.t[:], cst_d[:, 0:128], ident_b.ds, writes=[ident_b])
        T.dma(POOL, trineg.t[:], cst_d[:, 128:256], trineg.ds, writes=[trineg])
        T.op(DVE, lambda: nc.vector.memset(ones_b.t[:], 1.0), writes=[ones_b])

        def gcol(c):
            return gv.t[:, c:c + 1]

        def MM(*a, **k):
            return nc.tensor.matmul(*a, **k) if T.live else None

        def MT(*a, **k):
            return nc.tensor.transpose(*a, **k) if T.live else None

        def cast_dma(buf, out_ap, in_ap, ncols, ds, chunk=512):
            nchunk = (ncols + chunk - 1) // chunk
            w = (ncols + nchunk - 1) // nchunk
            for a in range(0, ncols, w):
                b = min(ncols, a + w)
                T.dma(POOL, out_ap[:, a:b], in_ap[:, a:b], ds, writes=[buf])

        class Stream:
            def __init__(self, stack, shape, nslots, chunk=512):
                self.chunk = chunk
                self.slots = [sbt(stack, shape, BF, ds=True) for _ in range(nslots)]
                self.n = nslots
                self.jobs = []
                self.issued = 0
                self.base = 0

            def start(self, jobs):
                self.jobs = self.jobs + list(jobs)

            def _issue(self, k):
                sl = self.slots[k % self.n]
                cast_dma(sl, sl.t, self.jobs[k], sl.t.shape[1], sl.ds, chunk=self.chunk)

            def get(self, i):
                hi = min(i + self.n - 1, len(self.jobs) - 1)
                while self.issued <= hi:
                    self._issue(self.issued)
                    self.issued += 1
                return self.slots[i % self.n]

        def sq_and_sum(stack_bufs, src_ap_fn, src_bufs, ntiles, ssb, ncols=512):
            sqb = stack_bufs
            for i in range(ntiles):
                q = sqb[i % len(sqb)]
                T.op(ACT, lambda q=q, i=i: nc.scalar.activation(out=q.t[:, :ncols], in_=src_ap_fn(i), func=AF.Square),
                     reads=src_bufs, writes=[q])
                T.op(PE, lambda q=q, i=i: MM(ssb.t[:, :ncols], lhsT=ones_b.t[:], rhs=q.t[:, :ncols],
                                                          start=(i == 0), stop=(i == ntiles - 1)),
                     reads=[q, ones_b], writes=[ssb])

        def rstd_from(ssb, rt, rstd, dim, ncols=512):
            T.op(DVE, lambda: nc.vector.tensor_scalar(out=rt.t[:, :ncols], in0=ssb.t[:, :ncols], scalar1=1.0 / dim,
                                                      scalar2=EPS, op0=ALU.mult, op1=ALU.add),
                 reads=[ssb], writes=[rt])
            T.op(ACT, lambda: nc.scalar.activation(out=rt.t[:, :ncols], in_=rt.t[:, :ncols], func=AF.Sqrt),
                 reads=[rt], writes=[rt])
            T.op(DVE, lambda: nc.vector.reciprocal(out=rstd.t[:, :ncols], in_=rt.t[:, :ncols]),
                 reads=[rt], writes=[rstd])

        def prenorm(c0, g0, outb, ocol0, sqb, ssb, rt, rstd):
            sq_and_sum(sqb, lambda i: xT.t[:, i, c0:c0 + 512], [xT], 8, ssb)
            rstd_from(ssb, rt, rstd, 1024.0)
            for kc in range(8):
                T.op(DVE, lambda kc=kc: nc.vector.scalar_tensor_tensor(
                    out=outb.t[:, kc, ocol0:ocol0 + 512], in0=xT.t[:, kc, c0:c0 + 512], scalar=gcol(g0 + kc),
                    in1=rstd.t[:, :], op0=ALU.mult, op1=ALU.mult), reads=[xT, rstd, gv], writes=[outb])

        def down_project(stream, job0, nk, rhs_fn, rhs_bufs, fTs, c0s, gpost, half_scale, ybanks, ssbs, sqb, rt, rstd):
            pend = None
            step = 0
            ng = len(c0s)
            for dt in range(8):
                sl = stream.get(job0 + dt)
                for ti in range(ng):
                    yb = ybanks[step % 2]
                    q = sqb[step % 2]
                    step += 1
                    fT, ssb = fTs[ti], ssbs[ti]
                    T.pre(PE, reads=[sl] + rhs_bufs, writes=[yb])
                    for k in range(nk):
                        ins = MM(yb.t[:, :], lhsT=sl.t[:, k * 128:(k + 1) * 128], rhs=rhs_fn(k, ti),
                                               start=(k == 0), stop=(k == nk - 1))
                    T.post(PE, ins, reads=[sl] + rhs_bufs, writes=[yb])
                    if pend is not None:
                        pend()
                    T.op(ACT, lambda q=q, yb=yb: nc.scalar.activation(out=q.t[:, :], in_=yb.t[:, :], func=AF.Square),
                         reads=[yb], writes=[q])
                    T.op(DVE, lambda yb=yb, dt=dt, fT=fT: nc.vector.tensor_copy(out=fT.t[:, dt, :], in_=yb.t[:, :]),
                         reads=[yb], writes=[fT])

                    def pend(q=q, dt=dt, ssb=ssb):
                        T.op(PE, lambda: MM(ssb.t[:, :], lhsT=ones_b.t[:], rhs=q.t[:, :],
                                                          start=(dt == 0), stop=(dt == 7)),
                             reads=[q, ones_b], writes=[ssb])
            pend()
            T.mark('dp_finalize')
            for ti in range(ng):
                fT, c0 = fTs[ti], c0s[ti]
                rstd_from(ssbs[ti], rt, rstd, 1024.0)
                for dt in range(8):
                    T.op(DVE, lambda dt=dt, fT=fT: nc.vector.scalar_tensor_tensor(
                        out=fT.t[:, dt, :], in0=fT.t[:, dt, :], scalar=gcol(gpost + dt), in1=rstd.t[:, :],
                        op0=ALU.mult, op1=ALU.mult), reads=[fT, rstd, gv], writes=[fT])
                    T.op(DVE, lambda dt=dt, fT=fT, c0=c0: nc.vector.scalar_tensor_tensor(
                        out=xT.t[:, dt, c0:c0 + 512], in0=fT.t[:, dt, :], scalar=half_scale, in1=xT.t[:, dt, c0:c0 + 512],
                        op0=ALU.mult, op1=ALU.add), reads=[fT, xT], writes=[xT])

        def ffn(gpre, gpost, wgu_d, wd_d):
            with contextlib.ExitStack() as fs:
                xn = sbt(fs, [128, 8, 1024], BF, dj=True)
                actT = sbt(fs, [128, NFT, 1024], BF, dj=True)
                fTs = [sbt(fs, [128, 8, 512], F32) for _ in range(2)]
                sg = [sbt(fs, [128, 512], BF) for _ in range(2)]
                sqb = [sbt(fs, [128, 512], BF) for _ in range(2)]
                rt = sbt(fs, [128, 512], F32)
                rstd = sbt(fs, [128, 512], F32)
                SA = Stream(fs, [128, 2048], 4, chunk=1024)
                SB = Stream(fs, [128, DFF], 3, chunk=1408)
                for half in range(2):
                    SA.start([wgu_d[ft] for ft in range(NFT)])
                    SB.start([wd_d[dt] for dt in range(8)])
                rsn = [sbt(fs, [128, 512], F32) for _ in range(2)]

                def stats(hh, ssbanks):
                    for tg in range(2):
                        cc = hh + tg * 512
                        sq_and_sum(sqb, lambda i, cc=cc: xT.t[:, i, cc:cc + 512], [xT], 8, ssbanks[tg])
                        rstd_from(ssbanks[tg], rt, rsn[tg], 1024.0)

                def apply(hh):
                    for tg in range(2):
                        for kc in range(8):
                            T.op(DVE, lambda kc=kc, tg=tg: nc.vector.scalar_tensor_tensor(
                                out=xn.t[:, kc, tg * 512:(tg + 1) * 512], in0=xT.t[:, kc, hh + tg * 512:hh + (tg + 1) * 512],
                                scalar=gcol(gpre + kc), in1=rsn[tg].t[:, :], op0=ALU.mult, op1=ALU.mult),
                                reads=[xT, rsn[tg], gv], writes=[xn])

                SA.get(0)
                stats(0, [banks[6], banks[7]])
                apply(0)
                for half in range(2):
                    h0 = half * 1024
                    for ft in range(NFT):
                        sl = SA.get(half * NFT + ft)
                        if half == 0 and ft == 10:
                            stats(1024, [banks[4], banks[5]])
                        for tg in range(2):
                            hg, hu = banks[2 * tg], banks[2 * tg + 1]
                            T.pre(PE, reads=[sl, xn], writes=[hg, hu])
                            for kc in range(8):
                                MM(hg.t[:, :], lhsT=sl.t[:, kc * 128:(kc + 1) * 128],
                                   rhs=xn.t[:, kc, tg * 512:(tg + 1) * 512], start=(kc == 0), stop=(kc == 7))
                            for kc in range(8):
                                ins = MM(hu.t[:, :], lhsT=sl.t[:, 1024 + kc * 128:1024 + (kc + 1) * 128],
                                         rhs=xn.t[:, kc, tg * 512:(tg + 1) * 512], start=(kc == 0), stop=(kc == 7))
                            T.post(PE, ins, reads=[sl, xn], writes=[hg, hu])
                            s_ = sg[tg]
                            T.op(ACT, lambda s_=s_, hg=hg: nc.scalar.activation(out=s_.t[:, :], in_=hg.t[:, :], func=AF.Silu),
                                 reads=[hg], writes=[s_])
                            T.op(DVE, lambda s_=s_, hu=hu, ft=ft, tg=tg: nc.vector.tensor_tensor(
                                out=actT.t[:, ft, tg * 512:(tg + 1) * 512], in0=hu.t[:, :], in1=s_.t[:, :], op=ALU.mult),
                                reads=[hu, s_], writes=[actT])
                    if half == 0:
                        apply(1024)
                    down_project(SB, half * 8, NFT, lambda k, ti: actT.t[:, k, ti * 512:(ti + 1) * 512], [actT],
                                 fTs, [h0, h0 + 512], gpost, 0.5, [banks[4], banks[5]], [banks[6], banks[7]], sqb, rt, rstd)

        T.barrier()
        if STOP >= 1:
            ffn(G_F1PRE, G_F1POST, w1gu_d, w1d_d)
        T.barrier()

        with contextlib.ExitStack() as ms:
          if STOP >= 2:
            usT = sbt(ms, [128, 4, S], BF, dj=True)
            matt = sbt(ms, [128, 4, S], BF, dj=True)
            with contextlib.ExitStack() as as_:
                Kpk = sbt(as_, [128, 4, S], BF, dj=True)
                Kx = sbt(as_, [128, S], BF, ds=True)
                V = sbt(as_, [128, 16, 512], BF, dj=True)
                Qm = sbt(as_, [128, 8, 512], BF)
                Qx = sbt(as_, [128, 8, 512], BF, ds=True)
                un = sbt(as_, [128, 8, 512], BF, dj=True)
                ksum = sbt(as_, [128, 4, 8], F32)
                ksum_b = sbt(as_, [128, 4, 8], BF)
                Gsb = sbt(as_, [128, 32, 8], F32)
                M8 = sbt(as_, [128, 32, 8], F32)
                SEL = sbt(as_, [128, 32, 8], F32)
                mbias = sbt(as_, [128, 4, 64], F32)
                MTsb = sbt(as_, [64, 512], BF)
                pT = [sbt(as_, [128, 512], BF) for _ in range(6)]
                rden = sbt(as_, [128, 512], F32)
                sqb = [sbt(as_, [128, 512], BF) for _ in range(2)]
                rt = sbt(as_, [128, 512], F32)
                rstd = sbt(as_, [128, 512], F32)
                att = sbt(as_, [128, 4, 512], F32)
                WS = Stream(as_, [128, 1024], 4, chunk=1024)
                T.op(DVE, lambda: nc.vector.memset(ksum.t[:], 0.0), writes=[ksum])
                T.op(DVE, lambda: nc.vector.memset(Kx.t[:], 0.0), writes=[Kx])
                T.op(DVE, lambda: nc.vector.memset(Qx.t[:], 0.0), writes=[Qx])
                T.op(DVE, lambda: nc.vector.memset(Qm.t[:], 0.0), writes=[Qm])
                Qx_ds2 = T.newds()
                cast_dma(Kx, Kx.t[0:12, :], kaug_d, S, Kx.ds)
                for tg in range(4):
                    WS.start([win_d[n] for n in range(16)])
                for tg in range(4):
                    c0 = tg * 512
                    WS.get(tg * 16)
                    T.dma(POOL, Qx.t[8:12, :, :], qaug_d[:, :, c0:c0 + 512], Qx.ds, writes=[Qx])
                    prenorm(c0, G_MIXPRE, un, 0, sqb, banks[6], rt, rstd)
                    for n in range(8):
                        sl = WS.get(tg * 16 + n)
                        pb = banks[n % 2]
                        T.pre(PE, reads=[sl, un], writes=[pb])
                        for kc in range(8):
                            ins = MM(pb.t[:, :], lhsT=sl.t[:, kc * 128:(kc + 1) * 128], rhs=un.t[:, kc, :],
                                                   start=(kc == 0), stop=(kc == 7))
                        T.post(PE, ins, reads=[sl, un], writes=[pb])
                        if n < 4:
                            T.op(ACT, lambda pb=pb, n=n: nc.scalar.copy(out=Kpk.t[:, n, c0:c0 + 512], in_=pb.t[:, :]),
                                 reads=[pb], writes=[Kpk])
                            T.op(DVE, lambda pb=pb, n=n: nc.vector.tensor_reduce(
                                out=ksum.t[:, n, 2 * tg:2 * tg + 2], in_=pb.t[:, :].rearrange("p (a b) -> p a b", b=256),
                                axis=AX.X, op=ALU.add), reads=[pb], writes=[ksum])
                        else:
                            for e in range(2):
                                T.op(ACT, lambda pb=pb, n=n, e=e: nc.scalar.mul(out=Qm.t[64 * e:64 * e + 64, 2 * (n - 4) + e, :],
                                                                               in_=pb.t[64 * e:64 * e + 64, :], mul=0.125),
                                     reads=[pb], writes=[Qm])
                    if tg < 2:
                        T.op(DVE, lambda: nc.vector.memset(mbias.t[:], 0.0), writes=[mbias])
                    else:
                        T.op(DVE, lambda: nc.vector.tensor_copy(out=ksum_b.t[:], in_=ksum.t[:]), reads=[ksum], writes=[ksum_b])
                        gb = banks[7]
                        T.pre(PE, reads=[Qm, ksum_b], writes=[gb])
                        for qt in range(4):
                            for h in range(8):
                                e, hp = h % 2, h // 2
                                ins = MM(gb.t[:, (qt * 8 + h) * 8:(qt * 8 + h) * 8 + 8],
                                                       lhsT=Qm.t[:, h, qt * 128:(qt + 1) * 128],
                                                       rhs=ksum_b.t[:, hp, :], start=True, stop=True)
                        T.post(PE, ins, reads=[Qm, ksum_b], writes=[gb])
                        T.op(DVE, lambda: nc.vector.memset(Gsb.t[:], -1e30), writes=[Gsb])
                        for half2 in range(2):
                            nb = 2 * tg + half2
                            T.op(DVE, lambda half2=half2, nb=nb: nc.vector.tensor_copy(
                                out=Gsb.t[:, 16 * half2:16 * half2 + 16, 0:nb],
                                in_=gb.t[:, 128 * half2:128 * half2 + 128].rearrange("p (a b) -> p a b", b=8)[:, :, 0:nb]),
                                reads=[gb], writes=[Gsb])
                        for i in range(32):
                            T.op(DVE, lambda i=i: nc.vector.max(out=M8.t[:, i, :], in_=Gsb.t[:, i, :]), reads=[Gsb], writes=[M8])
                        T.op(DVE, lambda: nc.vector.tensor_tensor(out=SEL.t[:], in0=Gsb.t[:], in1=M8.t[:, :, 2:3].to_broadcast([128, 32, 8]),
                                                                  op=ALU.is_ge), reads=[Gsb, M8], writes=[SEL])
                        T.op(DVE, lambda: nc.vector.tensor_scalar(out=mbias.t[:].rearrange("p a (h n) -> p (a h) n", n=8), in0=SEL.t[:],
                                                                  scalar1=1.0, scalar2=BIG, op0=ALU.subtract, op1=ALU.mult),
                             reads=[SEL], writes=[mbias])
                        for half2 in range(2):
                            nb = 2 * tg + half2
                            T.op(DVE, lambda half2=half2, nb=nb: nc.vector.memset(
                                mbias.t[:, 2 * half2:2 * half2 + 2, :].rearrange("p a (h n) -> p a h n", n=8)[:, :, :, nb:nb + 1], 0.0),
                                writes=[mbias])
                    for c in range(4):
                        sl = WS.get(tg * 16 + 8 + c)
                        pb = banks[2 + c % 2]
                        T.pre(PE, reads=[sl, un], writes=[pb])
                        for tt in range(4):
                            for kc in range(8):
                                ins = MM(pb.t[:, tt * 128:(tt + 1) * 128], lhsT=un.t[:, kc, tt * 128:(tt + 1) * 128],
                                                       rhs=sl.t[:, kc * 128:(kc + 1) * 128], start=(kc == 0), stop=(kc == 7))
                        T.post(PE, ins, reads=[sl, un], writes=[pb])
                        T.op(DVE, lambda pb=pb, c=c: nc.vector.tensor_copy(
                            out=V.t[:, 4 * tg:4 * tg + 4, c * 128:(c + 1) * 128],
                            in_=pb.t[:, :].rearrange("p (a b) -> p a b", b=128)), reads=[pb], writes=[V])
                    for k in range(4):
                        sl = WS.get(tg * 16 + 12 + k)
                        pb = banks[4 + k % 2]
                        T.pre(PE, reads=[sl, un], writes=[pb])
                        for kc in range(8):
                            ins = MM(pb.t[:, :], lhsT=sl.t[:, kc * 128:(kc + 1) * 128], rhs=un.t[:, kc, :],
                                                   start=(kc == 0), stop=(kc == 7))
                        T.post(PE, ins, reads=[sl, un], writes=[pb])
                        T.op(ACT, lambda pb=pb, k=k: nc.scalar.copy(out=usT.t[:, k, c0:c0 + 512], in_=pb.t[:, :]),
                             reads=[pb], writes=[usT])
                    mb = banks[7]
                    T.pre(PE, reads=[mbias, ident_f], writes=[mb])
                    for qt in range(4):
                        ins = MT(mb.t[0:64, qt * 128:(qt + 1) * 128], mbias.t[:, qt, :], ident_f.t[:, :])
                    T.post(PE, ins, reads=[mbias, ident_f], writes=[mb])
                    T.op(ACT, lambda: nc.scalar.copy(out=MTsb.t[:, :], in_=mb.t[0:64, :]), reads=[mb], writes=[MTsb])
                    for h in range(8):
                        T.dma(SP, Qx.t[0:8, h, :], MTsb.t[8 * h:8 * h + 8, :], Qx_ds2, reads=[MTsb], writes=[Qx])
                    nkt = 4 * tg + 4
                    for hp in range(4):
                        numP, denP = banks[2 + hp % 2], banks[4 + hp % 2]

                        def head_thunks(e):
                            h = 2 * hp + e
                            rows = slice(64 * e, 64 * e + 64)
                            sbanks = (banks[0], banks[1]) if e == 0 else (banks[6], banks[7])
                            th = []
                            prev = None
                            for kt in range(nkt):
                                j = kt - 4 * tg
                                ql = 128 * j if j > 0 else 0
                                sP = sbanks[kt % 2]
                                p = pT[3 * e + kt % 3]

                                def s_step(kt=kt, j=j, ql=ql, sP=sP):
                                    rd = [Kpk, Qm, Kx, Qx, ident_b, trineg]
                                    T.pre(PE, reads=rd, writes=[sP])
                                    MM(sP.t[:, ql:512], lhsT=Kpk.t[:, hp, kt * 128:(kt + 1) * 128], rhs=Qm.t[:, h, ql:512], start=True, stop=False)
                                    ins = MM(sP.t[:, ql:512], lhsT=Kx.t[:, kt * 128:(kt + 1) * 128], rhs=Qx.t[:, h, ql:512], start=False, stop=(j < 0))
                                    if j >= 0:
                                        ins = MM(sP.t[:, ql:ql + 128], lhsT=ident_b.t[:, :], rhs=trineg.t[:, :], start=False, stop=True)
                                    T.post(PE, ins, reads=rd, writes=[sP])

                                def e_step(ql=ql, sP=sP, p=p):
                                    T.op(ACT, lambda: nc.scalar.activation(out=p.t[:, ql:512], in_=sP.t[:, ql:512], func=AF.Exp), reads=[sP], writes=[p])

                                def pv_step(kt=kt, ql=ql, p=p):
                                    T.pre(PE, reads=[p, V, ones_b], writes=[numP, denP])
                                    MM(numP.t[rows, ql:512], lhsT=V.t[:, kt, h * 64:(h + 1) * 64], rhs=p.t[:, ql:512], start=(kt == 0), stop=(kt == nkt - 1))
                                    ins2 = MM(denP.t[rows, ql:512], lhsT=ones_b.t[:, 0:64], rhs=p.t[:, ql:512], start=(kt == 0), stop=(kt == nkt - 1))
                                    T.post(PE, ins2, reads=[p, V, ones_b], writes=[numP, denP])
                                th.append(s_step)
                                th.append(e_step)
                                if prev is not None:
                                    th.append(prev)
                                prev = pv_step
                            th.append(prev)
                            return th
                        for fa, fb in zip(head_thunks(0), head_thunks(1)):
                            fa()
                            fb()
                        T.op(DVE, lambda: nc.vector.reciprocal(out=rden.t[:, :], in_=denP.t[:, :]), reads=[denP], writes=[rden])
                        T.op(DVE, lambda: nc.vector.tensor_tensor(out=att.t[:, hp, :], in0=numP.t[:, :], in1=rden.t[:, :],
                                                                  op=ALU.mult), reads=[numP, rden], writes=[att])
                    sq_and_sum(sqb, lambda i: att.t[:, i, :], [att], 4, banks[6])
                    rstd_from(banks[6], rt, rstd, 512.0)
                    for hp in range(4):
                        T.op(DVE, lambda hp=hp: nc.vector.scalar_tensor_tensor(
                            out=matt.t[:, hp, c0:c0 + 512], in0=att.t[:, hp, :], scalar=gcol(G_ATT + hp), in1=rstd.t[:, :],
                            op0=ALU.mult, op1=ALU.mult), reads=[att, rstd, gv], writes=[matt])
            T.barrier()
            if STOP >= 3:
             with contextlib.ExitStack() as ss_:
                CS = sbt(ss_, [128, 2, 16, 128], BF)
                BB = sbt(ss_, [128, 16, 2, 128], BF)
                CC = sbt(ss_, [128, 16, 3, 64], BF)
                DDm = sbt(ss_, [128, 512], BF, ds=True)
                wglu = sbt(ss_, [128, 2048], BF, ds=True)
                mag_s = sbt(ss_, [128, 16], F32)
                ECS = sbt(ss_, [128, 2, 16], F32)
                Hc = sbt(ss_, [128, 2, 16], F32)
                Hm = sbt(ss_, [128, 2, 16], F32)
                magT = sbt(ss_, [128, 16, 128], F32)
                cast_dma(DDm, DDm.t, dlay_d, 512, DDm.ds)
                cast_dma(wglu, wglu.t, wglu_d, 2048, wglu.ds)

                def sincos(stk, ang, n, sinb, cosb, tmpF, tmpI):
                    T.op(DVE, lambda: nc.vector.tensor_scalar(out=tmpI.t[:, :n], in0=ang.t[:, :n], scalar1=1.0 / (2 * PI), scalar2=None,
                                                              op0=ALU.mult), reads=[ang], writes=[tmpI])
                    T.op(DVE, lambda: nc.vector.tensor_copy(out=tmpF.t[:, :n], in_=tmpI.t[:, :n]), reads=[tmpI], writes=[tmpF])
                    T.op(DVE, lambda: nc.vector.scalar_tensor_tensor(out=tmpF.t[:, :n], in0=tmpF.t[:, :n], scalar=-2 * PI, in1=ang.t[:, :n],
                                                                     op0=ALU.mult, op1=ALU.add), reads=[tmpF, ang], writes=[tmpF])
                    T.op(DVE, lambda: nc.vector.tensor_scalar(out=tmpF.t[:, :n], in0=tmpF.t[:, :n], scalar1=-PI, scalar2=PI,
                                                              op0=ALU.max, op1=ALU.min), reads=[tmpF], writes=[tmpF])
                    T.op(ACT, lambda: nc.scalar.activation(out=sinb.t[:, :n], in_=tmpF.t[:, :n], func=AF.Sin), reads=[tmpF], writes=[sinb])
                    T.op(ACT, lambda: nc.scalar.activation(out=tmpF.t[:, :n], in_=tmpF.t[:, :n], func=AF.Abs), reads=[tmpF, sinb], writes=[tmpF])
                    T.op(DVE, lambda: nc.vector.tensor_scalar(out=tmpF.t[:, :n], in0=tmpF.t[:, :n], scalar1=-1.0, scalar2=PI / 2,
                                                              op0=ALU.mult, op1=ALU.add), reads=[tmpF], writes=[tmpF])
                    T.op(ACT, lambda: nc.scalar.activation(out=cosb.t[:, :n], in_=tmpF.t[:, :n], func=AF.Sin), reads=[tmpF], writes=[cosb])

                TT = lambda o, a, b, op, rd, wr: T.op(DVE, lambda: nc.vector.tensor_tensor(out=o, in0=a, in1=b, op=op), reads=rd, writes=wr)
                with contextlib.ExitStack() as st:
                    lamst = sbt(st, [128, 48], F32, ds=True)
                    iota = sbt(st, [128, 128], F32, ds=True)
                    clay = sbt(st, [128, 2 * 1024], F32, ds=True)
                    T.dma(SP, lamst.t[:], lamst_d, lamst.ds, writes=[lamst])
                    T.dma(SP, iota.t[:], cst_d[:, 256:384], iota.ds, writes=[iota])
                    T.dma(SP, clay.t[:], clay_d, clay.ds, writes=[clay])
                    W = [sbt(st, [128, 2048], F32) for _ in range(3)]
                    WI = sbt(st, [128, 2048], I32)
                    sS = sbt(st, [128, 2048], F32)
                    sC = sbt(st, [128, 2048], F32)
                    dts, th_s, e1 = W[0], W[1], W[2]
                    T.op(ACT, lambda: nc.scalar.activation(out=dts.t[:, :16], in_=lamst.t[:, 32:48], func=AF.Exp), reads=[lamst], writes=[dts])
                    TT(e1.t[:, :16], lamst.t[:, 0:16], dts.t[:, :16], ALU.mult, [lamst, dts], [e1])
                    T.op(ACT, lambda: nc.scalar.activation(out=mag_s.t[:, :], in_=e1.t[:, :16], func=AF.Exp), reads=[e1], writes=[mag_s])
                    TT(th_s.t[:, :16], lamst.t[:, 16:32], dts.t[:, :16], ALU.mult, [lamst, dts], [th_s])
                    for j in range(16):
                        T.op(DVE, lambda j=j: nc.vector.tensor_scalar(out=magT.t[:, j, :], in0=iota.t[:, :], scalar1=0.0,
                                                                      scalar2=mag_s.t[:, j:j + 1], op0=ALU.mult, op1=ALU.add),
                             reads=[iota, mag_s], writes=[magT])
                    T.op(DVE, lambda: nc.vector.memset(magT.t[:, :, 0:1], 0.0), writes=[magT])
                    PH = W[2]
                    for j in range(16):
                        T.op(DVE, lambda j=j: nc.vector.tensor_scalar(out=PH.t[:, j * 128:(j + 1) * 128], in0=iota.t[:, :],
                                                                      scalar1=th_s.t[:, j:j + 1], scalar2=None, op0=ALU.mult),
                             reads=[iota, th_s], writes=[PH])
                    sincos(st, PH, 2048, sS, sC, W[0], WI)
                    j3 = lambda ap: ap.rearrange("p (j t) -> p j t", t=128)
                    T.op(DVE, lambda: nc.vector.tensor_copy(out=CS.t[:, 0, :, :], in_=j3(sC.t[:, :])), reads=[sC], writes=[CS])
                    T.op(DVE, lambda: nc.vector.tensor_copy(out=CS.t[:, 1, :, :], in_=j3(sS.t[:, :])), reads=[sS], writes=[CS])
                    T.op(DVE, lambda: nc.vector.tensor_copy(out=ECS.t[:, 0, :], in_=j3(sC.t[:, :])[:, :, 127]), reads=[sC], writes=[ECS])
                    T.op(DVE, lambda: nc.vector.tensor_copy(out=ECS.t[:, 1, :], in_=j3(sS.t[:, :])[:, :, 127]), reads=[sS], writes=[ECS])
                    c3 = lambda ap: ap.rearrange("p (j c) -> p j c", c=64)
                    T.op(DVE, lambda: nc.vector.tensor_copy(out=CC.t[:, :, 0, :], in_=c3(clay.t[:, 0:1024])), reads=[clay], writes=[CC])
                    T.op(DVE, lambda: nc.vector.tensor_scalar(out=CC.t[:, :, 1, :], in0=c3(clay.t[:, 0:1024]), scalar1=-1.0, scalar2=None,
                                                              op0=ALU.mult), reads=[clay], writes=[CC])
                    T.op(DVE, lambda: nc.vector.tensor_scalar(out=CC.t[:, :, 2, :], in0=c3(clay.t[:, 1024:2048]), scalar1=-1.0, scalar2=None,
                                                              op0=ALU.mult), reads=[clay], writes=[CC])
                T.barrier()
                with contextlib.ExitStack() as st:
                    NH = 1024
                    lamh = sbt(st, [128, 3 * NH], F32, ds=True)
                    blh = sbt(st, [128, 2 * NH], F32, ds=True)
                    W = [sbt(st, [128, NH], F32) for _ in range(7)]
                    WI = sbt(st, [128, NH], I32)
                    sS = sbt(st, [128, NH], F32)
                    sC = sbt(st, [128, NH], F32)
                    for hf in range(2):
                        for q in range(3):
                            T.dma(SP, lamh.t[:, q * NH:(q + 1) * NH], lambc_d[:, q * 2048 + hf * NH:q * 2048 + (hf + 1) * NH], lamh.ds, writes=[lamh])
                        for q in range(2):
                            T.dma(SP, blh.t[:, q * NH:(q + 1) * NH], blay_d[:, q * 2048 + hf * NH:q * 2048 + (hf + 1) * NH], blh.ds, writes=[blh])
                        lr, li, ldt = lamh.t[:, 0:NH], lamh.t[:, NH:2 * NH], lamh.t[:, 2 * NH:3 * NH]
                        dtb, thb, magb = W[0], W[1], W[2]
                        T.op(ACT, lambda: nc.scalar.activation(out=dtb.t[:, :], in_=ldt, func=AF.Exp), reads=[lamh], writes=[dtb])
                        TT(magb.t[:, :], lr, dtb.t[:, :], ALU.mult, [lamh, dtb], [magb])
                        T.op(ACT, lambda: nc.scalar.activation(out=magb.t[:, :], in_=magb.t[:, :], func=AF.Exp), reads=[magb], writes=[magb])
                        TT(thb.t[:, :], li, dtb.t[:, :], ALU.mult, [lamh, dtb], [thb])
                        sincos(st, thb, NH, sS, sC, W[4], WI)
                        are, aim, den, fre, fim, tmp = W[3], W[4], W[5], W[6], W[0], W[1]
                        TT(are.t[:, :], magb.t[:, :], sC.t[:, :], ALU.mult, [magb, sC], [are])
                        TT(aim.t[:, :], magb.t[:, :], sS.t[:, :], ALU.mult, [magb, sS], [aim])
                        T.op(DVE, lambda: nc.vector.tensor_scalar(out=are.t[:, :], in0=are.t[:, :], scalar1=-1.0, scalar2=None, op0=ALU.add),
                             reads=[are], writes=[are])
                        TT(den.t[:, :], lr, lr, ALU.mult, [lamh], [den])
                        TT(tmp.t[:, :], li, li, ALU.mult, [lamh], [tmp])
                        TT(den.t[:, :], den.t[:, :], tmp.t[:, :], ALU.add, [den, tmp], [den])
                        T.op(DVE, lambda: nc.vector.reciprocal(out=den.t[:, :], in_=den.t[:, :]), reads=[den], writes=[den])
                        TT(fre.t[:, :], are.t[:, :], lr, ALU.mult, [are, lamh], [fre])
                        TT(tmp.t[:, :], aim.t[:, :], li, ALU.mult, [aim, lamh], [tmp])
                        TT(fre.t[:, :], fre.t[:, :], tmp.t[:, :], ALU.add, [fre, tmp], [fre])
                        TT(fre.t[:, :], fre.t[:, :], den.t[:, :], ALU.mult, [fre, den], [fre])
                        TT(fim.t[:, :], aim.t[:, :], lr, ALU.mult, [aim, lamh], [fim])
                        TT(tmp.t[:, :], are.t[:, :], li, ALU.mult, [are, lamh], [tmp])
                        TT(fim.t[:, :], fim.t[:, :], tmp.t[:, :], ALU.subtract, [fim, tmp], [fim])
                        TT(fim.t[:, :], fim.t[:, :], den.t[:, :], ALU.mult, [fim, den], [fim])
                        bre, bim = blh.t[:, 0:NH], blh.t[:, NH:2 * NH]
                        t2, t3 = W[2], W[3]
                        v3 = lambda ap: ap.rearrange("p (j c) -> p j c", c=128)
                        js = slice(8 * hf, 8 * hf + 8)
                        TT(t2.t[:, :], fre.t[:, :], bre, ALU.mult, [fre, blh], [t2])
                        TT(t3.t[:, :], fim.t[:, :], bim, ALU.mult, [fim, blh], [t3])
                        TT(BB.t[:, js, 0, :], v3(t2.t[:, :]), v3(t3.t[:, :]), ALU.subtract, [t2, t3], [BB])
                        TT(t2.t[:, :], fre.t[:, :], bim, ALU.mult, [fre, blh], [t2])
                        TT(t3.t[:, :], fim.t[:, :], bre, ALU.mult, [fim, blh], [t3])
                        TT(BB.t[:, js, 1, :], v3(t2.t[:, :]), v3(t3.t[:, :]), ALU.add, [t2, t3], [BB])
                T.barrier()
                TTq = sbt(ss_, [128, 4, 4, 128], BF)
                XTq = sbt(ss_, [128, 2, 4, 128], BF)
                Gq = sbt(ss_, [128, 2, 4, 128], BF)
                Pq = [sbt(ss_, [128, 4, 4, 128], BF) for _ in range(2)]
                ctmp = sbt(ss_, [128, 4, 4], F32)
                ygf = sbt(ss_, [128, 4, 512], F32)
                ygb = sbt(ss_, [128, 4, 512], BF)
                sig = sbt(ss_, [128, 512], F32)
                mssm = sbt(ss_, [128, 4, 512], BF)
                fT = sbt(ss_, [128, 8, 512], F32)
                sqb = [sbt(ss_, [128, 512], BF) for _ in range(2)]
                rt = sbt(ss_, [128, 512], F32)
                rstd = sbt(ss_, [128, 512], F32)
                WO = Stream(ss_, [128, 1024], 3, chunk=1024)
                for tg in range(4):
                    WO.start([wout_d[dt] for dt in range(8)])
                TTqs = [TTq, sbt(ss_, [128, 4, 4, 128], BF)]
                XTqs = [XTq, sbt(ss_, [128, 2, 4, 128], BF)]
                Gqs = [Gq, sbt(ss_, [128, 2, 4, 128], BF)]
                Hc.dj = True
                Hm.dj = True

                def quad_thunks(c, cc, t0, k, par):
                    XA, XB = banks[2 * par], banks[2 * par + 1]
                    P, TT_, XT_, G_ = Pq[par], TTqs[par], XTqs[par], Gqs[par]
                    q4 = slice(4 * k, 4 * k + 4)
                    xa = XA.t[:, :].rearrange("p (a b) -> p a b", b=128)
                    xb = XB.t[:, :].rearrange("p (a b) -> p a b", b=128)
                    cosq, sinq = CS.t[:, 0, q4, :], CS.t[:, 1, q4, :]
                    th = []

                    def pe_x():
                        T.pre(PE, reads=[BB, usT], writes=[XA, XB])
                        for jl in range(4):
                            j = 4 * k + jl
                            MM(XA.t[:, jl * 128:(jl + 1) * 128], lhsT=BB.t[:, j, 0, :], rhs=usT.t[:, k, t0:t0 + 128], start=True, stop=True)
                            ins = MM(XB.t[:, jl * 128:(jl + 1) * 128], lhsT=BB.t[:, j, 1, :], rhs=usT.t[:, k, t0:t0 + 128], start=True, stop=True)
                        T.post(PE, ins, reads=[BB, usT], writes=[XA, XB])
                    th.append(pe_x)

                    def tt(out, in0, in1, op, rd, wr):
                        th.append(lambda: T.op(DVE, lambda: nc.vector.tensor_tensor(out=out, in0=in0, in1=in1, op=op), reads=rd, writes=wr))
                    tt(TT_.t[:, 0], xa, cosq, ALU.mult, [XA, CS], [TT_])
                    tt(TT_.t[:, 1], xb, sinq, ALU.mult, [XB, CS], [TT_])
                    tt(TT_.t[:, 2], xb, cosq, ALU.mult, [XB, CS], [TT_])
                    tt(TT_.t[:, 3], xa, sinq, ALU.mult, [XA, CS], [TT_])
                    tt(XT_.t[:, 0], TT_.t[:, 0], TT_.t[:, 1], ALU.add, [TT_], [XT_])
                    tt(XT_.t[:, 1], TT_.t[:, 2], TT_.t[:, 3], ALU.subtract, [TT_], [XT_])
                    if c > 0:
                        tt(XT_.t[:, :, :, 0], XT_.t[:, :, :, 0], Hm.t[:, :, q4], ALU.add, [XT_, Hm], [XT_])
                    for r in range(2):
                        th.append(lambda r=r: T.op(DVE, lambda: nc.vector.tensor_tensor_scan(
                            out=G_.t[:, r, :, :].rearrange("p a b -> p (a b)"), data0=magT.t[:, q4, :].rearrange("p a b -> p (a b)"),
                            data1=XT_.t[:, r, :, :].rearrange("p a b -> p (a b)"), initial=0.0, op0=ALU.mult, op1=ALU.add),
                            reads=[XT_, magT], writes=[G_]))
                    tt(P.t[:, 0:2], G_.t[:, 0:2], CS.t[:, 0:2, q4, :], ALU.mult, [G_, CS], [P])
                    tt(P.t[:, 2], G_.t[:, 1], cosq, ALU.mult, [G_, CS], [P])
                    tt(P.t[:, 3], G_.t[:, 0], sinq, ALU.mult, [G_, CS], [P])
                    tt(Hc.t[:, 0, q4], P.t[:, 0, :, 127], P.t[:, 1, :, 127], ALU.subtract, [P], [Hc])
                    tt(Hc.t[:, 1, q4], P.t[:, 2, :, 127], P.t[:, 3, :, 127], ALU.add, [P], [Hc])
                    for r in range(2):
                        tt(Hm.t[:, r, q4], Hc.t[:, r, q4], mag_s.t[:, q4], ALU.mult, [Hc, mag_s], [Hm])

                    def pe_c():
                        Y = banks[4 + k]
                        rd = [CC, P, DDm, usT]
                        T.pre(PE, reads=rd, writes=[Y])
                        for e in range(2):
                            rows = slice(64 * e, 64 * e + 64)
                            yo = Y.t[rows, cc * 128:(cc + 1) * 128]
                            first = True
                            for jl in (2 * e, 2 * e + 1):
                                j = 4 * k + jl
                                for (ci, pi_) in ((0, 0), (1, 1), (2, 2), (2, 3)):
                                    MM(yo, lhsT=CC.t[:, j, ci, :], rhs=P.t[:, pi_, jl, :], start=first, stop=False)
                                    first = False
                            ins = MM(yo, lhsT=DDm.t[:, k * 128 + 64 * e:k * 128 + 64 * e + 64], rhs=usT.t[:, k, t0:t0 + 128], start=False, stop=True)
                        T.post(PE, ins, reads=rd, writes=[Y])
                    th.append(pe_c)
                    return th

                for tg in range(4):
                    c0 = tg * 512
                    for cc in range(4):
                        c = 4 * tg + cc
                        t0 = c0 + 128 * cc
                        for kp in (0, 2):
                            A_ = quad_thunks(c, cc, t0, kp, 0)
                            B_ = quad_thunks(c, cc, t0, kp + 1, 1)
                            for fa, fb in zip(A_, B_):
                                fa()
                                fb()
                    for k in range(4):
                        Y = banks[4 + k]
                        T.op(ACT, lambda k=k, Y=Y: nc.scalar.activation(out=ygf.t[:, k, :], in_=Y.t[:, :], func=AF.Gelu_apprx_tanh),
                             reads=[Y], writes=[ygf])
                        T.op(DVE, lambda k=k: nc.vector.tensor_copy(out=ygb.t[:, k, :], in_=ygf.t[:, k, :]), reads=[ygf], writes=[ygb])
                    for ko in range(4):
                        GP = banks[ko % 2]
                        T.pre(PE, reads=[wglu, ygb], writes=[GP])
                        for ki in range(4):
                            ins = MM(GP.t[:, :], lhsT=wglu.t[:, ki * 512 + ko * 128:ki * 512 + (ko + 1) * 128], rhs=ygb.t[:, ki, :],
                                                   start=(ki == 0), stop=(ki == 3))
                        T.post(PE, ins, reads=[wglu, ygb], writes=[GP])
                        T.op(ACT, lambda GP=GP, ko=ko: nc.scalar.activation(out=sig.t[:, :], in_=GP.t[:, :], func=AF.Sigmoid,
                                                                          bias=gcol(G_BGLU + ko)), reads=[GP, gv], writes=[sig])
                        T.op(DVE, lambda ko=ko: nc.vector.tensor_tensor(out=ygf.t[:, ko, :], in0=ygf.t[:, ko, :], in1=sig.t[:, :], op=ALU.mult),
                             reads=[ygf, sig], writes=[ygf])
                    sq_and_sum(sqb, lambda i: ygf.t[:, i, :], [ygf], 4, banks[2])
                    rstd_from(banks[2], rt, rstd, 512.0)
                    for ko in range(4):
                        T.op(DVE, lambda ko=ko: nc.vector.scalar_tensor_tensor(
                            out=mssm.t[:, ko, :], in0=ygf.t[:, ko, :], scalar=gcol(G_SSM + ko), in1=rstd.t[:, :],
                            op0=ALU.mult, op1=ALU.mult), reads=[ygf, rstd, gv], writes=[mssm])
                    down_project(WO, tg * 8, 8,
                                 lambda m, ti, c0=c0: (matt.t[:, m, c0:c0 + 512] if m < 4 else mssm.t[:, m - 4, :]), [matt, mssm],
                                 [fT], [c0], G_MIXPOST, 1.0, [banks[0], banks[1]], [banks[3]], sqb, rt, rstd)
            T.barrier()

        if STOP >= 4:
            ffn(G_F2PRE, G_F2POST, w2gu_d, w2d_d)
        T.barrier()
        T.live = True
        T.cut = 10 ** 9
        T.barrier()
        od = T.newds()
        T.dma(SP, out_d, xT.t[:], od, reads=[xT])
        SP.e.wait_ge(od.sem, od.cnt)
    return nc


def _shared_layouts(inp):
    f = np.float32
    A = lambda k: np.ascontiguousarray(np.asarray(inp[k], dtype=f)[0])
    o = {}
    gv = np.zeros((128, 64), f)

    def put(c0, v):
        n = v.shape[0] // 128
        gv[:, c0:c0 + n] = v.reshape(n, 128).T
    put(G_F1PRE, A("ffn1_pre_g")); put(G_F1POST, A("ffn1_post_g")); put(G_MIXPRE, A("mix_pre_g"))
    put(G_ATT, A("attn_out_g")); put(G_SSM, A("ssm_out_g")); put(G_MIXPOST, A("mix_post_g"))
    put(G_F2PRE, A("ffn2_pre_g")); put(G_F2POST, A("ffn2_post_g")); put(G_BGLU, A("b_glu"))
    o["gv"] = gv

    def gu(wg, wu):
        a = wg.reshape(8, 128, NFT, 128).transpose(2, 1, 0, 3)
        b = wu.reshape(8, 128, NFT, 128).transpose(2, 1, 0, 3)
        return np.ascontiguousarray(np.concatenate([a, b], axis=2).reshape(NFT, 128, 2048))

    def dn(wd):
        return np.ascontiguousarray(wd.reshape(NFT, 128, 8, 128).transpose(2, 1, 0, 3).reshape(8, 128, DFF))
    o["w1gu"] = gu(A("ffn1_w_gate"), A("ffn1_w_up")); o["w1d"] = dn(A("ffn1_w_down"))
    o["w2gu"] = gu(A("ffn2_w_gate"), A("ffn2_w_up")); o["w2d"] = dn(A("ffn2_w_down"))
    win = A("w_in")
    order = [512 + 128 * i for i in range(4)] + [128 * i for i in range(4)] + [1024 + 128 * i for i in range(8)]
    o["win"] = np.ascontiguousarray(np.stack([win[:, c:c + 128].reshape(8, 128, 128).transpose(1, 0, 2).reshape(128, 1024) for c in order]))
    o["wglu"] = np.ascontiguousarray(A("w_glu").reshape(4, 128, 512).transpose(1, 0, 2).reshape(128, 2048))
    wo = A("w_out")
    o["wout"] = np.ascontiguousarray(wo.reshape(8, 128, 8, 128).transpose(2, 1, 0, 3).reshape(8, 128, 1024))
    lr, li, ld = A("lam_re"), A("lam_im"), A("log_dt")
    st = lambda a: a.reshape(16, 2, 64).transpose(1, 2, 0).reshape(128, 16)
    ldf = np.repeat(ld[:, None], 64, axis=1)
    o["lamst"] = np.ascontiguousarray(np.concatenate([st(lr), st(li), st(ldf)], axis=1))
    bc = np.concatenate([lr.reshape(-1), li.reshape(-1), ldf.reshape(-1)])
    o["lambc"] = np.ascontiguousarray(np.broadcast_to(bc[None, :], (128, 6144)))
    bre, bim = A("b_re"), A("b_im")
    bl = np.zeros((2, 128, 16, 128), f)
    cre, cim = A("c_re"), A("c_im")
    cl = np.zeros((2, 128, 16, 64), f)
    for j in range(16):
        q = j % 4
        for g2 in range(2):
            g = 2 * j + g2
            r0 = 32 * q + 16 * g2
            bl[0, r0:r0 + 16, j, 64 * g2:64 * g2 + 64] = bre[g].T
            bl[1, r0:r0 + 16, j, 64 * g2:64 * g2 + 64] = bim[g].T
            cc0 = 32 * (j % 2) + 16 * g2
            cl[0, 64 * g2:64 * g2 + 64, j, cc0:cc0 + 16] = cre[g].T
            cl[1, 64 * g2:64 * g2 + 64, j, cc0:cc0 + 16] = cim[g].T
    o["blay"] = np.ascontiguousarray(bl.transpose(1, 0, 2, 3).reshape(128, 4096))
    o["clay"] = np.ascontiguousarray(cl.transpose(1, 0, 2, 3).reshape(128, 2048))
    dsk = A("d_skip").reshape(512)
    dl = np.zeros((128, 4, 128), f)
    for k in range(4):
        dl[np.arange(128), k, np.arange(128)] = dsk[k * 128:(k + 1) * 128]
    o["dlay"] = dl.reshape(128, 512)
    pos = np.arange(S)
    ka = np.zeros((12, S), f)
    ka[pos // 256, pos] = 1.0
    ka[8] = 1.0; ka[9] = 1.0; ka[10] = 128.0 * (pos // 128); ka[11] = pos % 128
    o["kaugc"] = ka
    qa = np.zeros((4, 8, S), f)
    for h in range(8):
        sl = 2.0 ** (-(h + 1))
        qa[0, h] = -sl * 128.0 * (pos // 128); qa[1, h] = -sl * (pos % 128); qa[2, h] = sl; qa[3, h] = sl
    o["qaugc"] = qa
    cst = np.zeros((128, 384), f)
    cst[:, 0:128] = np.eye(128, dtype=f)
    kk, qq = np.meshgrid(np.arange(128), np.arange(128), indexing="ij")
    cst[:, 128:256] = np.where(kk > qq, -BIG, 0.0)
    cst[:, 256:384] = np.arange(1, 129, dtype=f)[None, :]
    o["cst"] = cst
    return o


_NC = None


def kernel(**inputs):
    global _NC
    x = np.asarray(inputs["x"], dtype=np.float32)
    nb = x.shape[0]
    sh = _shared_layouts(inputs)
    in_maps = []
    for b in range(nb):
        m = dict(sh)
        m["xT"] = np.ascontiguousarray(x[b].T.reshape(8, 128, S).transpose(1, 0, 2))
        in_maps.append(m)
    if _NC is None:
        _NC = build()
    res = run_bass_kernel_spmd(_NC, in_maps, core_ids=list(range(nb)))
    out = np.empty_like(x)
    for b in range(nb):
        yT = np.asarray(res.results[b]["yT"], dtype=np.float32)
        out[b] = yT.transpose(1, 0, 2).reshape(1024, S).T
    return out
```
